# Optimizing a Trainium2 kernel written in Bass

```python
import jax, jax.numpy as jnp
from jax import lax
import numpy as np

D_MODEL = 1024
BATCH = 2
SEQ = 8192
DEPTH = 2

CTX_LEN = 256
GRID_W = 64
HEAD_DIM = 64
EPS = 1e-6
FNET_GROUPS = 4
FNET_GROUP_DIM = 64
FNET_W = FNET_GROUPS * FNET_GROUP_DIM
RET_HEADS = 4
RET_W = RET_HEADS * HEAD_DIM
RET_CHUNK = 128
ATT_Q_HEADS = 8
ATT_KV_HEADS = 2
ATT_GROUP = ATT_Q_HEADS // ATT_KV_HEADS
ATT_QW = ATT_Q_HEADS * HEAD_DIM
ATT_KVW = ATT_KV_HEADS * HEAD_DIM
ATT_BLOCK = 128
ROPE_THETA = 10000.0
CONV_W = 256
CONV_K = 31
N_BRANCH = 4
IN_SIZES = (FNET_W, RET_W, RET_W, RET_W, RET_W, ATT_QW, ATT_KVW, ATT_KVW, 2 * CONV_W, N_BRANCH * D_MODEL)
IN_COLS = FNET_W + 4 * RET_W + ATT_QW + 2 * ATT_KVW + 2 * CONV_W + N_BRANCH * D_MODEL
FFN_DIM = 2816
N_EXPERTS = 8
TOP_K = 2
MOE_BLOCK = 128
N_DENSE = (DEPTH + 1) // 2
N_MOE = DEPTH // 2

kernel_name = "hybrid_fnet_retention_gqa_conformer_moe_dit"


def rms_norm(x, g):
    xf = x.astype(jnp.float32)
    y = xf * lax.rsqrt(jnp.mean(xf * xf, axis=-1, keepdims=True) + EPS)
    return (y * g.astype(jnp.float32)).astype(x.dtype)


def layer_norm(x, g, b):
    xf = x.astype(jnp.float32)
    mu = jnp.mean(xf, axis=-1, keepdims=True)
    var = jnp.mean(jnp.square(xf - mu), axis=-1, keepdims=True)
    y = (xf - mu) * lax.rsqrt(var + EPS) * g.astype(jnp.float32) + b.astype(jnp.float32)
    return y.astype(x.dtype)


def modulate(h, shift, scale):
    return h * (1.0 + scale) + shift


def rope_tables(rows):
    row = jnp.broadcast_to(jnp.arange(rows)[:, None], (rows, GRID_W)).reshape(-1).astype(jnp.float32)
    col = jnp.broadcast_to(jnp.arange(GRID_W)[None, :], (rows, GRID_W)).reshape(-1).astype(jnp.float32)
    n_axis = HEAD_DIM // 4
    inv = ROPE_THETA ** (-jnp.arange(n_axis, dtype=jnp.float32) / n_axis)
    ang = jnp.concatenate([row[:, None] * inv, col[:, None] * inv], axis=-1)
    return jnp.cos(ang), jnp.sin(ang)


def apply_rope(x, cos, sin):
    xf = x.astype(jnp.float32)
    x1, x2 = xf[..., :HEAD_DIM // 2], xf[..., HEAD_DIM // 2:]
    c, s = cos[None, :, None, :], sin[None, :, None, :]
    return jnp.concatenate([x1 * c - x2 * s, x1 * s + x2 * c], axis=-1).astype(x.dtype)


def fourier_mix(u):
    b, l, _ = u.shape
    ug = u.astype(jnp.float32).reshape(b, l, FNET_GROUPS, FNET_GROUP_DIM)
    y = jnp.fft.fft2(ug, axes=(1, 3), norm="ortho").real
    return y.reshape(b, l, FNET_W).astype(u.dtype)


def retention_scan(q, k, v, log_gamma, state0):
    b, l, h, d = q.shape
    n = l // RET_CHUNK
    pos = jnp.arange(RET_CHUNK, dtype=jnp.float32)
    diff = pos[:, None] - pos[None, :]
    decay = jnp.where(diff[None] >= 0.0,
                      jnp.exp(jnp.maximum(diff, 0.0)[None] * log_gamma[:, None, None]), 0.0)
    xi = jnp.exp((pos[:, None] + 1.0) * log_gamma[None, :])
    zeta = jnp.exp((RET_CHUNK - 1.0 - pos[:, None]) * log_gamma[None, :])
    g_chunk = jnp.exp(RET_CHUNK * log_gamma)

    def chunks(t):
        return t.reshape(b, n, RET_CHUNK, h, d).transpose(1, 0, 2, 3, 4)

    def step(state, qkv):
        qc, kc, vc = qkv
        att = jnp.einsum('bihd,bjhd->bhij', qc, kc) * decay
        o = (jnp.einsum('bhij,bjhe->bihe', att, vc)
             + jnp.einsum('bihd,bhde->bihe', qc, state) * xi[None, :, :, None])
        state = (state * g_chunk[None, :, None, None]
                 + jnp.einsum('bjhd,bjhe->bhde', kc * zeta[None, :, :, None], vc))
        return state, o

    state, o = lax.scan(step, state0, (chunks(q), chunks(k), chunks(v)))
    return o.transpose(1, 0, 2, 3, 4).reshape(b, l, h, d), state


def bidir_retention(q_c, k_c, v_c, q_l, k_l, v_l, lg_f, lg_b):
    b = q_l.shape[0]
    zero = jnp.zeros((b, RET_HEADS, HEAD_DIM, HEAD_DIM), jnp.float32)

    def flip(t):
        return t[:, ::-1]

    oc_f, sc_f = retention_scan(q_c, k_c, v_c, lg_f, zero)
    ol_f, _ = retention_scan(q_l, k_l, v_l, lg_f, sc_f)
    oc_b, sc_b = retention_scan(flip(q_c), flip(k_c), flip(v_c), lg_b, zero)
    ol_b, _ = retention_scan(flip(q_l), flip(k_l), flip(v_l), lg_b, sc_b)
    return oc_f + flip(oc_b), ol_f + flip(ol_b)


def retention_out(o, gate, g, w):
    b, l = o.shape[:2]
    y = rms_norm(o, g.reshape(RET_HEADS, HEAD_DIM)).reshape(b, l, RET_W).astype(gate.dtype)
    return (y * jax.nn.silu(gate)) @ w


def gqa_attend(q, k, v):
    b, lq = q.shape[:2]
    qg = q.reshape(b, lq, ATT_KV_HEADS, ATT_GROUP, HEAD_DIM)
    s = jnp.einsum('bqkgd,bskd->bkgqs', qg, k, preferred_element_type=jnp.float32) * (HEAD_DIM ** -0.5)
    p = jax.nn.softmax(s, axis=-1).astype(v.dtype)
    return jnp.einsum('bkgqs,bskd->bqkgd', p, v).reshape(b, lq, ATT_QW)


def latent_attention(q, k_all, v_all):
    b, s = q.shape[:2]
    nb = s // ATT_BLOCK
    qb = q.reshape(b, nb, ATT_BLOCK, ATT_Q_HEADS, HEAD_DIM).transpose(1, 0, 2, 3, 4)
    o = lax.map(lambda qblk: gqa_attend(qblk, k_all, v_all), qb)
    return o.transpose(1, 0, 2, 3).reshape(b, s, ATT_QW)


def conformer_conv(u, dw_w, dw_b, ln_g, ln_b, w_out):
    a, gt = jnp.split(u, 2, axis=-1)
    y = a * jax.nn.sigmoid(gt)
    y = lax.conv_general_dilated(y, dw_w[:, None, :], window_strides=(1,),
                                 padding=[(CONV_K // 2, CONV_K // 2)],
                                 dimension_numbers=('NWC', 'WIO', 'NWC'),
                                 feature_group_count=CONV_W) + dw_b
    return jax.nn.silu(layer_norm(y, ln_g, ln_b)) @ w_out


def merge_branches(branches, gate_pre, w_out):
    b, l, _ = gate_pre.shape
    g = jax.nn.sigmoid(gate_pre).reshape(b, l, N_BRANCH, D_MODEL)
    y0, y1, y2, y3 = branches
    merged = g[:, :, 0] * y0 + g[:, :, 1] * y1 + g[:, :, 2] * y2 + g[:, :, 3] * y3
    return merged @ w_out


def token_mixers(h_l, h_c, cos, sin, w_in, fnet_w, dec_f, dec_b, ret_g, ret_w,
                 qn_g, kn_g, att_w, dw_w, dw_b, cln_g, cln_b, conv_out, w_out, with_ctx):
    b, n_lat, _ = h_l.shape
    n_ctx = h_c.shape[1]
    splits = [int(s) for s in np.cumsum(IN_SIZES)[:-1]]
    fn_l, rq_l, rk_l, rv_l, rg_l, aq_l, ak_l, av_l, cv_l, gt_l = jnp.split(h_l @ w_in, splits, axis=-1)
    fn_c, rq_c, rk_c, rv_c, rg_c, aq_c, ak_c, av_c, cv_c, gt_c = jnp.split(h_c @ w_in, splits, axis=-1)

    def rheads(t, n):
        return t.reshape(b, n, RET_HEADS, HEAD_DIM).astype(jnp.float32)

    k_scale = HEAD_DIM ** -0.5
    lg_f, lg_b = jax.nn.log_sigmoid(dec_f.astype(jnp.float32)), jax.nn.log_sigmoid(dec_b.astype(jnp.float32))
    ro_c, ro_l = bidir_retention(
        rheads(rq_c, n_ctx), rheads(rk_c, n_ctx) * k_scale, rheads(rv_c, n_ctx),
        apply_rope(rheads(rq_l, n_lat), cos, sin), apply_rope(rheads(rk_l, n_lat), cos, sin) * k_scale,
        rheads(rv_l, n_lat), lg_f, lg_b)
    y_ret_l = retention_out(ro_l, rg_l, ret_g, ret_w)

    def aheads(t, n, hh):
        return t.reshape(b, n, hh, HEAD_DIM)

    q_l = apply_rope(rms_norm(aheads(aq_l, n_lat, ATT_Q_HEADS), qn_g), cos, sin)
    k_l = apply_rope(rms_norm(aheads(ak_l, n_lat, ATT_KV_HEADS), kn_g), cos, sin)
    k_c = rms_norm(aheads(ak_c, n_ctx, ATT_KV_HEADS), kn_g)
    v_l, v_c = aheads(av_l, n_lat, ATT_KV_HEADS), aheads(av_c, n_ctx, ATT_KV_HEADS)
    k_all = jnp.concatenate([k_c, k_l], axis=1)
    v_all = jnp.concatenate([v_c, v_l], axis=1)
    y_att_l = latent_attention(q_l, k_all, v_all) @ att_w

    y_fn_l = fourier_mix(fn_l) @ fnet_w
    y_cv_l = conformer_conv(cv_l, dw_w, dw_b, cln_g, cln_b, conv_out)
    out_l = merge_branches((y_fn_l, y_ret_l, y_att_l, y_cv_l), gt_l, w_out)

    if not with_ctx:
        return out_l, None

    q_c = rms_norm(aheads(aq_c, n_ctx, ATT_Q_HEADS), qn_g)
    y_att_c = gqa_attend(q_c, k_c, v_c) @ att_w
    y_ret_c = retention_out(ro_c, rg_c, ret_g, ret_w)
    y_fn_c = fourier_mix(fn_c) @ fnet_w
    y_cv_c = conformer_conv(cv_c, dw_w, dw_b, cln_g, cln_b, conv_out)
    out_c = merge_branches((y_fn_c, y_ret_c, y_att_c, y_cv_c), gt_c, w_out)
    return out_l, out_c


def swiglu(x, wg, wu, wd):
    return (jax.nn.silu(x @ wg) * (x @ wu)) @ wd


def moe_swiglu(x, router_w, wg, wu, wd):
    b, l, d = x.shape
    xt = x.reshape(-1, d)
    n_tok = xt.shape[0]
    logits = jnp.dot(xt, router_w, preferred_element_type=jnp.float32)
    top_v, top_e = lax.top_k(logits, TOP_K)
    top_w = jax.nn.softmax(top_v, axis=-1)
    e = top_e.reshape(-1)
    tok = jnp.repeat(jnp.arange(n_tok, dtype=jnp.int32), TOP_K)
    w = top_w.reshape(-1)
    n_assign = e.shape[0]
    order = jnp.argsort(e)
    e_s, tok_s, w_s = e[order], tok[order], w[order]
    counts = jnp.bincount(e, length=N_EXPERTS)
    start = jnp.cumsum(counts) - counts
    padded = (counts + MOE_BLOCK - 1) // MOE_BLOCK * MOE_BLOCK
    pad_end = jnp.cumsum(padded)
    pad_start = pad_end - padded
    dest = pad_start[e_s] + jnp.arange(n_assign) - start[e_s]
    n_blocks = -(-n_assign // MOE_BLOCK) + N_EXPERTS
    n_rows = n_blocks * MOE_BLOCK
    buf_tok = jnp.full((n_rows,), n_tok, jnp.int32).at[dest].set(tok_s)
    buf_w = jnp.zeros((n_rows,), jnp.float32).at[dest].set(w_s)
    blk_e = jnp.minimum(jnp.searchsorted(pad_end, jnp.arange(n_blocks) * MOE_BLOCK, side='right'),
                        N_EXPERTS - 1)
    x_pad = jnp.concatenate([xt, jnp.zeros((1, d), xt.dtype)], axis=0)
    xb = x_pad[buf_tok].reshape(n_blocks, MOE_BLOCK, d)

    def expert_block(args):
        xblk, ei = args
        return swiglu(xblk, wg[ei], wu[ei], wd[ei])

    yb = lax.map(expert_block, (xb, blk_e)).reshape(n_rows, d)
    y = jnp.zeros((n_tok + 1, d), jnp.float32).at[buf_tok].add(yb.astype(jnp.float32) * buf_w[:, None])
    return y[:n_tok].astype(x.dtype).reshape(b, l, d)


def setup_inputs(seed: int = 0) -> dict:
    key = jax.random.key(seed)
    keys = iter(jax.random.split(key, 40))
    f32 = jnp.float32

    def nrm(shape, scale):
        return jax.random.normal(next(keys), shape, f32) * scale

    def gain(shape):
        return 1.0 + nrm(shape, 0.02)

    gamma_logit = jnp.log(2.0 ** (5.0 + jnp.arange(RET_HEADS, dtype=f32)) - 1.0)
    return {
        "x": nrm((BATCH, SEQ, D_MODEL), 1.0),
        "c": nrm((BATCH, D_MODEL), 1.0),
        "ctx": nrm((BATCH, CTX_LEN, D_MODEL), 1.0),
        "c_ctx": nrm((D_MODEL,), 1.0),
        "ada_w": nrm((DEPTH, D_MODEL, 6 * D_MODEL), 0.5 * D_MODEL ** -0.5),
        "ada_b": nrm((DEPTH, 6 * D_MODEL), 0.02),
        "norm1_g": gain((DEPTH, D_MODEL)),
        "norm2_g": gain((DEPTH, D_MODEL)),
        "w_in": nrm((DEPTH, D_MODEL, IN_COLS), D_MODEL ** -0.5),
        "fnet_w": nrm((DEPTH, FNET_W, D_MODEL), FNET_W ** -0.5),
        "ret_decay_fwd": gamma_logit[None, :] + nrm((DEPTH, RET_HEADS), 0.05),
        "ret_decay_bwd": gamma_logit[None, :] + nrm((DEPTH, RET_HEADS), 0.05),
        "ret_norm_g": gain((DEPTH, RET_W)),
        "ret_w": nrm((DEPTH, RET_W, D_MODEL), RET_W ** -0.5),
        "attn_qn_g": gain((DEPTH, HEAD_DIM)),
        "attn_kn_g": gain((DEPTH, HEAD_DIM)),
        "attn_w": nrm((DEPTH, ATT_QW, D_MODEL), ATT_QW ** -0.5),
        "conv_dw_w": nrm((DEPTH, CONV_K, CONV_W), CONV_K ** -0.5),
        "conv_dw_b": nrm((DEPTH, CONV_W), 0.02),
        "conv_ln_g": gain((DEPTH, CONV_W)),
        "conv_ln_b": nrm((DEPTH, CONV_W), 0.02),
        "conv_w_out": nrm((DEPTH, CONV_W, D_MODEL), CONV_W ** -0.5),
        "w_out": nrm((DEPTH, D_MODEL, D_MODEL), D_MODEL ** -0.5),
        "ffn_w_gate": nrm((N_DENSE, D_MODEL, FFN_DIM), D_MODEL ** -0.5),
        "ffn_w_up": nrm((N_DENSE, D_MODEL, FFN_DIM), D_MODEL ** -0.5),
        "ffn_w_down": nrm((N_DENSE, FFN_DIM, D_MODEL), FFN_DIM ** -0.5),
        "router_w": nrm((N_MOE, D_MODEL, N_EXPERTS), D_MODEL ** -0.5),
        "moe_w_gate": nrm((N_MOE, N_EXPERTS, D_MODEL, FFN_DIM), D_MODEL ** -0.5),
        "moe_w_up": nrm((N_MOE, N_EXPERTS, D_MODEL, FFN_DIM), D_MODEL ** -0.5),
        "moe_w_down": nrm((N_MOE, N_EXPERTS, FFN_DIM, D_MODEL), FFN_DIM ** -0.5),
    }


def reference(x, c, ctx, c_ctx, ada_w, ada_b, norm1_g, norm2_g, w_in, fnet_w, ret_decay_fwd, ret_decay_bwd,
              ret_norm_g, ret_w, attn_qn_g, attn_kn_g, attn_w, conv_dw_w, conv_dw_b, conv_ln_g, conv_ln_b,
              conv_w_out, w_out, ffn_w_gate, ffn_w_up, ffn_w_down, router_w, moe_w_gate, moe_w_up, moe_w_down):
    n_lat = x.shape[1]
    rows = n_lat // GRID_W
    cos, sin = rope_tables(rows)
    n_ctx = ctx.shape[1]
    silu_c = jax.nn.silu(c)
    silu_cc = jax.nn.silu(c_ctx)
    h_lat, h_ctx = x, ctx
    for i in range(DEPTH):
        with_ctx = i < DEPTH - 1
        mod_l = jnp.split((silu_c @ ada_w[i] + ada_b[i])[:, None, :], 6, axis=-1)
        mod_c = jnp.split((silu_cc @ ada_w[i] + ada_b[i])[None, None, :], 6, axis=-1)
        a_l = modulate(rms_norm(h_lat, norm1_g[i]), mod_l[0], mod_l[1])
        a_c = modulate(rms_norm(h_ctx, norm1_g[i]), mod_c[0], mod_c[1])
        m_l, m_c = token_mixers(a_l, a_c, cos, sin, w_in[i], fnet_w[i], ret_decay_fwd[i], ret_decay_bwd[i],
                                ret_norm_g[i], ret_w[i], attn_qn_g[i], attn_kn_g[i], attn_w[i],
                                conv_dw_w[i], conv_dw_b[i], conv_ln_g[i], conv_ln_b[i], conv_w_out[i],
                                w_out[i], with_ctx)
        h_lat = h_lat + mod_l[2] * m_l
        j = i // 2
        if i % 2 == 0:
            ffn = lambda t, j=j: swiglu(t, ffn_w_gate[j], ffn_w_up[j], ffn_w_down[j])
        else:
            ffn = lambda t, j=j: moe_swiglu(t, router_w[j], moe_w_gate[j], moe_w_up[j], moe_w_down[j])
        f_l = modulate(rms_norm(h_lat, norm2_g[i]), mod_l[3], mod_l[4])
        if with_ctx:
            h_ctx = h_ctx + mod_c[2] * m_c
            f_c = modulate(rms_norm(h_ctx, norm2_g[i]), mod_c[3], mod_c[4])
            y = ffn(jnp.concatenate([f_c, f_l], axis=1))
            h_ctx = h_ctx + mod_c[5] * y[:, :n_ctx]
            h_lat = h_lat + mod_l[5] * y[:, n_ctx:]
        else:
            h_lat = h_lat + mod_l[5] * ffn(f_l)
    return h_lat
```

```python
from contextlib import ExitStack
import os
import numpy as np
import concourse.bass as bass
import concourse.mybir as mybir
from concourse.bass_utils import run_bass_kernel_spmd

F32 = mybir.dt.float32
BF16 = mybir.dt.bfloat16
U32 = mybir.dt.uint32
ALU = mybir.AluOpType
AF = mybir.ActivationFunctionType
AX = mybir.AxisListType

ENGS = ['pe', 'act', 'dve', 'pool', 'sp']
NS_DMA = 8
SAME_ENG_GAP = 3
EPS = 1e-6


class Op:
    __slots__ = ('eng', 'fn', 'idx', 'waits', 'signal', 'sigval', 'dma', 'dsem', 'dval', 'dpre', 'ndma')


class Prog:
    def __init__(self, nc):
        self.nc = nc
        self.ops = {e: [] for e in ENGS}
        self.last_w = {}
        self.readers = {}
        self.known = {e: {f: -1 for f in ENGS} for e in ENGS}
        self.known_dma = {e: set() for e in ENGS}
        self.dma_count = {e: 0 for e in ENGS}
        self.dma_semtot = {e: [0] * NS_DMA for e in ENGS}
        self.es = ExitStack()
        self.sems = {}
        self.dsems = {}
        self.bar = None

    def sb(self, name, shape, dt):
        return self.es.enter_context(self.nc.sbuf_tensor(name, list(shape), dt))

    def ps(self, name, shape, dt):
        return self.es.enter_context(self.nc.psum_tensor(name, list(shape), dt))

    def add(self, eng, fn, reads=(), writes=(), dma=False, ndma=1, force=False):
        self.nadd = getattr(self, 'nadd', 0) + 1
        if self.nadd > int(os.environ.get('KOPS', '100000000')) and not force:
            return None
        op = Op()
        op.eng = eng; op.fn = fn; op.dma = dma; op.signal = False; op.sigval = None; op.ndma = ndma
        lst = self.ops[eng]
        op.idx = len(lst)
        deps = []
        pr = [r for r in reads if r.startswith('ps')]
        if pr:
            reads = [r for r in reads if not r.startswith('ps')]
            writes = list(writes) + [r for r in pr if r not in writes]
        if self.bar is not None:
            deps.append(self.bar)
        for r in reads:
            w = self.last_w.get(r)
            if w is not None:
                deps.append(w)
        for w_ in writes:
            w = self.last_w.get(w_)
            if w is not None:
                deps.append(w)
            deps.extend(self.readers.get(w_, ()))
        waits = []
        best = {}
        for d in deps:
            if d is op:
                continue
            if d.dma:
                if id(d) in self.known_dma[eng]:
                    continue
                self.known_dma[eng].add(id(d))
                waits.append(d)
            else:
                if d.idx <= self.known[eng][d.eng]:
                    continue
                if d.eng == eng and not force:
                    if eng == 'pe' or eng == 'sp':
                        continue
                    if op.idx - d.idx >= SAME_ENG_GAP and not dma and eng != 'pool':
                        continue
                b = best.get(d.eng)
                if b is None or d.idx > b.idx:
                    best[d.eng] = d
        for f, d in best.items():
            self.known[eng][f] = d.idx
            d.signal = True
            waits.append(d)
        op.waits = waits
        if dma:
            i = self.dma_count[eng]
            self.dma_count[eng] += 1
            s = i % NS_DMA
            op.dsem = s
            op.dpre = self.dma_semtot[eng][s]
            self.dma_semtot[eng][s] += 16 * ndma
            op.dval = self.dma_semtot[eng][s]
        for r in reads:
            self.readers.setdefault(r, []).append(op)
        for w_ in writes:
            self.last_w[w_] = op
            self.readers[w_] = []
        lst.append(op)
        return op

    def barrier(self):
        keys = set(self.last_w.keys()) | set(self.readers.keys())
        keys = list(keys)
        last = None
        for e in ENGS:
            own = self.ops[e][-1] if self.ops[e] else None
            last = self.add(e, (lambda en: en.nop()), reads=(), writes=keys, force=True)
            if own is not None and not own.dma and own.fn is not None and own not in last.waits and own.idx > self.known[e][e]:
                own.signal = True
                last.waits.append(own)
                self.known[e][e] = own.idx
        self.bar = last
        self.last_w = {}
        self.readers = {}

    def emit(self):
        nc = self.nc
        for e in ENGS:
            self.sems[e] = self.es.enter_context(nc.semaphore('s_' + e))
            self.dsems[e] = [self.es.enter_context(nc.semaphore('d_%s%d' % (e, i))) for i in range(NS_DMA)]
        for e in ENGS:
            c = 0
            for op in self.ops[e]:
                if op.signal and not op.dma:
                    c += 1
                    op.sigval = c
        block = self.es.enter_context(nc.Block())
        prog = self

        def run(ename, eng):
            for op in prog.ops[ename]:
                for d in op.waits:
                    if d.dma:
                        eng.wait_ge(prog.dsems[d.eng][d.dsem], d.dval)
                    else:
                        eng.wait_ge(prog.sems[d.eng], d.sigval)
                if op.fn is None:
                    continue
                if op.dma:
                    sem = prog.dsems[ename][op.dsem]
                    if op.dpre > 0:
                        eng.wait_ge(sem, op.dpre)
                    r = op.fn(eng)
                    if not isinstance(r, (list, tuple)):
                        r = [r]
                    assert len(r) == op.ndma
                    for ins in r:
                        ins.then_inc(sem, 16)
                else:
                    r = op.fn(eng)
                    if op.signal:
                        if isinstance(r, (list, tuple)):
                            r = r[-1]
                        r.then_inc(prog.sems[ename], 1)

        @block.tensor
        def _(eng):
            run('pe', eng)

        @block.scalar
        def _(eng):
            run('act', eng)

        @block.vector
        def _(eng):
            run('dve', eng)

        @block.gpsimd
        def _(eng):
            run('pool', eng)

        @block.sync
        def _(eng):
            run('sp', eng)

    def close(self):
        self.es.close()


CF_COLS = {}


def _layout_cf(L0):
    names = [('identF', 128), ('DIFFP', 128), ('DIFFN', 128), ('MLT', 128), ('MGT', 128), ('I2', 128),
             ('IP1', 128), ('IB', 128), ('P127', 1), ('PJ', 1), ('C128T', 64), ('on128', 128), ('on64', 64),
             ('twc', L0), ('tws', L0)]
    off = 0
    d = {}
    for n, w in names:
        d[n] = (off, w)
        off += w
    return d, off


def _layout_cb(L0):
    names = [('ident', 128), ('c128', 128), ('s128', 128), ('c64', L0), ('s64', L0), ('bd1', 1024), ('bd2', 1024),
             ('cc', 512), ('sc', 512)]
    off = 0
    d = {}
    for n, w in names:
        d[n] = (off, w)
        off += w
    return d, off


def make_consts(L):
    L0 = L // 128
    T = 256 + L
    cfl, ncf = _layout_cf(L0)
    cbl, ncb = _layout_cb(L0)
    cf = np.zeros((128, ncf), np.float32)
    cb = np.zeros((128, ncb), np.float32)

    def setf(n, a):
        o, w = cfl[n]
        cf[:a.shape[0], o:o + w] = a

    def setb(n, a):
        o, w = cbl[n]
        cb[:a.shape[0], o:o + w] = a

    p = np.arange(128)
    j = p[:, None].astype(np.float64)
    i = p[None, :].astype(np.float64)
    setf('identF', np.eye(128))
    setf('DIFFP', np.maximum(i - j, 0))
    setf('DIFFN', np.maximum(j - i, 0))
    setf('MLT', (j < i).astype(np.float64))
    setf('MGT', (j > i).astype(np.float64))
    setf('I2', 2 * np.eye(128))
    setf('IP1', np.broadcast_to(i + 1, (64, 128)))
    setf('IB', np.broadcast_to(128 - i, (64, 128)))
    setf('P127', 127 - j)
    setf('PJ', j)
    setf('C128T', np.full((64, 64), 128.0))
    setf('on128', np.full((128, 128), 1.0 / 256))
    setf('on64', np.full((64, 64), 1.0 / 64))
    l0 = np.arange(L0)[None, :].astype(np.float64)
    setf('twc', np.cos(2 * np.pi * j * l0 / L))
    setf('tws', np.sin(2 * np.pi * j * l0 / L))
    setb('ident', np.eye(128))
    setb('c128', np.cos(2 * np.pi * j * i / 128))
    setb('s128', np.sin(2 * np.pi * j * i / 128))
    a0 = np.arange(L0)[:, None].astype(np.float64)
    b0 = np.arange(L0)[None, :].astype(np.float64)
    setb('c64', np.cos(2 * np.pi * a0 * b0 / L0) / np.sqrt(L))
    setb('s64', np.sin(2 * np.pi * a0 * b0 / L0) / np.sqrt(L))
    c = np.arange(64)[:, None].astype(np.float64)
    jj = np.arange(64)[None, :].astype(np.float64)
    C64 = np.cos(2 * np.pi * c * jj / 64) / 8.0
    S64 = np.sin(2 * np.pi * c * jj / 64) / 8.0
    BDC = np.zeros((256, 256)); BDS = np.zeros((256, 256))
    for g in range(4):
        BDC[g * 64:(g + 1) * 64, g * 64:(g + 1) * 64] = C64
        BDS[g * 64:(g + 1) * 64, g * 64:(g + 1) * 64] = S64
    bd1 = np.concatenate([BDC, -BDS], 1)
    bd2 = np.concatenate([-BDS, -BDC], 1)
    setb('bd1', bd1.reshape(2, 128, 512).transpose(1, 0, 2).reshape(128, 1024))
    setb('bd2', bd2.reshape(2, 128, 512).transpose(1, 0, 2).reshape(128, 1024))
    lc = np.arange(256)[:, None].astype(np.float64)
    kc = np.arange(256)[None, :].astype(np.float64)
    CC = np.cos(2 * np.pi * lc * kc / 256) / 16.0
    SC = np.sin(2 * np.pi * lc * kc / 256) / 16.0
    setb('cc', CC.reshape(2, 128, 256).transpose(1, 0, 2).reshape(128, 512))
    setb('sc', SC.reshape(2, 128, 256).transpose(1, 0, 2).reshape(128, 512))
    rows = L // 64
    row = np.repeat(np.arange(rows), 64).astype(np.float32)
    col = np.tile(np.arange(64), rows).astype(np.float32)
    inv = (np.float32(10000.0) ** (-np.arange(16, dtype=np.float32) / np.float32(16))).astype(np.float32)
    ang = np.concatenate([row[:, None] * inv, col[:, None] * inv], -1).astype(np.float32)
    rope = np.zeros((T, 64), np.float32)
    rope[:256, :32] = 1.0
    rope[256:, :32] = np.cos(ang)
    rope[256:, 32:] = np.sin(ang)
    return cf, cb, rope


def build(L, FFC, debug=False, NQ=4):
    C = 256
    LQ = L // NQ
    T = C + L
    NT = T // 128
    L0 = L // 128
    HP = 2 if FFC % 2 == 0 else 1
    CH = FFC // HP
    FD = FFC * 128
    NE = 8
    cfl, ncf = _layout_cf(L0)
    cbl, ncb = _layout_cb(L0)
    nc = bass.Bass("TRN2", target_bir_lowering=False)
    P = Prog(nc)

    def din(name, shape, dt=F32):
        return nc.dram_tensor(name, list(shape), dt, kind="ExternalInput").ap()

    def dscr(name, shape, dt):
        return nc.dram_tensor(name, list(shape), dt, kind=("ExternalOutput" if debug else "Internal")).ap()

    x = din("x", [L, 1024]); ctx = din("ctx", [C, 1024]); ccols = din("ccols", [128, 16])
    ada_w = din("ada_w", [2, 1024, 6144]); ada_b = din("ada_b", [2, 6144])
    n1g = din("norm1_g", [2, 1024]); n2g = din("norm2_g", [2, 1024])
    w_in = din("w_in", [2, 1024, 6656])
    fnet_w = din("fnet_w", [2, 256, 1024]); ret_w = din("ret_w", [2, 256, 1024]); attn_w = din("attn_w", [2, 512, 1024])
    conv_wo = din("conv_w_out", [2, 256, 1024]); w_out = din("w_out", [2, 1024, 1024])
    ffn_wg = din("ffn_w_gate", [1, 1024, FD]); ffn_wu = din("ffn_w_up", [1, 1024, FD]); ffn_wd = din("ffn_w_down", [1, FD, 1024])
    moe_wg = din("moe_w_gate", [1, NE, 1024, FD]); moe_wu = din("moe_w_up", [1, NE, 1024, FD]); moe_wd = din("moe_w_down", [1, NE, FD, 1024])
    dec2 = din("dec2", [2, 8]); rng = din("rng", [2, 64, 4]); dwa = din("dwa", [2, 128, 62]); cba = din("cba", [2, 128, 6])
    gqk_in = din("gqk", [2, 640]); rwT = din("rwT", [NE, 1024])
    cf_in = din("cf", [128, ncf]); cb_in = din("cb", [128, ncb]); rope_in = din("rope", [T, 64])
    own_idx = nc.dram_tensor("own_idx", [128, LQ // 128], U32, kind="ExternalInput").ap()
    out = nc.dram_tensor("out", [LQ, 1024], F32, kind="ExternalOutput").ap()
    hq = dscr("hq", [LQ, 1024], F32)
    qtm = dscr("qtm", [T, 512], BF16)
    atm = dscr("atm", [T, 1024], BF16)
    rctm = dscr("rctm", [T, 512], BF16)
    qTq = dscr("qTq", [512, LQ], BF16)
    aTq1 = dscr("aTq1", [1024, LQ], BF16)
    brTq = dscr("brTq", [1280, LQ], BF16)
    qgroups = [(k_ * 512, 512) for k_ in range(LQ // 512)]
    aTq = dscr("aTq", [1024, LQ], BF16)

    hb = dscr("hb", [T, 1024], F32)
    aT = dscr("aT", [1024, T], BF16)
    modr = dscr("modr", [2, 128, 6144], F32)
    qkT = dscr("qkT", [1152, T], BF16)
    tmo = dscr("tmo", [T, 640], BF16)
    ab1 = dscr("ab1", [T, 512], BF16); ab2 = dscr("ab2", [T, 512], BF16)
    rgT = dscr("rgT", [256, T], BF16); cyT = dscr("cyT", [256, T], BF16)
    zs = dscr("zs", [128, L0, 512], BF16)
    ytm = dscr("ytm", [T, 256], BF16)
    brT = dscr("brT", [1280, T], BF16)

    groups = [(0, 256)] + [(256 + 512 * i_, 512) for i_ in range(L // 512)]

    cf = P.sb("cf_sb", [128, ncf], F32)
    cb = P.sb("cb_sb", [128, ncb], BF16)
    wts = P.sb("wts_sb", [128, NT, 8], F32)
    ARW = 46600
    arena = P.sb("arena", [128, ARW], F32)
    pw = [P.ps("pw%d" % i_, [128, 1024], F32) for i_ in range(4)]
    psf = [pw[i_ // 2][:, (i_ % 2) * 512:(i_ % 2 + 1) * 512] for i_ in range(8)]
    psb = pw[3][:, 512:1024].bitcast(BF16)
    PK = ['ps%d' % i_ for i_ in range(8)]
    st = {'off': 0, 'n': 0}

    def CF(n, parts=128):
        o, w = cfl[n]
        return cf[0:parts, o:o + w]

    def CB(n, parts=128):
        o, w = cbl[n]
        return cb[0:parts, o:o + w]

    def areset():
        st['off'] = 0

    def alloc(shape, dt, name=None):
        n = 1
        for s in shape[1:]:
            n *= s
        words = n if dt in (F32, U32) else (n + 1) // 2
        words = (words + 7) // 8 * 8
        assert st['off'] + words <= ARW, ("arena overflow", st['off'], words)
        v = arena[0:shape[0], st['off']:st['off'] + words]
        st['off'] += words
        if dt != F32:
            v = v.bitcast(dt)
        v = v[:, 0:n]
        if len(shape) == 3:
            v = v.rearrange("p (a b) -> p a b", a=shape[1])
        elif len(shape) == 4:
            v = v.rearrange("p (a b c) -> p a b c", a=shape[1], b=shape[2])
        st['n'] += 1
        return v

    def dma(q, o, i, r, w):
        P.add(q, (lambda e, o=o, i=i: e.dma_start(out=o, in_=i)), r, w, dma=True)

    def mmg(o, pairs, r, w, tr=False):
        def fn(e, o=o, pairs=pairs):
            n = len(pairs)
            ins = None
            for ii, (l, rh) in enumerate(pairs):
                ins = e.matmul(o, lhsT=l, rhs=rh, start=(ii == 0), stop=(ii == n - 1))
            return ins
        P.add('pe', fn, r, w)

    def mms(items, r, w):
        def fn(e, items=items):
            ins = None
            for o, pairs in items:
                n = len(pairs)
                for ii, (l, rh) in enumerate(pairs):
                    ins = e.matmul(o, lhsT=l, rhs=rh, start=(ii == 0), stop=(ii == n - 1))
            return ins
        P.add('pe', fn, r, w)

    def trs(items, r, w):
        def fn(e, items=items):
            ins = None
            for o, i in items:
                ins = e.transpose(o, i, CB('ident'))
            return ins
        P.add('pe', fn, r, w)

    def act(o, i, func, r, w, **kw):
        P.add('act', (lambda e, o=o, i=i, func=func, kw=kw: e.activation(out=o, in_=i, func=func, **kw)), r, w)

    def tt(eng, o, a, b, op, r, w):
        P.add(eng, (lambda e, o=o, a=a, b=b, op=op: e.tensor_tensor(out=o, in0=a, in1=b, op=op)), r, w)

    def ts(eng, o, a, s1, s2, op0, op1, r, w):
        if s2 is None:
            P.add(eng, (lambda e, o=o, a=a, s1=s1, op0=op0: e.tensor_scalar(out=o, in0=a, scalar1=s1, scalar2=None, op0=op0)), r, w)
        else:
            P.add(eng, (lambda e, o=o, a=a, s1=s1, s2=s2, op0=op0, op1=op1: e.tensor_scalar(out=o, in0=a, scalar1=s1, scalar2=s2, op0=op0, op1=op1)), r, w)

    def stt(o, a, s, b, op0, op1, r, w):
        P.add('dve', (lambda e, o=o, a=a, s=s, b=b, op0=op0, op1=op1: e.scalar_tensor_tensor(out=o, in0=a, scalar=s, in1=b, op0=op0, op1=op1)), r, w)

    def cp(eng, o, i, r, w):
        if eng == 'act':
            act(o, i, AF.Copy, r, w)
        else:
            P.add(eng, (lambda e, o=o, i=i: e.tensor_copy(out=o, in_=i)), r, w)

    def recip(o, i, r, w):
        P.add('dve', (lambda e, o=o, i=i: e.reciprocal(out=o, in_=i)), r, w)

    def red(o, i, op, r, w):
        P.add('dve', (lambda e, o=o, i=i, op=op: e.tensor_reduce(out=o, in_=i, axis=AX.X, op=op)), r, w)

    def mset(eng, o, v, w):
        P.add(eng, (lambda e, o=o, v=v: e.memset(o, v)), (), w)

    dma('sp', cf[:], cf_in, [], ['cf'])
    dma('pool', cb[:], cb_in, [], ['cb'])
    CK = ['cf', 'cb']

    def hsrc(layer0, t):
        if layer0:
            return ctx[t * 128:(t + 1) * 128, :] if t < 2 else x[(t - 2) * 128:(t - 1) * 128, :]
        return hb[t * 128:(t + 1) * 128, :]

    def stage_mod(i):
        areset()
        cc_t = alloc([128, 16], F32); scol = alloc([128, 16], F32); srep = alloc([128, 16, 128], F32)
        adw = [alloc([128, 8, 512], F32) for _ in range(2)]
        adb = [alloc([128, 512], F32) for _ in range(2)]
        mo = [alloc([128, 512], F32) for _ in range(2)]
        dma('sp', cc_t, ccols, [], ['cc_t'])
        act(scol, cc_t, AF.Silu, ['cc_t'], ['scol'])
        cp('dve', srep, scol.unsqueeze(2).to_broadcast([128, 16, 128]), ['scol'], ['srep'])
        for cg in range(12):
            r_ = cg % 2
            cs_ = slice(cg * 512, (cg + 1) * 512)
            dma('sp', adw[r_], ada_w[i, :, cs_].rearrange("(k p) n -> p k n", p=128), [], ['adw%d' % r_])
            dma('sp', adb[r_], ada_b[i, cs_].partition_broadcast(128), [], ['adb%d' % r_])
            for wh in range(2):
                mmg(psf[wh][:, :], [(srep[:, wh * 8 + k, :], adw[r_][:, k, :]) for k in range(8)],
                    ['srep', 'adw%d' % r_], [PK[wh]])
                tt('dve', mo[wh], psf[wh][:, :], adb[r_], ALU.add, [PK[wh], 'adb%d' % r_], ['mo%d' % wh])
                dma('sp', modr[wh, :, cs_], mo[wh], ['mo%d' % wh], ['modr'])
        P.barrier()

    def norm_mod_tile(t, wh, ht, ss, a1, a2, gs, sh, junk, ktag, a2f=None):
        gk = 'gs_l%d' % wh
        sk = 'sh_l%d' % wh
        act(junk, ht, AF.Square, [ktag + 'ht'], ['junk', ktag + 'ss'], accum_out=ss)
        act(ss, ss, AF.Sqrt, [ktag + 'ss'], [ktag + 'ss'], scale=1.0 / 1024, bias=EPS)
        recip(ss, ss, [ktag + 'ss'], [ktag + 'ss'])
        stt(a1, ht, ss[:, 0:1], gs[wh], ALU.mult, ALU.mult, [ktag + 'ht', ktag + 'ss', gk], ['a1'])
        if a2f is not None:
            tt('dve', a2f, a1, sh[wh], ALU.add, ['a1', sk], ['a2f'])
            cp('pool', a2, a2f, ['a2f'], ['a2'])
        else:
            tt('pool', a2, a1, sh[wh], ALU.add, ['a1', sk], ['a2'])

    def load_mod(i, gvec, shift_col, scale_col):
        gs = [alloc([128, 1024], F32) for _ in range(2)]
        sh = [alloc([128, 1024], F32) for _ in range(2)]
        grep = alloc([128, 1024], F32)
        dma('sp', grep, gvec[i].partition_broadcast(128), [], ['grep'])
        for wh in range(2):
            dma('sp', sh[wh], modr[wh, :, shift_col * 1024:(shift_col + 1) * 1024], [], ['sh_l%d' % wh])
            dma('sp', gs[wh], modr[wh, :, scale_col * 1024:(scale_col + 1) * 1024], [], ['gs_l%d' % wh])
            stt(gs[wh], gs[wh], 1.0, grep, ALU.add, ALU.mult, ['gs_l%d' % wh, 'grep'], ['gs_l%d' % wh])
        return gs, sh

    def stage_proj(i):
        layer0 = (i == 0)
        areset()
        wi = alloc([128, 8, 2560], BF16)
        dma('pool', wi, w_in[i, :, 0:2560].rearrange("(k p) n -> p k n", p=128), [], ['wi'])
        gs, sh = load_mod(i, n1g, 0, 1)
        gq = alloc([128, 640], F32)
        dma('sp', gq, gqk_in[i].partition_broadcast(128), [], ['gq'])
        ht = [alloc([128, 1024], F32) for _ in range(2)]
        ss = [alloc([128, 1], F32) for _ in range(2)]
        junk = alloc([128, 1024], BF16)
        a1 = alloc([128, 1024], F32); a2 = alloc([128, 1024], BF16)
        agrp = [alloc([128, 8, 512], BF16) for _ in range(2)]
        qkg = alloc([128, 9, 512], BF16)
        xq = alloc([128, 1152], F32); sq = alloc([128, 640], F32); ssq = alloc([128, 10], F32)
        xr = alloc([128, 18, 64], BF16)
        t1 = alloc([128, 18, 32], F32); t2 = alloc([128, 18, 32], F32); t3 = alloc([128, 18, 32], F32); t4 = alloc([128, 18, 32], F32)
        tmt = alloc([128, 640], BF16)
        cs_t = [alloc([128, 64], F32) for _ in range(2)]
        fnT = alloc([128, 2, 512], BF16); rgt = alloc([128, 2, 512], BF16); cy = alloc([128, 2, 512], BF16)
        sg = alloc([128, 512], F32)
        abt1 = alloc([128, 4, 512], BF16); abt2 = alloc([128, 4, 512], BF16)
        bd1 = CB('bd1').rearrange("p (a b) -> p a b", a=2); bd2 = CB('bd2').rearrange("p (a b) -> p a b", a=2)
        x3 = xq.rearrange("p (h d) -> p h d", h=18)
        xrf = xr.rearrange("p h d -> p (h d)")
        tix = 0
        ptiles = [g0 // 128 + j for (g0, N) in groups for j in range(N // 128)]

        def pload(k):
            t_ = ptiles[k]
            dma('sp', ht[k % 2], hsrc(layer0, t_), [], ['r%dht' % (k % 2)])
            dma('sp', cs_t[k % 2], rope_in[t_ * 128:(t_ + 1) * 128, :], [], ['r%dcs' % (k % 2)])

        for gi, (g0, N) in enumerate(groups):
            gb = gi % 2
            ag = agrp[gb]
            AK = 'ag%d' % gb
            wh = 1 if gi == 0 else 0
            nt = N // 128
            for j in range(nt):
                t = g0 // 128 + j
                r_ = tix % 2
                tix += 1
                k_ = 'r%d' % r_
                js = slice(j * 128, (j + 1) * 128)
                if tix == 1:
                    pload(0)
                if tix < len(ptiles):
                    pload(tix)
                norm_mod_tile(t, wh, ht[r_], ss[r_], a1, a2, gs, sh, junk, k_)
                if not layer0:
                    dma('sp', atm[t * 128:(t + 1) * 128, :], a2, ['a2'], ['atm'])
                for hf in range(2):
                    mms([(psf[7][:, kk * 128:(kk + 1) * 128], [(a2[:, (4 * hf + kk) * 128:(4 * hf + kk + 1) * 128], CB('ident'))]) for kk in range(4)],
                        ['a2', 'cb'], [PK[7]])
                    cp('act', ag[:, 4 * hf:4 * hf + 4, js], psf[7][:, :].rearrange("p (k t) -> p k t", k=4), [PK[7]], [AK])
                lhs = [ag[:, k, js] for k in range(8)]
                mms([(psf[0][:, 0:512], [(lhs[k], wi[:, k, 1280:1792]) for k in range(8)]),
                     (psf[1][:, 0:128], [(lhs[k], wi[:, k, 1792:1920]) for k in range(8)]),
                     (psf[1][:, 128:384], [(lhs[k], wi[:, k, 256:512]) for k in range(8)]),
                     (psf[2][:, 0:256], [(lhs[k], wi[:, k, 512:768]) for k in range(8)]),
                     (psf[2][:, 256:384], [(lhs[k], wi[:, k, 1920:2048]) for k in range(8)]),
                     (psf[3][:, 0:256], [(lhs[k], wi[:, k, 768:1024]) for k in range(8)])],
                    [AK, 'wi'], [PK[0], PK[1], PK[2], PK[3]])
                cp('act', xq[:, 0:512], psf[0][:, 0:512], [PK[0]], ['xq_a'])
                cp('dve', xq[:, 512:896], psf[1][:, 0:384], [PK[1]], ['xq_b'])
                act(xq[:, 896:1152], psf[2][:, 0:256], AF.Copy, [PK[2]], ['xq_c'], scale=0.125)
                cp('dve', tmt[:, 0:128], psf[2][:, 256:384], [PK[2]], ['tmt_a'])
                cp('act', tmt[:, 128:384], psf[3][:, 0:256], [PK[3]], ['tmt_b'])
                tt('dve', sq, xq[:, 0:640], xq[:, 0:640], ALU.mult, ['xq_a', 'xq_b'], ['sq'])
                red(ssq, sq.rearrange("p (h d) -> p h d", h=10), ALU.add, ['sq'], ['ssq'])
                act(ssq, ssq, AF.Sqrt, ['ssq'], ['ssq'], scale=1.0 / 64, bias=EPS)
                recip(ssq, ssq, ['ssq'], ['ssq'])
                tt('dve', x3[:, 0:10, :], x3[:, 0:10, :], ssq.unsqueeze(2).to_broadcast([128, 10, 64]), ALU.mult,
                   ['xq_a', 'xq_b', 'ssq'], ['xq_a', 'xq_b'])
                tt('pool', xq[:, 0:640], xq[:, 0:640], gq, ALU.mult, ['xq_a', 'xq_b', 'gq'], ['xq_a', 'xq_b'])
                cb_ = cs_t[r_][:, 0:32].unsqueeze(1).to_broadcast([128, 18, 32])
                sb_ = cs_t[r_][:, 32:64].unsqueeze(1).to_broadcast([128, 18, 32])
                XQ = ['xq_a', 'xq_b', 'xq_c']
                tt('dve', t1, x3[:, :, 0:32], cb_, ALU.mult, XQ + [k_ + 'cs'], ['t1'])
                tt('pool', t2, x3[:, :, 32:64], sb_, ALU.mult, XQ + [k_ + 'cs'], ['t2'])
                tt('dve', xr[:, :, 0:32], t1, t2, ALU.subtract, ['t1', 't2'], ['xr_a'])
                tt('pool', t3, x3[:, :, 0:32], sb_, ALU.mult, XQ + [k_ + 'cs'], ['t3'])
                tt('dve', t4, x3[:, :, 32:64], cb_, ALU.mult, XQ + [k_ + 'cs'], ['t4'])
                tt('pool', xr[:, :, 32:64], t3, t4, ALU.add, ['t3', 't4'], ['xr_b'])
                if not layer0:
                    dma('sp', qtm[t * 128:(t + 1) * 128, :], xrf[:, 0:512], ['xr_a', 'xr_b'], ['qtm'])
                cp('pool', tmt[:, 384:640], xrf[:, 896:1152], ['xr_a', 'xr_b'], ['tmt_c'])
                dma('sp', tmo[t * 128:(t + 1) * 128, :], tmt, ['tmt_a', 'tmt_b', 'tmt_c'], ['tmo'])
                for (b0, nb_) in ((0, 4), (4, 4), (8, 1)):
                    mms([(psf[7][:, kk * 128:(kk + 1) * 128], [(xrf[:, (b0 + kk) * 128:(b0 + kk + 1) * 128], CB('ident'))]) for kk in range(nb_)],
                        ['xr_a', 'xr_b', 'cb'], [PK[7]])
                    cp('act', qkg[:, b0:b0 + nb_, js], psf[7][:, 0:nb_ * 128].rearrange("p (k t) -> p k t", k=nb_), [PK[7]], ['qkg'])
            dma('sp', qkT[:, g0:g0 + N].rearrange("(b p) t -> p b t", p=128), qkg[:, :, 0:N], ['qkg'], ['qkT'])
            dma('sp', aT[:, g0:g0 + N].rearrange("(k p) t -> p k t", p=128), ag[:, :, 0:N], [AK], ['aT'])
            rhs = [ag[:, k, 0:N] for k in range(8)]
            for oc in range(2):
                mmg(psf[4][:, 0:N], [(wi[:, k, oc * 128:(oc + 1) * 128], rhs[k]) for k in range(8)], [AK, 'wi'], [PK[4]])
                cp('act', fnT[:, oc, 0:N], psf[4][:, 0:N], [PK[4]], ['fnT'])
                mmg(psf[5][:, 0:N], [(wi[:, k, 1024 + oc * 128:1024 + (oc + 1) * 128], rhs[k]) for k in range(8)], [AK, 'wi'], [PK[5]])
                act(rgt[:, oc, 0:N], psf[5][:, 0:N], AF.Silu, [PK[5]], ['rgt'])
            dma('sp', rgT[:, g0:g0 + N].rearrange("(c p) t -> p c t", p=128), rgt[:, :, 0:N], ['rgt'], ['rgT'])
            for oc in range(2):
                mmg(psf[4][:, 0:N], [(wi[:, k, 2048 + oc * 128:2048 + (oc + 1) * 128], rhs[k]) for k in range(8)], [AK, 'wi'], [PK[4]])
                mmg(psf[5][:, 0:N], [(wi[:, k, 2304 + oc * 128:2304 + (oc + 1) * 128], rhs[k]) for k in range(8)], [AK, 'wi'], [PK[5]])
                act(sg[:, 0:N], psf[5][:, 0:N], AF.Sigmoid, [PK[5]], ['sg'])
                tt('dve', cy[:, oc, 0:N], psf[4][:, 0:N], sg[:, 0:N], ALU.mult, [PK[4], 'sg'], ['cy'])
            dma('sp', cyT[:, g0:g0 + N].rearrange("(c p) t -> p c t", p=128), cy[:, :, 0:N], ['cy'], ['cyT'])
            for j in range(nt):
                js = slice(j * 128, (j + 1) * 128)
                mmg(psf[6][:, :], [(fnT[:, fc, js], bd1[:, fc, :]) for fc in range(2)], ['fnT', 'cb'], [PK[6]])
                cp('act', abt1[:, j, :], psf[6][:, :], [PK[6]], ['abt1'])
                mmg(psf[6][:, :], [(fnT[:, fc, js], bd2[:, fc, :]) for fc in range(2)], ['fnT', 'cb'], [PK[6]])
                cp('dve', abt2[:, j, :], psf[6][:, :], [PK[6]], ['abt2'])
            dma('sp', ab1[g0:g0 + N, :].rearrange("(j p) f -> p j f", p=128), abt1[:, 0:nt, :], ['abt1'], ['ab1'])
            dma('sp', ab2[g0:g0 + N, :].rearrange("(j p) f -> p j f", p=128), abt2[:, 0:nt, :], ['abt2'], ['ab2'])
        P.barrier()

    def stage_attn(i, with_ctx, own=False):
        areset()
        Qsrc = qTq if own else qkT
        Bdst = brTq if own else brT
        glist = qgroups if own else [g for gi_, g in enumerate(groups) if not (gi_ == 0 and not with_ctx)]
        ka = alloc([128, T], BF16)
        va = alloc([128, NT, 128], BF16)
        qt = alloc([128, 4, 512], BF16)
        mset('pool', ka[64:128, :], 0.0, ['ka_z'])
        mset('pool', qt[64:128, :, :], 0.0, ['qt_z'])
        pt = [alloc([128, 2, 512], BF16) for _ in range(3)]
        rc = alloc([128, 512], F32)
        ot = alloc([64, 4, 512], BF16)
        mset('pool', va[:, :, 64:128], 1.0, ['va1'])
        cnt = 0
        for kv in range(2):
            dma('sp', ka[0:64, :], qkT[512 + kv * 64:512 + (kv + 1) * 64, :], [], ['ka'])
            dma('sp', va[:, :, 0:64], tmo[:, kv * 64:(kv + 1) * 64].rearrange("(n p) d -> p n d", p=128), [], ['va0'])
            for (g0, N) in glist:
                kbs = [0, 1] if (g0 == 0 and not own) else list(range(NT))
                dma('sp', qt[0:64, :, 0:N], Qsrc[kv * 256:(kv + 1) * 256, g0:g0 + N].rearrange("(h d) t -> d h t", d=64), [], ['qt'])
                for pr in range(2):
                    its = list(kbs)
                    base = cnt
                    cnt += len(its)

                    def emit_S(ii, its=its, base=base, N=N, pr=pr):
                        kb = its[ii]
                        r_ = (base + ii) % 3
                        mms([(pw[r_][:, a_ * 512:a_ * 512 + N], [(ka[:, kb * 128:(kb + 1) * 128], qt[:, 2 * pr + a_, 0:N])]) for a_ in range(2)],
                            ['ka', 'qt', 'ka_z', 'qt_z'], [PK[2 * r_], PK[2 * r_ + 1]])
                        act(pt[r_][:, :, 0:N], pw[r_][:, :].rearrange("p (a b) -> p a b", a=2)[:, :, 0:N], AF.Exp,
                            [PK[2 * r_], PK[2 * r_ + 1]], ['pt%d' % r_], scale=0.125)

                    def emit_PV(ii, its=its, base=base, N=N, pr=pr):
                        kb = its[ii]
                        r_ = (base + ii) % 3

                        def fn(e, kb=kb, r_=r_, N=N, s_=(ii == 0), sp_=(ii == len(its) - 1)):
                            ins = None
                            for a_ in range(2):
                                ins = e.matmul(psf[6 + a_][:, 0:N], lhsT=va[:, kb, :], rhs=pt[r_][:, a_, 0:N], start=s_, stop=sp_)
                            return ins
                        P.add('pe', fn, ['va0', 'va1', 'pt%d' % r_], [PK[6], PK[7]])

                    for ii in range(min(2, len(its))):
                        emit_S(ii)
                    for ii in range(len(its)):
                        if ii + 2 < len(its):
                            emit_S(ii + 2)
                        emit_PV(ii)
                    for a_ in range(2):
                        hh = 2 * pr + a_
                        recip(rc[64:128, 0:N], psf[6 + a_][64:128, 0:N], [PK[6 + a_]], ['rc'])
                        tt('dve', ot[:, hh, 0:N], psf[6 + a_][0:64, 0:N], rc[64:128, 0:N], ALU.mult, [PK[6 + a_], 'rc'], ['ot'])
                dma('sp', Bdst[512 + kv * 256:512 + (kv + 1) * 256, g0:g0 + N].rearrange("(h d) t -> d h t", d=64), ot[:, :, 0:N], ['ot'], ['brT'])
        P.barrier()

    def stage_fnet(i, with_ctx, tr=True):
        areset()
        LB = min(16, L0)
        x1b = alloc([128, LB, 512], BF16); x2b = alloc([128, LB, 512], BF16); zb = alloc([128, LB, 512], BF16)
        u1 = alloc([128, 256], F32); u2 = alloc([128, 256], F32)
        z3 = alloc([L0, 16, 512], BF16); ysb = alloc([L0, 16, 256], BF16)
        xc = alloc([128, 2, 512], BF16); yc = alloc([128, 2, 256], BF16)
        yt = [alloc([128, 256], BF16) for _ in range(2)]
        yT = [alloc([128, 2, 128], BF16) for _ in range(2)]
        a1v = ab1[256:256 + L, :].rearrange("(p a) f -> p a f", a=L0)
        a2v = ab2[256:256 + L, :].rearrange("(p a) f -> p a f", a=L0)
        twc = CF('twc'); tws = CF('tws')
        for blk in range(L0 // LB):
            bs = slice(blk * LB, (blk + 1) * LB)
            dma('sp', x1b, a1v[:, bs, :], [], ['x1b'])
            dma('sp', x2b, a2v[:, bs, :], [], ['x2b'])
            for a in range(LB):
                l0 = blk * LB + a
                pz = psf[a % 2]
                k_ = PK[a % 2]
                mmg(pz[:, :], [(CB('c128'), x1b[:, a, :]), (CB('s128'), x2b[:, a, :])], ['x1b', 'x2b', 'cb'], [k_])
                ts('dve', u1, pz[:, 256:512], tws[:, l0:l0 + 1], None, ALU.mult, None, [k_, 'cf'], ['u1'])
                stt(zb[:, a, 0:256], pz[:, 0:256], twc[:, l0:l0 + 1], u1, ALU.mult, ALU.add, [k_, 'u1', 'cf'], ['zb'])
                ts('dve', u2, pz[:, 0:256], tws[:, l0:l0 + 1], None, ALU.mult, None, [k_, 'cf'], ['u2'])
                stt(zb[:, a, 256:512], pz[:, 256:512], twc[:, l0:l0 + 1], u2, ALU.mult, ALU.subtract, [k_, 'u2', 'cf'], ['zb'])
            dma('sp', zs[:, bs, :], zb, ['zb'], ['zs'])
        P.barrier()
        yv = ytm[256:256 + L, :].rearrange("(k0 k1) f -> k0 k1 f", k1=128)
        for blk in range(8):
            bs = slice(blk * 16, (blk + 1) * 16)
            dma('sp', z3, zs[bs, :, :].rearrange("k a f -> a k f"), [], ['z3'])
            for kk in range(16):
                pz = psf[2 + (kk // 2) % 2]
                k_ = PK[2 + (kk // 2) % 2]
                mmg(pz[0:L0, (kk % 2) * 256:(kk % 2 + 1) * 256], [(CB('c64', L0), z3[:, kk, 0:256]), (CB('s64', L0), z3[:, kk, 256:512])],
                    ['z3', 'cb'], [k_])
                if kk % 2 == 1:
                    cp('act', ysb[:, kk - 1:kk + 1, :], pz[0:L0, :].rearrange("p (a b) -> p a b", a=2), [k_], ['ysb'])
            dma('sp', yv[:, bs, :], ysb, ['ysb'], ['ytm'])
        if with_ctx:
            dma('sp', xc, ab1[0:256, :].rearrange("(c p) f -> p c f", p=128), [], ['xc'])
            ccv = CB('cc').rearrange("p (a b) -> p a b", a=2); scv = CB('sc').rearrange("p (a b) -> p a b", a=2)
            for kc in range(2):
                ks = slice(kc * 128, (kc + 1) * 128)
                mmg(psf[4 + kc][:, 0:256], [(ccv[:, lc, ks], xc[:, lc, 0:256]) for lc in range(2)] + [(scv[:, lc, ks], xc[:, lc, 256:512]) for lc in range(2)],
                    ['xc', 'cb'], [PK[4 + kc]])
                cp('act', yc[:, kc, :], psf[4 + kc][:, 0:256], [PK[4 + kc]], ['yc'])
            dma('sp', ytm[0:256, :].rearrange("(c p) f -> p c f", p=128), yc, ['yc'], ['ytm'])
        P.barrier()
        for t in (range(0 if with_ctx else 2, NT) if tr else []):
            r_ = t % 2
            dma('sp', yt[r_], ytm[t * 128:(t + 1) * 128, :], [], ['yt%d' % r_])
            trs([(psb[:, c_ * 128:(c_ + 1) * 128], yt[r_][:, c_ * 128:(c_ + 1) * 128]) for c_ in range(2)], ['yt%d' % r_, 'cb'], ['ps7'])
            cp('act', yT[r_], psb[:, 0:256].rearrange("p (a b) -> p a b", a=2), ['ps7'], ['yT%d' % r_])
            dma('sp', brT[0:256, t * 128:(t + 1) * 128].rearrange("(c p) t -> p c t", p=128), yT[r_], ['yT%d' % r_], ['brT'])
        P.barrier()

    def stage_ret(i, with_ctx):
        areset()
        Sf_all = alloc([64, NT, 4, 64], BF16); Sb_all = alloc([64, NT, 4, 64], BF16)
        lgt = alloc([128, 8], F32); e1 = alloc([128, 8], F32)
        dcomb = alloc([128, 4, 128], BF16); ef = alloc([128, 128], F32); eb = alloc([128, 128], F32)
        xif = alloc([64, 4, 128], BF16); xib = alloc([64, 4, 128], BF16)
        zf = alloc([128, 4], F32); zbk = alloc([128, 4], F32)
        gcf = alloc([64, 4, 64], F32); gcb = alloc([64, 4, 64], F32)
        grn = alloc([64, 4], F32)
        S = alloc([64, 4, 64], F32)
        tmn = [alloc([128, 640], BF16) for _ in range(2)]
        kz = alloc([128, 4, 64], BF16)
        qk = [alloc([64, 8, 128], BF16) for _ in range(2)]
        rgn = [alloc([64, 4, 128], BF16) for _ in range(2)]
        am = alloc([128, 4, 128], BF16); qxf = alloc([64, 4, 128], BF16); qxb = alloc([64, 4, 128], BF16)
        osq = alloc([64, 512], F32); rs = alloc([64, 512], F32); o1 = alloc([64, 512], F32)
        ob = [alloc([64, 4, 128], BF16) for _ in range(2)]
        dma('sp', lgt, dec2[i].partition_broadcast(128), [], ['lgt'])
        dma('sp', grn, rng[i], [], ['grn'])
        act(e1, lgt, AF.Exp, ['lgt'], ['e1'], scale=-1.0)
        act(e1, e1, AF.Ln, ['e1'], ['e1'], bias=1.0)
        ts('dve', lgt, e1, -1.0, None, ALU.mult, None, ['e1'], ['lgt'])
        for h in range(4):
            act(ef, CF('DIFFP'), AF.Exp, ['lgt', 'cf'], ['ef'], scale=lgt[:, h:h + 1])
            tt('dve', ef, ef, CF('MLT'), ALU.mult, ['ef', 'cf'], ['ef'])
            act(eb, CF('DIFFN'), AF.Exp, ['lgt', 'cf'], ['eb'], scale=lgt[:, 4 + h:5 + h])
            tt('dve', eb, eb, CF('MGT'), ALU.mult, ['eb', 'cf'], ['eb'])
            tt('dve', ef, ef, eb, ALU.add, ['ef', 'eb'], ['ef'])
            tt('dve', dcomb[:, h, :], ef, CF('I2'), ALU.add, ['ef', 'cf'], ['dcomb'])
            act(xif[:, h, :], CF('IP1', 64), AF.Exp, ['lgt', 'cf'], ['xif'], scale=lgt[0:64, h:h + 1])
            act(xib[:, h, :], CF('IB', 64), AF.Exp, ['lgt', 'cf'], ['xib'], scale=lgt[0:64, 4 + h:5 + h])
            act(zf[:, h:h + 1], CF('P127'), AF.Exp, ['lgt', 'cf'], ['zf'], scale=lgt[:, h:h + 1])
            act(zbk[:, h:h + 1], CF('PJ'), AF.Exp, ['lgt', 'cf'], ['zbk'], scale=lgt[:, 4 + h:5 + h])
            act(gcf[:, h, :], CF('C128T', 64), AF.Exp, ['lgt', 'cf'], ['gcf'], scale=lgt[0:64, h:h + 1])
            act(gcb[:, h, :], CF('C128T', 64), AF.Exp, ['lgt', 'cf'], ['gcb'], scale=lgt[0:64, 4 + h:5 + h])

        S2 = alloc([64, 4, 64], F32)
        kz2 = alloc([128, 4, 64], BF16)
        tmb = [alloc([128, 640], BF16) for _ in range(2)]

        def mk_sweep(order, z, zk, gc, gk, S_all, sk, Sx, skey, kzx, kzkey, tmx, tmkey, psx, pskey):
            def init():
                mset('dve', Sx, 0.0, [skey])

            def step(ii):
                n = order[ii]
                r_ = ii % 2
                cp('act', S_all[:, n], Sx, [skey], [sk])
                dma('sp', tmx[r_], tmo[n * 128:(n + 1) * 128, :], [], [tmkey + str(r_)])
                tt('dve', kzx, tmx[r_][:, 384:640].rearrange("p (h d) -> p h d", h=4), z.unsqueeze(2).to_broadcast([128, 4, 64]), ALU.mult,
                   [tmkey + str(r_), zk], [kzkey])
                mms([(psx[0:64, h * 64:(h + 1) * 64], [(kzx[:, h, :], tmx[r_][:, 128 + h * 64:128 + (h + 1) * 64])]) for h in range(4)],
                    [kzkey, tmkey + str(r_)], [pskey])
                tt('dve', Sx, Sx, gc, ALU.mult, [skey, gk], [skey])
                tt('dve', Sx.rearrange("p h d -> p (h d)"), Sx.rearrange("p h d -> p (h d)"), psx[0:64, 0:256], ALU.add, [skey, pskey], [skey])
            return init, step

        fi, fs_ = mk_sweep(list(range(NT)), zf, 'zf', gcf, 'gcf', Sf_all, 'Sf', S, 'S', kz, 'kz', tmn, 'tmn', psf[0], PK[0])
        bi, bs_ = mk_sweep([1, 0] + list(range(NT - 1, 1, -1)), zbk, 'zbk', gcb, 'gcb', Sb_all, 'Sb', S2, 'S2', kz2, 'kz2', tmb, 'tmb', psf[4], PK[4])
        fi(); bi()
        for ii in range(NT):
            fs_(ii)
            bs_(ii)
        amL = [alloc([128, 4, 128], BF16) for _ in range(2)]
        qxfL = [alloc([64, 4, 128], BF16) for _ in range(2)]; qxbL = [alloc([64, 4, 128], BF16) for _ in range(2)]
        osqL = [alloc([64, 512], F32) for _ in range(2)]; rsL = [alloc([64, 512], F32) for _ in range(2)]; o1L = [alloc([64, 512], F32) for _ in range(2)]
        chunks = list(range(0 if with_ctx else 2, NT))

        def oload(ii):
            n = chunks[ii]
            r_ = ii % 2
            ns = slice(n * 128, (n + 1) * 128)
            dma('sp', qk[r_], qkT[640:1152, ns].rearrange("(h d) t -> d h t", d=64), [], ['qk%d' % r_])
            dma('sp', tmn[r_], tmo[ns, :], [], ['tmn%d' % r_])
            dma('sp', rgn[r_], rgT[:, ns].rearrange("(h e) t -> e h t", e=64), [], ['rgn%d' % r_])

        if chunks:
            oload(0)
        for ii, n in enumerate(chunks):
            if ii + 1 < len(chunks):
                oload(ii + 1)
            r_ = ii % 2
            q_ = str(r_)
            pA, pB, pC = psf[1 + 4 * r_], psf[2 + 4 * r_], psf[3 + 4 * r_]
            kA, kB, kC = PK[1 + 4 * r_], PK[2 + 4 * r_], PK[3 + 4 * r_]
            am_ = amL[r_]; qxf_ = qxfL[r_]; qxb_ = qxbL[r_]; osq_ = osqL[r_]; rs_ = rsL[r_]; o1_ = o1L[r_]
            ns = slice(n * 128, (n + 1) * 128)
            mms([(pA[:, h * 128:(h + 1) * 128], [(qk[r_][:, 4 + h, :], qk[r_][:, h, :])]) for h in range(4)], ['qk%d' % r_], [kA])
            tt('dve', am_.rearrange("p h t -> p (h t)"), pA[:, :], dcomb.rearrange("p h t -> p (h t)"), ALU.mult, [kA, 'dcomb'], ['am' + q_])
            tt('dve', qxf_, qk[r_][:, 0:4, :], xif, ALU.mult, ['qk%d' % r_, 'xif'], ['qxf' + q_])
            tt('pool', qxb_, qk[r_][:, 0:4, :], xib, ALU.mult, ['qk%d' % r_, 'xib'], ['qxb' + q_])
            mms([(pB[0:64, h * 128:(h + 1) * 128],
                  [(tmn[r_][:, 128 + h * 64:128 + (h + 1) * 64], am_[:, h, :]), (Sf_all[:, n, h, :], qxf_[:, h, :]), (Sb_all[:, n, h, :], qxb_[:, h, :])])
                 for h in range(4)], ['tmn%d' % r_, 'am' + q_, 'Sf', 'Sb', 'qxf' + q_, 'qxb' + q_], [kB])
            act(osq_, pB[0:64, :], AF.Square, [kB], ['osq' + q_])
            mmg(pC[0:64, :], [(CF('on64', 64), osq_)], ['osq' + q_, 'cf'], [kC])
            act(rs_, pC[0:64, :], AF.Sqrt, [kC], ['rs' + q_], bias=EPS)
            recip(rs_, rs_, ['rs' + q_], ['rs' + q_])
            tt('dve', o1_, pB[0:64, :], rs_, ALU.mult, [kB, 'rs' + q_], ['o1' + q_])
            o13 = o1_.rearrange("p (h t) -> p h t", h=4)
            tt('dve', o13, o13, grn.unsqueeze(2).to_broadcast([64, 4, 128]), ALU.mult, ['o1' + q_, 'grn'], ['o1' + q_])
            tt('pool', ob[r_], o13, rgn[r_], ALU.mult, ['o1' + q_, 'rgn%d' % r_], ['ob%d' % r_])
            dma('sp', brT[256:512, ns].rearrange("(h e) t -> e h t", e=64), ob[r_], ['ob%d' % r_], ['brT'])
        P.barrier()

    def stage_conv(i, with_ctx):
        areset()
        yb = alloc([128, 2, L + 30], BF16)
        ybc = alloc([128, 2, C + 30], BF16)
        diag = alloc([128, 2, 31, 128], BF16)
        dgw = alloc([128, 62], F32); cbp = alloc([128, 6], F32)
        z = alloc([128, 2, 512], F32); zq = alloc([128, 2, 512], F32)
        mu = alloc([128, 512], F32); var = alloc([128, 512], F32)
        co = [alloc([128, 2, 512], BF16) for _ in range(2)]
        dma('sp', dgw, dwa[i], [], ['dgw'])
        dma('sp', cbp, cba[i], [], ['cbp'])
        mset('pool', yb[:, :, 0:15], 0.0, ['yb_h0'])
        mset('pool', yb[:, :, L + 15:L + 30], 0.0, ['yb_h1'])
        mset('pool', ybc[:, :, 0:15], 0.0, ['ybc_h0'])
        mset('pool', ybc[:, :, C + 15:C + 30], 0.0, ['ybc_h1'])
        dma('sp', yb[:, :, 15:15 + L], cyT[:, 256:256 + L].rearrange("(c p) t -> p c t", p=128), [], ['yb'])
        dma('sp', ybc[:, :, 15:15 + C], cyT[:, 0:C].rearrange("(c p) t -> p c t", p=128), [], ['ybc'])
        for c_ in range(2):
            for tap in range(31):
                ts('dve', diag[:, c_, tap, :], CF('identF'), dgw[:, c_ * 31 + tap:c_ * 31 + tap + 1], None, ALU.mult, None, ['cf', 'dgw'], ['diag'])
        for gi, (g0, N) in enumerate(groups):
            if gi == 0 and not with_ctx:
                continue
            buf, bk, off = (ybc, ['ybc', 'ybc_h0', 'ybc_h1'], 0) if gi == 0 else (yb, ['yb', 'yb_h0', 'yb_h1'], g0 - 256)
            r_ = gi % 2
            for c_ in range(2):
                mmg(psf[c_][:, 0:N], [(diag[:, c_, tap, :], buf[:, c_, off + tap:off + tap + N]) for tap in range(31)], ['diag'] + bk, [PK[c_]])
                act(z[:, c_, 0:N], psf[c_][:, 0:N], AF.Identity, [PK[c_], 'cbp'], ['z%d' % c_], bias=cbp[:, c_ * 3:c_ * 3 + 1])
                act(zq[:, c_, 0:N], z[:, c_, 0:N], AF.Square, ['z%d' % c_], ['zq%d' % c_])
            mmg(psf[2][:, 0:N], [(CF('on128'), z[:, c_, 0:N]) for c_ in range(2)], ['z0', 'z1', 'cf'], [PK[2]])
            mmg(psf[3][:, 0:N], [(CF('on128'), zq[:, c_, 0:N]) for c_ in range(2)], ['zq0', 'zq1', 'cf'], [PK[3]])
            cp('dve', mu[:, 0:N], psf[2][:, 0:N], [PK[2]], ['mu'])
            tt('dve', var[:, 0:N], mu[:, 0:N], mu[:, 0:N], ALU.mult, ['mu'], ['var'])
            tt('dve', var[:, 0:N], psf[3][:, 0:N], var[:, 0:N], ALU.subtract, [PK[3], 'var'], ['var'])
            act(var[:, 0:N], var[:, 0:N], AF.Sqrt, ['var'], ['var'], bias=EPS)
            recip(var[:, 0:N], var[:, 0:N], ['var'], ['var'])
            for c_ in range(2):
                zk = 'z%d' % c_
                tt('dve', z[:, c_, 0:N], z[:, c_, 0:N], mu[:, 0:N], ALU.subtract, [zk, 'mu'], [zk])
                tt('dve', z[:, c_, 0:N], z[:, c_, 0:N], var[:, 0:N], ALU.mult, [zk, 'var'], [zk])
                ts('dve', z[:, c_, 0:N], z[:, c_, 0:N], cbp[:, c_ * 3 + 1:c_ * 3 + 2], cbp[:, c_ * 3 + 2:c_ * 3 + 3], ALU.mult, ALU.add, [zk, 'cbp'], [zk])
                act(co[r_][:, c_, 0:N], z[:, c_, 0:N], AF.Silu, [zk], ['co%d' % r_])
            dma('sp', brT[1024:1280, g0:g0 + N].rearrange("(c p) t -> p c t", p=128), co[r_][:, :, 0:N], ['co%d' % r_], ['brT'])
        P.barrier()

    def stage_merge(i, with_ctx, own=False):
        layer0 = (i == 0)
        areset()
        Asrc = aTq1 if own else aT
        Bsrc = brTq if own else brT
        mgl = [(0, g0, N) for (g0, N) in qgroups] if own else [((1 if gi_ == 0 else 0), g0, N) for gi_, (g0, N) in enumerate(groups) if not (gi_ == 0 and not with_ctx)]
        if own:
            idx = alloc([128, LQ // 128], U32)
            dma('sp', idx, own_idx, [], ['idx'])
        wg = alloc([128, 8, 4096], BF16)
        wbr = alloc([128, 10, 1024], BF16)
        wo = alloc([128, 8, 1024], BF16)
        dma('pool', wg, w_in[i, :, 2560:6656].rearrange("(k p) n -> p k n", p=128), [], ['wg'])
        dma('pool', wbr[:, 0:2, :], fnet_w[i].rearrange("(k p) n -> p k n", p=128), [], ['wbr0'])
        dma('pool', wbr[:, 2:4, :], ret_w[i].rearrange("(k p) n -> p k n", p=128), [], ['wbr1'])
        dma('pool', wbr[:, 4:8, :], attn_w[i].rearrange("(k p) n -> p k n", p=128), [], ['wbr2'])
        dma('pool', wbr[:, 8:10, :], conv_wo[i].rearrange("(k p) n -> p k n", p=128), [], ['wbr3'])
        dma('pool', wo, w_out[i].rearrange("(k p) n -> p k n", p=128), [], ['wo'])
        gate = [alloc([128, 1024], F32) for _ in range(2)]
        for wh in range(2):
            dma('sp', gate[wh], modr[wh, :, 2048:3072], [], ['gate%d' % wh])
        at = [alloc([128, 8, 512], BF16)] * 2
        bt = [alloc([128, 10, 512], BF16)] * 2
        sg = [alloc([128, 512], BF16) for _ in range(2)]
        macc = alloc([128, 512], F32); tmp = alloc([128, 512], F32)
        mg = alloc([128, 8, 512], BF16)
        ht = [alloc([128, 1024], F32) for _ in range(2)]
        tq = alloc([128, 1024], F32)
        KB = {0: [0, 1], 1: [2, 3], 2: [4, 5, 6, 7], 3: [8, 9]}
        tix = 0
        mtiles = [g0 // 128 + j for (wh_, g0, N) in mgl for j in range(N // 128)]

        def mload(k):
            t_ = mtiles[k]
            if own:
                P.add('pool', (lambda e, o=ht[k % 2], ix=idx[:, t_:t_ + 1]: e.indirect_dma_start(
                    out=o, out_offset=None, in_=hb, in_offset=bass.IndirectOffsetOnAxis(ap=ix, axis=0))),
                    ['idx'], ['ht%d' % (k % 2)], dma=True)
            else:
                dma('sp', ht[k % 2], hsrc(layer0, t_), ['hb%d' % t_], ['ht%d' % (k % 2)])

        for (wh, g0, N) in mgl:
            r_ = 0
            dma('sp', at[r_][:, :, 0:N], Asrc[:, g0:g0 + N].rearrange("(k p) t -> p k t", p=128), [], ['at%d' % r_])
            dma('sp', bt[r_][:, :, 0:N], Bsrc[:, g0:g0 + N].rearrange("(k p) t -> p k t", p=128), [], ['bt%d' % r_])
            for fc in range(8):
                for b in range(4):
                    mmg(psf[b % 2][:, 0:N], [(wg[:, k, b * 1024 + fc * 128:b * 1024 + (fc + 1) * 128], at[r_][:, k, 0:N]) for k in range(8)],
                        ['wg', 'at%d' % r_], [PK[b % 2]])
                    act(sg[b % 2][:, 0:N], psf[b % 2][:, 0:N], AF.Sigmoid, [PK[b % 2]], ['sg%d' % (b % 2)])
                    mmg(psf[2 + b % 2][:, 0:N], [(wbr[:, kb, fc * 128:(fc + 1) * 128], bt[r_][:, kb, 0:N]) for kb in KB[b]],
                        ['wbr%d' % b, 'bt%d' % r_], [PK[2 + b % 2]])
                    if b == 0:
                        tt('dve', macc[:, 0:N], psf[2][:, 0:N], sg[0][:, 0:N], ALU.mult, [PK[2], 'sg0'], ['macc'])
                    else:
                        tt('dve', tmp[:, 0:N], psf[2 + b % 2][:, 0:N], sg[b % 2][:, 0:N], ALU.mult, [PK[2 + b % 2], 'sg%d' % (b % 2)], ['tmp'])
                        if b < 3:
                            tt('pool', macc[:, 0:N], macc[:, 0:N], tmp[:, 0:N], ALU.add, ['macc', 'tmp'], ['macc'])
                        else:
                            tt('pool', mg[:, fc, 0:N], macc[:, 0:N], tmp[:, 0:N], ALU.add, ['macc', 'tmp'], ['mg'])
            for j in range(N // 128):
                t = g0 // 128 + j
                h_ = tix % 2
                tix += 1
                js = slice(j * 128, (j + 1) * 128)
                if tix == 1:
                    mload(0)
                if tix < len(mtiles):
                    mload(tix)
                for half in range(2):
                    hs = slice(half * 512, (half + 1) * 512)
                    mmg(psf[4 + half][:, :], [(mg[:, fc, js], wo[:, fc, hs]) for fc in range(8)], ['mg', 'wo'], [PK[4 + half]])
                    tt('dve', tq[:, hs], psf[4 + half][:, :], gate[wh][:, hs], ALU.mult, [PK[4 + half], 'gate%d' % wh], ['tq'])
                    tt('pool', ht[h_][:, hs], ht[h_][:, hs], tq[:, hs], ALU.add, ['ht%d' % h_, 'tq'], ['ht%d' % h_])
                dma('sp', (hq if own else hb)[t * 128:(t + 1) * 128, :], ht[h_], ['ht%d' % h_], ['hb%d' % t])
        P.barrier()

    def stage_fm2tm(i):
        areset()
        fm = [alloc([128, 4, 128], BF16) for _ in range(2)]
        tmr = [alloc([128, 512], BF16) for _ in range(2)]
        for t in range(2, NT):
            r_ = t % 2
            tsl = slice(t * 128, (t + 1) * 128)
            dma('sp', fm[r_][:, 0:2, :], brT[256:512, tsl].rearrange("(c p) t -> p c t", p=128), [], ['fm%da' % r_])
            dma('sp', fm[r_][:, 2:4, :], brT[1024:1280, tsl].rearrange("(c p) t -> p c t", p=128), [], ['fm%db' % r_])
            trs([(psb[:, c_ * 128:(c_ + 1) * 128], fm[r_][:, c_, :]) for c_ in range(4)], ['fm%da' % r_, 'fm%db' % r_, 'cb'], ['ps7'])
            cp('act', tmr[r_], psb[:, 0:512], ['ps7'], ['tmr%d' % r_])
            dma('sp', rctm[tsl, :], tmr[r_], ['tmr%d' % r_], ['rctm'])
        P.barrier()

    def stage_compact(i, srcs):
        areset()
        idx = alloc([128, LQ // 128], U32)
        dma('sp', idx, own_idx, [], ['idx'])
        gbuf = {}
        cbuf = {}
        for si, (src, W, dsts) in enumerate(srcs):
            gbuf[si] = [alloc([128, W], BF16) for _ in range(2)]
            cbuf[si] = [alloc([128, W // 128, 128], BF16) for _ in range(2)]
        for j in range(LQ // 128):
            r_ = j % 2
            jsl = slice(j * 128, (j + 1) * 128)
            for si, (src, W, dsts) in enumerate(srcs):
                gk = 'g%d_%d' % (si, r_)
                ck = 'c%d_%d' % (si, r_)
                P.add('pool', (lambda e, o=gbuf[si][r_], ix=idx[:, j:j + 1], src=src: e.indirect_dma_start(
                    out=o, out_offset=None, in_=src, in_offset=bass.IndirectOffsetOnAxis(ap=ix, axis=0))),
                    ['idx'], [gk], dma=True)
                nb = W // 128
                trs([(psb[:, b_ * 128:(b_ + 1) * 128], gbuf[si][r_][:, b_ * 128:(b_ + 1) * 128]) for b_ in range(nb)], [gk, 'cb'], ['ps7'])
                cp('act', cbuf[si][r_], psb[:, 0:W].rearrange("p (k t) -> p k t", k=nb), ['ps7'], [ck])
                for (dst, row0, blk0, nblk) in dsts:
                    dma('sp', dst[row0:row0 + nblk * 128, jsl].rearrange("(c p) t -> p c t", p=128), cbuf[si][r_][:, blk0:blk0 + nblk, :], [ck], ['cdst'])
        P.barrier()

    def stage_ffn_norm(i, with_ctx, moe):
        areset()
        gs, sh = load_mod(i, n2g, 3, 4)
        ht = [alloc([128, 1024], F32) for _ in range(2)]
        ss = [alloc([128, 1], F32) for _ in range(2)]
        junk = alloc([128, 1024], BF16)
        a1 = alloc([128, 1024], F32); a2 = alloc([128, 1024], BF16)
        agrp = [alloc([128, 8, 512], BF16) for _ in range(2)]
        if moe:
            a2f = alloc([128, 1024], F32)
            rw = alloc([128, NE, 1024], F32)
            junk2 = alloc([128, 1024], F32)
            lg = alloc([128, 8], F32); lg2 = alloc([128, 8], F32)
            m1 = alloc([128, 1], F32); m2 = alloc([128, 1], F32); dd = alloc([128, 1], F32); w1 = alloc([128, 1], F32)
            eq1 = alloc([128, 8], F32); eq2 = alloc([128, 8], F32)
            idx = alloc([128, LQ // 128], U32)
            dma('sp', rw, rwT.partition_broadcast(128), [], ['rw'])
            dma('sp', idx, own_idx, [], ['idx'])
            glist = [(0, k * 512, 512) for k in range(LQ // 512)]
        else:
            glist = [((1 if gi == 0 else 0), g0, N) for gi, (g0, N) in enumerate(groups) if not (gi == 0 and not with_ctx)]
        tiles = [(gi, j) for gi, (wh, g0, N) in enumerate(glist) for j in range(N // 128)]

        def load(k):
            gi, j = tiles[k]
            wh, g0, N = glist[gi]
            t = g0 // 128 + j
            r_ = k % 2
            dma('sp', ht[r_], (hq if moe else hb)[t * 128:(t + 1) * 128, :], [], ['r%dht' % r_])

        if tiles:
            load(0)
        for k in range(len(tiles)):
            if k + 1 < len(tiles):
                load(k + 1)
            gi, j = tiles[k]
            wh, g0, N = glist[gi]
            t = g0 // 128 + j
            r_ = k % 2
            k_ = 'r%d' % r_
            gb = gi % 2
            ag = agrp[gb]
            AK = 'ag%d' % gb
            js = slice(j * 128, (j + 1) * 128)
            norm_mod_tile(t, wh, ht[r_], ss[r_], a1, a2, gs, sh, junk, k_, a2f=(a2f if moe else None))
            trs([(psb[:, kk * 128:(kk + 1) * 128], a2[:, kk * 128:(kk + 1) * 128]) for kk in range(8)], ['a2', 'cb'], ['ps7'])
            cp('act', ag[:, :, js], psb[:, :].rearrange("p (k t) -> p k t", k=8), ['ps7'], [AK])
            if moe:
                for e_ in range(NE):
                    tt('dve', junk2, a2f, rw[:, e_, :], ALU.mult, ['a2f', 'rw'], ['junk2'])
                    red(lg[:, e_:e_ + 1], junk2, ALU.add, ['junk2'], ['lg'])
                red(m1, lg, ALU.max, ['lg'], ['m1'])
                ts('dve', eq1, lg, m1[:, 0:1], None, ALU.is_equal, None, ['lg', 'm1'], ['eq1'])
                stt(lg2, eq1, -1e30, lg, ALU.mult, ALU.add, ['eq1', 'lg'], ['lg2'])
                red(m2, lg2, ALU.max, ['lg2'], ['m2'])
                ts('dve', eq2, lg2, m2[:, 0:1], None, ALU.is_equal, None, ['lg2', 'm2'], ['eq2'])
                tt('dve', dd, m2, m1, ALU.subtract, ['m1', 'm2'], ['dd'])
                act(dd, dd, AF.Sigmoid, ['dd'], ['dd'])
                ts('dve', w1, dd, -1.0, 1.0, ALU.mult, ALU.add, ['dd'], ['w1'])
                ts('dve', eq1, eq1, w1[:, 0:1], None, ALU.mult, None, ['eq1', 'w1'], ['eq1'])
                stt(wts[:, t, :], eq2, dd[:, 0:1], eq1, ALU.mult, ALU.add, ['eq2', 'dd', 'eq1'], ['wts'])
            if j == N // 128 - 1:
                dst = aTq if moe else aT
                dma('sp', dst[:, g0:g0 + N].rearrange("(k p) t -> p k t", p=128), ag[:, :, 0:N], [AK], ['aT'])
        P.barrier()

    def stage_ffn_pass(i, with_ctx, wgd, wud, wdd, hp, expert, final):
        areset()
        moe = expert is not None
        c0 = hp * CH * 128
        wgt = alloc([128, 8, CH * 128], BF16); wut = alloc([128, 8, CH * 128], BF16); wdt = alloc([128, CH, 1024], BF16)
        dma('pool', wgt, wgd[:, c0:c0 + CH * 128].rearrange("(k p) n -> p k n", p=128), [], ['wgt'])
        dma('pool', wut, wud[:, c0:c0 + CH * 128].rearrange("(k p) n -> p k n", p=128), [], ['wut'])
        dma('pool', wdt, wdd[c0:c0 + CH * 128, :].rearrange("(k p) n -> p k n", p=128), [], ['wdt'])
        gate = [alloc([128, 1024], F32) for _ in range(2)]
        for wh in range(2):
            dma('sp', gate[wh], modr[wh, :, 5120:6144], [], ['gate%d' % wh])
        ft = [alloc([128, 8, 512], BF16) for _ in range(2)]
        sl = [alloc([128, 512], BF16) for _ in range(2)]
        actT = alloc([128, CH, 512], BF16)
        ht = [alloc([128, 1024], F32) for _ in range(2)]
        tq = alloc([128, 1024], F32)
        if moe:
            glist = [(0, k * 512, 512) for k in range(LQ // 512)]
            hsrc_, asrc_ = hq, aTq
        else:
            glist = [((1 if gi == 0 else 0), g0, N) for gi, (g0, N) in enumerate(groups) if not (gi == 0 and not with_ctx)]
            hsrc_, asrc_ = hb, aT
        tiles = [(gi, j) for gi, (wh, g0, N) in enumerate(glist) for j in range(N // 128)]

        def load_ft(gi):
            wh, g0, N = glist[gi]
            dma('sp', ft[gi % 2][:, :, 0:N], asrc_[:, g0:g0 + N].rearrange("(k p) t -> p k t", p=128), [], ['ft%d' % (gi % 2)])

        def load_ht(k):
            gi, j = tiles[k]
            wh, g0, N = glist[gi]
            t = g0 // 128 + j
            dma('sp', ht[k % 2], hsrc_[t * 128:(t + 1) * 128, :], ['hb%d' % t], ['ht%d' % (k % 2)])

        load_ft(0)
        load_ht(0)
        k = 0
        for gi, (wh, g0, N) in enumerate(glist):
            r_ = gi % 2
            if gi + 1 < len(glist):
                load_ft(gi + 1)
            for c_ in range(CH):
                q_ = c_ % 2
                cs_ = slice(c_ * 128, (c_ + 1) * 128)
                mmg(psf[q_][:, 0:N], [(wgt[:, kk, cs_], ft[r_][:, kk, 0:N]) for kk in range(8)], ['wgt', 'ft%d' % r_], [PK[q_]])
                mmg(psf[2 + q_][:, 0:N], [(wut[:, kk, cs_], ft[r_][:, kk, 0:N]) for kk in range(8)], ['wut', 'ft%d' % r_], [PK[2 + q_]])
                act(sl[q_][:, 0:N], psf[q_][:, 0:N], AF.Silu, [PK[q_]], ['sl%d' % q_])
                tt('dve', actT[:, c_, 0:N], psf[2 + q_][:, 0:N], sl[q_][:, 0:N], ALU.mult, [PK[2 + q_], 'sl%d' % q_], ['actT'])
            for j in range(N // 128):
                t = g0 // 128 + j
                h_ = k % 2
                if k + 1 < len(tiles):
                    load_ht(k + 1)
                k += 1
                js = slice(j * 128, (j + 1) * 128)
                for half in range(2):
                    hs = slice(half * 512, (half + 1) * 512)
                    mmg(psf[4 + half][:, :], [(actT[:, c_, js], wdt[:, c_, hs]) for c_ in range(CH)], ['actT', 'wdt'], [PK[4 + half]])
                    if not moe:
                        tt('dve', tq[:, hs], psf[4 + half][:, :], gate[wh][:, hs], ALU.mult, [PK[4 + half], 'gate%d' % wh], ['tq'])
                    else:
                        stt(tq[:, hs], psf[4 + half][:, :], wts[:, t, expert:expert + 1], gate[wh][:, hs], ALU.mult, ALU.mult,
                            [PK[4 + half], 'gate%d' % wh, 'wts'], ['tq'])
                    tt('pool', ht[h_][:, hs], ht[h_][:, hs], tq[:, hs], ALU.add, ['ht%d' % h_, 'tq'], ['ht%d' % h_])
                if final:
                    dma('sp', out[t * 128:(t + 1) * 128, :], ht[h_], ['ht%d' % h_], ['out'])
                else:
                    dma('sp', hsrc_[t * 128:(t + 1) * 128, :], ht[h_], ['ht%d' % h_], ['hb%d' % t])
        P.barrier()

    def stage_moe(i):
        areset()
        wset = [(alloc([128, 8, CH * 128], BF16), alloc([128, 8, CH * 128], BF16), alloc([128, CH, 1024], BF16)) for _ in range(2)]
        gate0 = alloc([128, 1024], F32)
        dma('sp', gate0, modr[0, :, 5120:6144], [], ['gate0'])
        ftL = [alloc([128, 8, 512], BF16) for _ in range(2)]
        sl = [alloc([128, 512], BF16) for _ in range(2)]
        actT = alloc([128, CH, 512], BF16)
        ht = [alloc([128, 1024], F32) for _ in range(2)]
        tq = alloc([128, 1024], F32)
        passes = [(e_, hp) for e_ in range(NE) for hp in range(HP)]
        glist = [(k_ * 512, 512) for k_ in range(LQ // 512)]
        tiles = [(gi, j) for gi, (g0, N) in enumerate(glist) for j in range(N // 128)]
        seq = [(pi, k) for pi in range(len(passes)) for k in range(len(tiles))]

        def loadw(pi):
            e_, hp = passes[pi]
            c0 = hp * CH * 128
            w_ = wset[pi % 2]
            sfx = str(pi % 2)
            dma('pool', w_[0], moe_wg[0, e_][:, c0:c0 + CH * 128].rearrange("(k p) n -> p k n", p=128), [], ['wgt' + sfx])
            dma('pool', w_[1], moe_wu[0, e_][:, c0:c0 + CH * 128].rearrange("(k p) n -> p k n", p=128), [], ['wut' + sfx])
            dma('pool', w_[2], moe_wd[0, e_][c0:c0 + CH * 128, :].rearrange("(k p) n -> p k n", p=128), [], ['wdt' + sfx])

        def load_ht(si):
            pi, k = seq[si]
            gi, j = tiles[k]
            t = glist[gi][0] // 128 + j
            dma('sp', ht[si % 2], hq[t * 128:(t + 1) * 128, :], ['hb%d' % t], ['ht%d' % (si % 2)])

        gseq = [(pi_, gi_) for pi_ in range(len(passes)) for gi_ in range(len(glist))]

        def load_ft(gq):
            g0_, N_ = glist[gseq[gq][1]]
            dma('sp', ftL[gq % 2][:, :, 0:N_], aTq[:, g0_:g0_ + N_].rearrange("(k p) t -> p k t", p=128), [], ['ft%d' % (gq % 2)])

        loadw(0)
        load_ht(0)
        load_ft(0)
        si = 0
        gq = 0
        for pi, (e_, hp) in enumerate(passes):
            if pi + 1 < len(passes):
                loadw(pi + 1)
            wgt, wut, wdt = wset[pi % 2]
            sfx = str(pi % 2)
            final = (pi == len(passes) - 1)
            for gi, (g0, N) in enumerate(glist):
                ft = ftL[gq % 2]
                fk = 'ft%d' % (gq % 2)
                if gq + 1 < len(gseq):
                    load_ft(gq + 1)
                gq += 1
                for c_ in range(CH):
                    q_ = c_ % 2
                    cs_ = slice(c_ * 128, (c_ + 1) * 128)
                    mmg(psf[q_][:, 0:N], [(wgt[:, kk, cs_], ft[:, kk, 0:N]) for kk in range(8)], ['wgt' + sfx, fk], [PK[q_]])
                    mmg(psf[2 + q_][:, 0:N], [(wut[:, kk, cs_], ft[:, kk, 0:N]) for kk in range(8)], ['wut' + sfx, fk], [PK[2 + q_]])
                    act(sl[q_][:, 0:N], psf[q_][:, 0:N], AF.Silu, [PK[q_]], ['sl%d' % q_])
                    tt('dve', actT[:, c_, 0:N], psf[2 + q_][:, 0:N], sl[q_][:, 0:N], ALU.mult, [PK[2 + q_], 'sl%d' % q_], ['actT'])
                for j in range(N // 128):
                    t = g0 // 128 + j
                    h_ = si % 2
                    if si + 1 < len(seq):
                        load_ht(si + 1)
                    si += 1
                    js = slice(j * 128, (j + 1) * 128)
                    for half in range(2):
                        hs = slice(half * 512, (half + 1) * 512)
                        mmg(psf[4 + half][:, :], [(actT[:, c_, js], wdt[:, c_, hs]) for c_ in range(CH)], ['actT', 'wdt' + sfx], [PK[4 + half]])
                        stt(tq[:, hs], psf[4 + half][:, :], wts[:, t, e_:e_ + 1], gate0[:, hs], ALU.mult, ALU.mult,
                            [PK[4 + half], 'gate0', 'wts'], ['tq'])
                        tt('pool', ht[h_][:, hs], ht[h_][:, hs], tq[:, hs], ALU.add, ['ht%d' % h_, 'tq'], ['ht%d' % h_])
                    if final:
                        dma('sp', out[t * 128:(t + 1) * 128, :], ht[h_], ['ht%d' % h_], ['out'])
                    else:
                        dma('sp', hq[t * 128:(t + 1) * 128, :], ht[h_], ['ht%d' % h_], ['hb%d' % t])
        P.barrier()

    P.barrier()
    maxstage = int(os.environ.get('KSTAGES', '999'))
    sc = {'n': 0}

    def S(fn, *a, **kw):
        if sc['n'] < maxstage:
            fn(*a, **kw)
        sc['n'] += 1

    for i in range(2):
        with_ctx = (i == 0)
        S(stage_mod, i)
        print("ops after mod", P.nadd)
        S(stage_proj, i)
        print("ops after proj", P.nadd)
        if i == 0:
            S(stage_attn, i, with_ctx)
            S(stage_fnet, i, with_ctx)
            S(stage_ret, i, with_ctx)
            S(stage_conv, i, with_ctx)
            S(stage_merge, i, with_ctx)
        else:
            S(stage_compact, i, [(qtm, 512, [(qTq, 0, 0, 4)]), (atm, 1024, [(aTq1, 0, 0, 8)])])
            S(stage_attn, i, with_ctx, own=True)
            S(stage_fnet, i, with_ctx, tr=False)
            S(stage_ret, i, with_ctx)
            S(stage_conv, i, with_ctx)
            S(stage_fm2tm, i)
            S(stage_compact, i, [(ytm, 256, [(brTq, 0, 0, 2)]), (rctm, 512, [(brTq, 256, 0, 2), (brTq, 1024, 2, 2)])])
            S(stage_merge, i, with_ctx, own=True)
        if i == 0:
            S(stage_ffn_norm, i, with_ctx, moe=False)
            for hp in range(HP):
                S(stage_ffn_pass, i, with_ctx, ffn_wg[0], ffn_wu[0], ffn_wd[0], hp, None, False)
        else:
            S(stage_ffn_norm, i, with_ctx, moe=True)
            S(stage_moe, i)
    P.add('sp', None, ['out'], [])
    P.emit()
    P.close()
    return nc


def host_inputs(inp, L, b):
    f = lambda a: np.ascontiguousarray(np.asarray(a), dtype=np.float32)
    cf, cb, rope = make_consts(L)
    c = f(inp["c"])[b]; cc = f(inp["c_ctx"])
    ccols = np.concatenate([c.reshape(8, 128).T, cc.reshape(8, 128).T], 1)
    dec2 = np.concatenate([f(inp["ret_decay_fwd"]), f(inp["ret_decay_bwd"])], 1)
    rng = f(inp["ret_norm_g"]).reshape(2, 4, 64).transpose(0, 2, 1)
    dwa = f(inp["conv_dw_w"]).reshape(2, 31, 2, 128).transpose(0, 3, 2, 1).reshape(2, 128, 62)
    cba = np.stack([f(inp["conv_dw_b"]), f(inp["conv_ln_g"]), f(inp["conv_ln_b"])], -1).reshape(2, 2, 128, 3).transpose(0, 2, 1, 3).reshape(2, 128, 6)
    gqk = np.concatenate([np.tile(f(inp["attn_qn_g"]), (1, 8)), np.tile(f(inp["attn_kn_g"]), (1, 2))], 1)
    rwT = f(inp["router_w"])[0].T
    d = {"x": f(inp["x"])[b], "ctx": f(inp["ctx"])[b], "ccols": ccols, "dec2": dec2, "rng": rng, "dwa": dwa, "cba": cba,
         "gqk": gqk, "rwT": rwT, "cf": cf, "cb": cb, "rope": rope}
    for k in ["ada_w", "ada_b", "norm1_g", "norm2_g", "w_in", "fnet_w", "ret_w", "attn_w", "conv_w_out", "w_out",
              "ffn_w_gate", "ffn_w_up", "ffn_w_down", "moe_w_gate", "moe_w_up", "moe_w_down"]:
        d[k] = f(inp[k])
    return {k: np.ascontiguousarray(v, dtype=np.float32) for k, v in d.items()}


def run(inputs, debug=False):
    L = int(np.asarray(inputs["x"]).shape[1])
    FD = int(np.asarray(inputs["ffn_w_gate"]).shape[2])
    NQ = 4
    LQ = L // NQ
    nc = build(L, FD // 128, debug=debug, NQ=NQ)
    base = [host_inputs(inputs, L, b) for b in range(2)]
    in_maps = []
    for cid in range(2 * NQ):
        b, q = cid // NQ, cid % NQ
        d = dict(base[b])
        jj = np.arange(LQ // 128)[None, :]
        pp = np.arange(128)[:, None]
        d["own_idx"] = np.ascontiguousarray((256 + q * LQ + jj * 128 + pp).astype(np.uint32))
        in_maps.append(d)
    res = run_bass_kernel_spmd(nc, in_maps, core_ids=list(range(2 * NQ)))
    outp = np.zeros((2, L, 1024), np.float32)
    for cid in range(2 * NQ):
        b, q = cid // NQ, cid % NQ
        outp[b, q * LQ:(q + 1) * LQ] = np.asarray(res.results[cid]["out"], dtype=np.float32)
    if debug:
        return outp, res.results
    return outp


def kernel(**inputs):
    return run(inputs)
```

```python
from contextlib import ExitStack
import os
import numpy as np
import concourse.bass as bass
import concourse.mybir as mybir
from concourse.bass_utils import run_bass_kernel_spmd

F32 = mybir.dt.float32
BF16 = mybir.dt.bfloat16
U32 = mybir.dt.uint32
ALU = mybir.AluOpType
AF = mybir.ActivationFunctionType
AX = mybir.AxisListType

ENGS = ['pe', 'act', 'dve', 'pool', 'sp']
NS_DMA = 8
SAME_ENG_GAP = 3
EPS = 1e-6


class Op:
    __slots__ = ('eng', 'fn', 'idx', 'waits', 'signal', 'sigval', 'dma', 'dsem', 'dval', 'dpre', 'ndma')


class Prog:
    def __init__(self, nc):
        self.nc = nc
        self.ops = {e: [] for e in ENGS}
        self.last_w = {}
        self.readers = {}
        self.known = {e: {f: -1 for f in ENGS} for e in ENGS}
        self.known_dma = {e: set() for e in ENGS}
        self.dma_count = {e: 0 for e in ENGS}
        self.dma_semtot = {e: [0] * NS_DMA for e in ENGS}
        self.es = ExitStack()
        self.sems = {}
        self.dsems = {}
        self.bar = None

    def sb(self, name, shape, dt):
        return self.es.enter_context(self.nc.sbuf_tensor(name, list(shape), dt))

    def ps(self, name, shape, dt):
        return self.es.enter_context(self.nc.psum_tensor(name, list(shape), dt))

    def add(self, eng, fn, reads=(), writes=(), dma=False, ndma=1, force=False):
        self.nadd = getattr(self, 'nadd', 0) + 1
        if self.nadd > int(os.environ.get('KOPS', '100000000')) and not force:
            return None
        op = Op()
        op.eng = eng; op.fn = fn; op.dma = dma; op.signal = False; op.sigval = None; op.ndma = ndma
        lst = self.ops[eng]
        op.idx = len(lst)
        deps = []
        pr = [r for r in reads if r.startswith('ps')]
        if pr:
            reads = [r for r in reads if not r.startswith('ps')]
            writes = list(writes) + [r for r in pr if r not in writes]
        if self.bar is not None:
            deps.append(self.bar)
        for r in reads:
            w = self.last_w.get(r)
            if w is not None:
                deps.append(w)
        for w_ in writes:
            w = self.last_w.get(w_)
            if w is not None:
                deps.append(w)
            deps.extend(self.readers.get(w_, ()))
        waits = []
        best = {}
        for d in deps:
            if d is op:
                continue
            if d.dma:
                if id(d) in self.known_dma[eng]:
                    continue
                self.known_dma[eng].add(id(d))
                waits.append(d)
            else:
                if d.idx <= self.known[eng][d.eng]:
                    continue
                if d.eng == eng and not force:
                    if eng == 'pe' or eng == 'sp':
                        continue
                    if op.idx - d.idx >= SAME_ENG_GAP and not dma and eng != 'pool':
                        continue
                b = best.get(d.eng)
                if b is None or d.idx > b.idx:
                    best[d.eng] = d
        for f, d in best.items():
            self.known[eng][f] = d.idx
            d.signal = True
            waits.append(d)
        op.waits = waits
        if dma:
            i = self.dma_count[eng]
            self.dma_count[eng] += 1
            s = i % NS_DMA
            op.dsem = s
            op.dpre = self.dma_semtot[eng][s]
            self.dma_semtot[eng][s] += 16 * ndma
            op.dval = self.dma_semtot[eng][s]
        for r in reads:
            self.readers.setdefault(r, []).append(op)
        for w_ in writes:
            self.last_w[w_] = op
            self.readers[w_] = []
        lst.append(op)
        return op

    def barrier(self):
        keys = set(self.last_w.keys()) | set(self.readers.keys())
        keys = list(keys)
        last = None
        for e in ENGS:
            own = self.ops[e][-1] if self.ops[e] else None
            last = self.add(e, (lambda en: en.nop()), reads=(), writes=keys, force=True)
            if own is not None and not own.dma and own.fn is not None and own not in last.waits and own.idx > self.known[e][e]:
                own.signal = True
                last.waits.append(own)
                self.known[e][e] = own.idx
        self.bar = last
        self.last_w = {}
        self.readers = {}

    def emit(self):
        nc = self.nc
        for e in ENGS:
            self.sems[e] = self.es.enter_context(nc.semaphore('s_' + e))
            self.dsems[e] = [self.es.enter_context(nc.semaphore('d_%s%d' % (e, i))) for i in range(NS_DMA)]
        for e in ENGS:
            c = 0
            for op in self.ops[e]:
                if op.signal and not op.dma:
                    c += 1
                    op.sigval = c
        block = self.es.enter_context(nc.Block())
        prog = self

        def run(ename, eng):
            for op in prog.ops[ename]:
                for d in op.waits:
                    if d.dma:
                        eng.wait_ge(prog.dsems[d.eng][d.dsem], d.dval)
                    else:
                        eng.wait_ge(prog.sems[d.eng], d.sigval)
                if op.fn is None:
                    continue
                if op.dma:
                    sem = prog.dsems[ename][op.dsem]
                    if op.dpre > 0:
                        eng.wait_ge(sem, op.dpre)
                    r = op.fn(eng)
                    if not isinstance(r, (list, tuple)):
                        r = [r]
                    assert len(r) == op.ndma
                    for ins in r:
                        ins.then_inc(sem, 16)
                else:
                    r = op.fn(eng)
                    if op.signal:
                        if isinstance(r, (list, tuple)):
                            r = r[-1]
                        r.then_inc(prog.sems[ename], 1)

        @block.tensor
        def _(eng):
            run('pe', eng)

        @block.scalar
        def _(eng):
            run('act', eng)

        @block.vector
        def _(eng):
            run('dve', eng)

        @block.gpsimd
        def _(eng):
            run('pool', eng)

        @block.sync
        def _(eng):
            run('sp', eng)

    def close(self):
        self.es.close()


CF_COLS = {}


def _layout_cf(L0):
    names = [('identF', 128), ('DIFFP', 128), ('DIFFN', 128), ('MLT', 128), ('MGT', 128), ('I2', 128),
             ('IP1', 128), ('IB', 128), ('P127', 1), ('PJ', 1), ('C128T', 64), ('on128', 128), ('on64', 64),
             ('twc', L0), ('tws', L0)]
    off = 0
    d = {}
    for n, w in names:
        d[n] = (off, w)
        off += w
    return d, off


def _layout_cb(L0):
    names = [('ident', 128), ('c128', 128), ('s128', 128), ('c64', L0), ('s64', L0), ('bd1', 1024), ('bd2', 1024),
             ('cc', 512), ('sc', 512)]
    off = 0
    d = {}
    for n, w in names:
        d[n] = (off, w)
        off += w
    return d, off


def make_consts(L):
    L0 = L // 128
    T = 256 + L
    cfl, ncf = _layout_cf(L0)
    cbl, ncb = _layout_cb(L0)
    cf = np.zeros((128, ncf), np.float32)
    cb = np.zeros((128, ncb), np.float32)

    def setf(n, a):
        o, w = cfl[n]
        cf[:a.shape[0], o:o + w] = a

    def setb(n, a):
        o, w = cbl[n]
        cb[:a.shape[0], o:o + w] = a

    p = np.arange(128)
    j = p[:, None].astype(np.float64)
    i = p[None, :].astype(np.float64)
    setf('identF', np.eye(128))
    setf('DIFFP', np.maximum(i - j, 0))
    setf('DIFFN', np.maximum(j - i, 0))
    setf('MLT', (j < i).astype(np.float64))
    setf('MGT', (j > i).astype(np.float64))
    setf('I2', 2 * np.eye(128))
    setf('IP1', np.broadcast_to(i + 1, (64, 128)))
    setf('IB', np.broadcast_to(128 - i, (64, 128)))
    setf('P127', 127 - j)
    setf('PJ', j)
    setf('C128T', np.full((64, 64), 128.0))
    setf('on128', np.full((128, 128), 1.0 / 256))
    setf('on64', np.full((64, 64), 1.0 / 64))
    l0 = np.arange(L0)[None, :].astype(np.float64)
    setf('twc', np.cos(2 * np.pi * j * l0 / L))
    setf('tws', np.sin(2 * np.pi * j * l0 / L))
    setb('ident', np.eye(128))
    setb('c128', np.cos(2 * np.pi * j * i / 128))
    setb('s128', np.sin(2 * np.pi * j * i / 128))
    a0 = np.arange(L0)[:, None].astype(np.float64)
    b0 = np.arange(L0)[None, :].astype(np.float64)
    setb('c64', np.cos(2 * np.pi * a0 * b0 / L0) / np.sqrt(L))
    setb('s64', np.sin(2 * np.pi * a0 * b0 / L0) / np.sqrt(L))
    c = np.arange(64)[:, None].astype(np.float64)
    jj = np.arange(64)[None, :].astype(np.float64)
    C64 = np.cos(2 * np.pi * c * jj / 64) / 8.0
    S64 = np.sin(2 * np.pi * c * jj / 64) / 8.0
    BDC = np.zeros((256, 256)); BDS = np.zeros((256, 256))
    for g in range(4):
        BDC[g * 64:(g + 1) * 64, g * 64:(g + 1) * 64] = C64
        BDS[g * 64:(g + 1) * 64, g * 64:(g + 1) * 64] = S64
    bd1 = np.concatenate([BDC, -BDS], 1)
    bd2 = np.concatenate([-BDS, -BDC], 1)
    setb('bd1', bd1.reshape(2, 128, 512).transpose(1, 0, 2).reshape(128, 1024))
    setb('bd2', bd2.reshape(2, 128, 512).transpose(1, 0, 2).reshape(128, 1024))
    lc = np.arange(256)[:, None].astype(np.float64)
    kc = np.arange(256)[None, :].astype(np.float64)
    CC = np.cos(2 * np.pi * lc * kc / 256) / 16.0
    SC = np.sin(2 * np.pi * lc * kc / 256) / 16.0
    setb('cc', CC.reshape(2, 128, 256).transpose(1, 0, 2).reshape(128, 512))
    setb('sc', SC.reshape(2, 128, 256).transpose(1, 0, 2).reshape(128, 512))
    rows = L // 64
    row = np.repeat(np.arange(rows), 64).astype(np.float32)
    col = np.tile(np.arange(64), rows).astype(np.float32)
    inv = (np.float32(10000.0) ** (-np.arange(16, dtype=np.float32) / np.float32(16))).astype(np.float32)
    ang = np.concatenate([row[:, None] * inv, col[:, None] * inv], -1).astype(np.float32)
    rope = np.zeros((T, 64), np.float32)
    rope[:256, :32] = 1.0
    rope[256:, :32] = np.cos(ang)
    rope[256:, 32:] = np.sin(ang)
    return cf, cb, rope


def build(L, FFC, debug=False, NQ=4):
    C = 256
    LQ = L // NQ
    T = C + L
    NT = T // 128
    L0 = L // 128
    HP = 2 if FFC % 2 == 0 else 1
    CH = FFC // HP
    FD = FFC * 128
    NE = 8
    cfl, ncf = _layout_cf(L0)
    cbl, ncb = _layout_cb(L0)
    nc = bass.Bass("TRN2", target_bir_lowering=False)
    P = Prog(nc)

    def din(name, shape, dt=F32):
        return nc.dram_tensor(name, list(shape), dt, kind="ExternalInput").ap()

    def dscr(name, shape, dt):
        return nc.dram_tensor(name, list(shape), dt, kind=("ExternalOutput" if debug else "Internal")).ap()

    x = din("x", [L, 1024]); ctx = din("ctx", [C, 1024]); ccols = din("ccols", [128, 16])
    ada_w = din("ada_w", [2, 1024, 6144]); ada_b = din("ada_b", [2, 6144])
    n1g = din("norm1_g", [2, 1024]); n2g = din("norm2_g", [2, 1024])
    w_in = din("w_in", [2, 1024, 6656])
    fnet_w = din("fnet_w", [2, 256, 1024]); ret_w = din("ret_w", [2, 256, 1024]); attn_w = din("attn_w", [2, 512, 1024])
    conv_wo = din("conv_w_out", [2, 256, 1024]); w_out = din("w_out", [2, 1024, 1024])
    ffn_wg = din("ffn_w_gate", [1, 1024, FD]); ffn_wu = din("ffn_w_up", [1, 1024, FD]); ffn_wd = din("ffn_w_down", [1, FD, 1024])
    moe_wg = din("moe_w_gate", [1, NE, 1024, FD]); moe_wu = din("moe_w_up", [1, NE, 1024, FD]); moe_wd = din("moe_w_down", [1, NE, FD, 1024])
    dec2 = din("dec2", [2, 8]); rng = din("rng", [2, 64, 4]); dwa = din("dwa", [2, 128, 62]); cba = din("cba", [2, 128, 6])
    gqk_in = din("gqk", [2, 640]); rwT = din("rwT", [NE, 1024])
    cf_in = din("cf", [128, ncf]); cb_in = din("cb", [128, ncb]); rope_in = din("rope", [T, 64])
    own_idx = nc.dram_tensor("own_idx", [128, LQ // 128], U32, kind="ExternalInput").ap()
    out = nc.dram_tensor("out", [LQ, 1024], F32, kind="ExternalOutput").ap()
    hq = dscr("hq", [LQ, 1024], F32)
    qtm = dscr("qtm", [T, 512], BF16)
    atm = dscr("atm", [T, 1024], BF16)
    rctm = dscr("rctm", [T, 512], BF16)
    qTq = dscr("qTq", [512, LQ], BF16)
    aTq1 = dscr("aTq1", [1024, LQ], BF16)
    brTq = dscr("brTq", [1280, LQ], BF16)
    qgroups = [(k_ * 512, 512) for k_ in range(LQ // 512)]
    aTq = dscr("aTq", [1024, LQ], BF16)

    hb = dscr("hb", [T, 1024], F32)
    aT = dscr("aT", [1024, T], BF16)
    modr = dscr("modr", [2, 128, 6144], F32)
    qkT = dscr("qkT", [1152, T], BF16)
    tmo = dscr("tmo", [T, 640], BF16)
    ab1 = dscr("ab1", [T, 512], BF16); ab2 = dscr("ab2", [T, 512], BF16)
    rgT = dscr("rgT", [256, T], BF16); cyT = dscr("cyT", [256, T], BF16)
    zs = dscr("zs", [128, L0, 512], BF16)
    ytm = dscr("ytm", [T, 256], BF16)
    brT = dscr("brT", [1280, T], BF16)

    groups = [(0, 256)] + [(256 + 512 * i_, 512) for i_ in range(L // 512)]

    cf = P.sb("cf_sb", [128, ncf], F32)
    cb = P.sb("cb_sb", [128, ncb], BF16)
    wts = P.sb("wts_sb", [128, NT, 8], F32)
    ARW = 46600
    arena = P.sb("arena", [128, ARW], F32)
    pw = [P.ps("pw%d" % i_, [128, 1024], F32) for i_ in range(4)]
    psf = [pw[i_ // 2][:, (i_ % 2) * 512:(i_ % 2 + 1) * 512] for i_ in range(8)]
    psb = pw[3][:, 512:1024].bitcast(BF16)
    PK = ['ps%d' % i_ for i_ in range(8)]
    st = {'off': 0, 'n': 0}

    def CF(n, parts=128):
        o, w = cfl[n]
        return cf[0:parts, o:o + w]

    def CB(n, parts=128):
        o, w = cbl[n]
        return cb[0:parts, o:o + w]

    def areset():
        st['off'] = 0

    def alloc(shape, dt, name=None):
        n = 1
        for s in shape[1:]:
            n *= s
        words = n if dt in (F32, U32) else (n + 1) // 2
        words = (words + 7) // 8 * 8
        assert st['off'] + words <= ARW, ("arena overflow", st['off'], words)
        v = arena[0:shape[0], st['off']:st['off'] + words]
        st['off'] += words
        if dt != F32:
            v = v.bitcast(dt)
        v = v[:, 0:n]
        if len(shape) == 3:
            v = v.rearrange("p (a b) -> p a b", a=shape[1])
        elif len(shape) == 4:
            v = v.rearrange("p (a b c) -> p a b c", a=shape[1], b=shape[2])
        st['n'] += 1
        return v

    def dma(q, o, i, r, w):
        P.add(q, (lambda e, o=o, i=i: e.dma_start(out=o, in_=i)), r, w, dma=True)

    def mmg(o, pairs, r, w, tr=False):
        def fn(e, o=o, pairs=pairs):
            n = len(pairs)
            ins = None
            for ii, (l, rh) in enumerate(pairs):
                ins = e.matmul(o, lhsT=l, rhs=rh, start=(ii == 0), stop=(ii == n - 1))
            return ins
        P.add('pe', fn, r, w)

    def mms(items, r, w):
        def fn(e, items=items):
            ins = None
            for o, pairs in items:
                n = len(pairs)
                for ii, (l, rh) in enumerate(pairs):
                    ins = e.matmul(o, lhsT=l, rhs=rh, start=(ii == 0), stop=(ii == n - 1))
            return ins
        P.add('pe', fn, r, w)

    def trs(items, r, w):
        def fn(e, items=items):
            ins = None
            for o, i in items:
                ins = e.transpose(o, i, CB('ident'))
            return ins
        P.add('pe', fn, r, w)

    def act(o, i, func, r, w, **kw):
        P.add('act', (lambda e, o=o, i=i, func=func, kw=kw: e.activation(out=o, in_=i, func=func, **kw)), r, w)

    def tt(eng, o, a, b, op, r, w):
        P.add(eng, (lambda e, o=o, a=a, b=b, op=op: e.tensor_tensor(out=o, in0=a, in1=b, op=op)), r, w)

    def ts(eng, o, a, s1, s2, op0, op1, r, w):
        if s2 is None:
            P.add(eng, (lambda e, o=o, a=a, s1=s1, op0=op0: e.tensor_scalar(out=o, in0=a, scalar1=s1, scalar2=None, op0=op0)), r, w)
        else:
            P.add(eng, (lambda e, o=o, a=a, s1=s1, s2=s2, op0=op0, op1=op1: e.tensor_scalar(out=o, in0=a, scalar1=s1, scalar2=s2, op0=op0, op1=op1)), r, w)

    def stt(o, a, s, b, op0, op1, r, w):
        P.add('dve', (lambda e, o=o, a=a, s=s, b=b, op0=op0, op1=op1: e.scalar_tensor_tensor(out=o, in0=a, scalar=s, in1=b, op0=op0, op1=op1)), r, w)

    def cp(eng, o, i, r, w):
        if eng == 'act':
            act(o, i, AF.Copy, r, w)
        else:
            P.add(eng, (lambda e, o=o, i=i: e.tensor_copy(out=o, in_=i)), r, w)

    def recip(o, i, r, w):
        P.add('dve', (lambda e, o=o, i=i: e.reciprocal(out=o, in_=i)), r, w)

    def red(o, i, op, r, w):
        P.add('dve', (lambda e, o=o, i=i, op=op: e.tensor_reduce(out=o, in_=i, axis=AX.X, op=op)), r, w)

    def mset(eng, o, v, w):
        P.add(eng, (lambda e, o=o, v=v: e.memset(o, v)), (), w)

    dma('sp', cf[:], cf_in, [], ['cf'])
    dma('pool', cb[:], cb_in, [], ['cb'])
    CK = ['cf', 'cb']

    def hsrc(layer0, t):
        if layer0:
            return ctx[t * 128:(t + 1) * 128, :] if t < 2 else x[(t - 2) * 128:(t - 1) * 128, :]
        return hb[t * 128:(t + 1) * 128, :]

    def stage_mod(i):
        areset()
        cc_t = alloc([128, 16], F32); scol = alloc([128, 16], F32); srep = alloc([128, 16, 128], F32)
        adw = [alloc([128, 8, 512], F32) for _ in range(2)]
        adb = [alloc([128, 512], F32) for _ in range(2)]
        mo = [alloc([128, 512], F32) for _ in range(2)]
        dma('sp', cc_t, ccols, [], ['cc_t'])
        act(scol, cc_t, AF.Silu, ['cc_t'], ['scol'])
        cp('dve', srep, scol.unsqueeze(2).to_broadcast([128, 16, 128]), ['scol'], ['srep'])
        for cg in range(12):
            r_ = cg % 2
            cs_ = slice(cg * 512, (cg + 1) * 512)
            dma('sp', adw[r_], ada_w[i, :, cs_].rearrange("(k p) n -> p k n", p=128), [], ['adw%d' % r_])
            dma('sp', adb[r_], ada_b[i, cs_].partition_broadcast(128), [], ['adb%d' % r_])
            for wh in range(2):
                mmg(psf[wh][:, :], [(srep[:, wh * 8 + k, :], adw[r_][:, k, :]) for k in range(8)],
                    ['srep', 'adw%d' % r_], [PK[wh]])
                tt('dve', mo[wh], psf[wh][:, :], adb[r_], ALU.add, [PK[wh], 'adb%d' % r_], ['mo%d' % wh])
                dma('sp', modr[wh, :, cs_], mo[wh], ['mo%d' % wh], ['modr'])
        P.barrier()

    def norm_mod_tile(t, wh, ht, ss, a1, a2, gs, sh, junk, ktag, a2f=None):
        gk = 'gs_l%d' % wh
        sk = 'sh_l%d' % wh
        act(junk, ht, AF.Square, [ktag + 'ht'], ['junk', ktag + 'ss'], accum_out=ss)
        act(ss, ss, AF.Sqrt, [ktag + 'ss'], [ktag + 'ss'], scale=1.0 / 1024, bias=EPS)
        recip(ss, ss, [ktag + 'ss'], [ktag + 'ss'])
        stt(a1, ht, ss[:, 0:1], gs[wh], ALU.mult, ALU.mult, [ktag + 'ht', ktag + 'ss', gk], ['a1'])
        if a2f is not None:
            tt('dve', a2f, a1, sh[wh], ALU.add, ['a1', sk], ['a2f'])
            cp('pool', a2, a2f, ['a2f'], ['a2'])
        else:
            tt('pool', a2, a1, sh[wh], ALU.add, ['a1', sk], ['a2'])

    def load_mod(i, gvec, shift_col, scale_col):
        gs = [alloc([128, 1024], F32) for _ in range(2)]
        sh = [alloc([128, 1024], F32) for _ in range(2)]
        grep = alloc([128, 1024], F32)
        dma('sp', grep, gvec[i].partition_broadcast(128), [], ['grep'])
        for wh in range(2):
            dma('sp', sh[wh], modr[wh, :, shift_col * 1024:(shift_col + 1) * 1024], [], ['sh_l%d' % wh])
            dma('sp', gs[wh], modr[wh, :, scale_col * 1024:(scale_col + 1) * 1024], [], ['gs_l%d' % wh])
            stt(gs[wh], gs[wh], 1.0, grep, ALU.add, ALU.mult, ['gs_l%d' % wh, 'grep'], ['gs_l%d' % wh])
        return gs, sh

    def stage_proj(i):
        layer0 = (i == 0)
        areset()
        wi = alloc([128, 8, 2560], BF16)
        dma('pool', wi, w_in[i, :, 0:2560].rearrange("(k p) n -> p k n", p=128), [], ['wi'])
        gs, sh = load_mod(i, n1g, 0, 1)
        gq = alloc([128, 640], F32)
        dma('sp', gq, gqk_in[i].partition_broadcast(128), [], ['gq'])
        ht = [alloc([128, 1024], F32) for _ in range(2)]
        ss = [alloc([128, 1], F32) for _ in range(2)]
        junk = alloc([128, 1024], BF16)
        a1 = alloc([128, 1024], F32); a2 = alloc([128, 1024], BF16)
        agrp = [alloc([128, 8, 512], BF16) for _ in range(2)]
        qkg = alloc([128, 9, 512], BF16)
        xq = alloc([128, 1152], F32); sq = alloc([128, 640], F32); ssq = alloc([128, 10], F32)
        xr = alloc([128, 18, 64], BF16)
        t1 = alloc([128, 18, 32], F32); t2 = alloc([128, 18, 32], F32); t3 = alloc([128, 18, 32], F32); t4 = alloc([128, 18, 32], F32)
        tmt = alloc([128, 640], BF16)
        cs_t = [alloc([128, 64], F32) for _ in range(2)]
        fnT = alloc([128, 2, 512], BF16); rgt = alloc([128, 2, 512], BF16); cy = alloc([128, 2, 512], BF16)
        sg = alloc([128, 512], F32)
        abt1 = alloc([128, 4, 512], BF16); abt2 = alloc([128, 4, 512], BF16)
        bd1 = CB('bd1').rearrange("p (a b) -> p a b", a=2); bd2 = CB('bd2').rearrange("p (a b) -> p a b", a=2)
        x3 = xq.rearrange("p (h d) -> p h d", h=18)
        xrf = xr.rearrange("p h d -> p (h d)")
        xqL = [xq, alloc([128, 1152], F32)]
        tmtL = [tmt, alloc([128, 640], BF16)]
        csL = [alloc([128, 64], F32) for _ in range(4)]
        tinfo = [(gi, j) for gi, (g0, N) in enumerate(groups) for j in range(N // 128)]
        ntl = len(tinfo)

        def pload(k):
            gi, j = tinfo[k]
            t_ = groups[gi][0] // 128 + j
            dma('sp', ht[k % 2], hsrc(layer0, t_), [], ['r%dht' % (k % 2)])
            dma('sp', csL[k % 4], rope_in[t_ * 128:(t_ + 1) * 128, :], [], ['cs%d' % (k % 4)])

        def front(k):
            gi, j = tinfo[k]
            g0, N = groups[gi]
            t = g0 // 128 + j
            r_ = k % 2
            q_ = str(r_)
            k_ = 'r%d' % r_
            ag = agrp[gi % 2]
            AK = 'ag%d' % (gi % 2)
            wh = 1 if gi == 0 else 0
            js = slice(j * 128, (j + 1) * 128)
            xq_ = xqL[r_]; tmt_ = tmtL[r_]
            if k + 1 < ntl:
                pload(k + 1)
            norm_mod_tile(t, wh, ht[r_], ss[r_], a1, a2, gs, sh, junk, k_)
            if not layer0:
                dma('sp', atm[t * 128:(t + 1) * 128, :], a2, ['a2'], ['atm'])
            trs([(psb[:, kk * 128:(kk + 1) * 128], a2[:, kk * 128:(kk + 1) * 128]) for kk in range(8)], ['a2', 'cb'], ['ps7'])
            cp('act', ag[:, :, js], psb[:, :].rearrange("p (k t) -> p k t", k=8), ['ps7'], [AK])
            lhs = [ag[:, kk, js] for kk in range(8)]
            mms([(psf[0][:, 0:512], [(lhs[kk], wi[:, kk, 1280:1792]) for kk in range(8)]),
                 (psf[1][:, 0:128], [(lhs[kk], wi[:, kk, 1792:1920]) for kk in range(8)]),
                 (psf[1][:, 128:384], [(lhs[kk], wi[:, kk, 256:512]) for kk in range(8)]),
                 (psf[2][:, 0:256], [(lhs[kk], wi[:, kk, 512:768]) for kk in range(8)]),
                 (psf[2][:, 256:384], [(lhs[kk], wi[:, kk, 1920:2048]) for kk in range(8)]),
                 (psf[3][:, 0:256], [(lhs[kk], wi[:, kk, 768:1024]) for kk in range(8)])],
                [AK, 'wi'], [PK[0], PK[1], PK[2], PK[3]])
            cp('act', xq_[:, 0:512], psf[0][:, 0:512], [PK[0]], ['xq_a' + q_])
            cp('dve', xq_[:, 512:896], psf[1][:, 0:384], [PK[1]], ['xq_b' + q_])
            act(xq_[:, 896:1152], psf[2][:, 0:256], AF.Copy, [PK[2]], ['xq_c' + q_], scale=0.125)
            cp('dve', tmt_[:, 0:128], psf[2][:, 256:384], [PK[2]], ['tmt_a' + q_])
            cp('act', tmt_[:, 128:384], psf[3][:, 0:256], [PK[3]], ['tmt_b' + q_])

        def back(k):
            gi, j = tinfo[k]
            g0, N = groups[gi]
            t = g0 // 128 + j
            r_ = k % 2
            q_ = str(r_)
            js = slice(j * 128, (j + 1) * 128)
            xq_ = xqL[r_]; tmt_ = tmtL[r_]
            x3_ = xq_.rearrange("p (h d) -> p h d", h=18)
            ck = 'cs%d' % (k % 4)
            XA, XB, XC = 'xq_a' + q_, 'xq_b' + q_, 'xq_c' + q_
            tt('dve', sq, xq_[:, 0:640], xq_[:, 0:640], ALU.mult, [XA, XB], ['sq'])
            red(ssq, sq.rearrange("p (h d) -> p h d", h=10), ALU.add, ['sq'], ['ssq'])
            act(ssq, ssq, AF.Sqrt, ['ssq'], ['ssq'], scale=1.0 / 64, bias=EPS)
            recip(ssq, ssq, ['ssq'], ['ssq'])
            tt('dve', x3_[:, 0:10, :], x3_[:, 0:10, :], ssq.unsqueeze(2).to_broadcast([128, 10, 64]), ALU.mult, [XA, XB, 'ssq'], [XA, XB])
            tt('pool', xq_[:, 0:640], xq_[:, 0:640], gq, ALU.mult, [XA, XB, 'gq'], [XA, XB])
            cb_ = csL[k % 4][:, 0:32].unsqueeze(1).to_broadcast([128, 18, 32])
            sb_ = csL[k % 4][:, 32:64].unsqueeze(1).to_broadcast([128, 18, 32])
            XQ = [XA, XB, XC]
            tt('dve', t1, x3_[:, :, 0:32], cb_, ALU.mult, XQ + [ck], ['t1'])
            tt('pool', t2, x3_[:, :, 32:64], sb_, ALU.mult, XQ + [ck], ['t2'])
            tt('dve', xr[:, :, 0:32], t1, t2, ALU.subtract, ['t1', 't2'], ['xr_a'])
            tt('pool', t3, x3_[:, :, 0:32], sb_, ALU.mult, XQ + [ck], ['t3'])
            tt('dve', t4, x3_[:, :, 32:64], cb_, ALU.mult, XQ + [ck], ['t4'])
            tt('pool', xr[:, :, 32:64], t3, t4, ALU.add, ['t3', 't4'], ['xr_b'])
            if not layer0:
                dma('sp', qtm[t * 128:(t + 1) * 128, :], xrf[:, 0:512], ['xr_a', 'xr_b'], ['qtm'])
            cp('pool', tmt_[:, 384:640], xrf[:, 896:1152], ['xr_a', 'xr_b'], ['tmt_c' + q_])
            dma('sp', tmo[t * 128:(t + 1) * 128, :], tmt_, ['tmt_a' + q_, 'tmt_b' + q_, 'tmt_c' + q_], ['tmo'])
            trs([(psb[:, b_ * 128:(b_ + 1) * 128], xrf[:, b_ * 128:(b_ + 1) * 128]) for b_ in range(8)], ['xr_a', 'xr_b', 'cb'], ['ps7'])
            cp('act', qkg[:, 0:8, js], psb[:, :].rearrange("p (k t) -> p k t", k=8), ['ps7'], ['qkg'])
            trs([(psb[:, 0:128], xrf[:, 1024:1152])], ['xr_a', 'xr_b', 'cb'], ['ps7'])
            cp('act', qkg[:, 8, js], psb[:, 0:128], ['ps7'], ['qkg'])

        def group_end(gi):
            g0, N = groups[gi]
            nt = N // 128
            ag = agrp[gi % 2]
            AK = 'ag%d' % (gi % 2)
            dma('sp', qkT[:, g0:g0 + N].rearrange("(b p) t -> p b t", p=128), qkg[:, :, 0:N], ['qkg'], ['qkT'])
            dma('sp', aT[:, g0:g0 + N].rearrange("(k p) t -> p k t", p=128), ag[:, :, 0:N], [AK], ['aT'])
            rhs = [ag[:, kk, 0:N] for kk in range(8)]
            for oc in range(2):
                mmg(psf[4][:, 0:N], [(wi[:, kk, oc * 128:(oc + 1) * 128], rhs[kk]) for kk in range(8)], [AK, 'wi'], [PK[4]])
                cp('act', fnT[:, oc, 0:N], psf[4][:, 0:N], [PK[4]], ['fnT'])
                mmg(psf[5][:, 0:N], [(wi[:, kk, 1024 + oc * 128:1024 + (oc + 1) * 128], rhs[kk]) for kk in range(8)], [AK, 'wi'], [PK[5]])
                act(rgt[:, oc, 0:N], psf[5][:, 0:N], AF.Silu, [PK[5]], ['rgt'])
            dma('sp', rgT[:, g0:g0 + N].rearrange("(c p) t -> p c t", p=128), rgt[:, :, 0:N], ['rgt'], ['rgT'])
            for oc in range(2):
                mmg(psf[4][:, 0:N], [(wi[:, kk, 2048 + oc * 128:2048 + (oc + 1) * 128], rhs[kk]) for kk in range(8)], [AK, 'wi'], [PK[4]])
                mmg(psf[5][:, 0:N], [(wi[:, kk, 2304 + oc * 128:2304 + (oc + 1) * 128], rhs[kk]) for kk in range(8)], [AK, 'wi'], [PK[5]])
                act(sg[:, 0:N], psf[5][:, 0:N], AF.Sigmoid, [PK[5]], ['sg'])
                tt('dve', cy[:, oc, 0:N], psf[4][:, 0:N], sg[:, 0:N], ALU.mult, [PK[4], 'sg'], ['cy'])
            dma('sp', cyT[:, g0:g0 + N].rearrange("(c p) t -> p c t", p=128), cy[:, :, 0:N], ['cy'], ['cyT'])
            for j in range(nt):
                js = slice(j * 128, (j + 1) * 128)
                mmg(psf[6][:, :], [(fnT[:, fc, js], bd1[:, fc, :]) for fc in range(2)], ['fnT', 'cb'], [PK[6]])
                cp('act', abt1[:, j, :], psf[6][:, :], [PK[6]], ['abt1'])
                mmg(psf[6][:, :], [(fnT[:, fc, js], bd2[:, fc, :]) for fc in range(2)], ['fnT', 'cb'], [PK[6]])
                cp('dve', abt2[:, j, :], psf[6][:, :], [PK[6]], ['abt2'])
            dma('sp', ab1[g0:g0 + N, :].rearrange("(j p) f -> p j f", p=128), abt1[:, 0:nt, :], ['abt1'], ['ab1'])
            dma('sp', ab2[g0:g0 + N, :].rearrange("(j p) f -> p j f", p=128), abt2[:, 0:nt, :], ['abt2'], ['ab2'])

        pload(0)
        front(0)
        for k in range(ntl):
            if k + 1 < ntl:
                front(k + 1)
            back(k)
            gi, j = tinfo[k]
            if j == groups[gi][1] // 128 - 1:
                group_end(gi)
        P.barrier()

    def stage_attn(i, with_ctx, own=False):
        areset()
        Qsrc = qTq if own else qkT
        Bdst = brTq if own else brT
        glist = qgroups if own else [g for gi_, g in enumerate(groups) if not (gi_ == 0 and not with_ctx)]
        ka = alloc([128, T], BF16)
        va = alloc([128, NT, 128], BF16)
        qt = alloc([128, 4, 512], BF16)
        mset('pool', ka[64:128, :], 0.0, ['ka_z'])
        mset('pool', qt[64:128, :, :], 0.0, ['qt_z'])
        pt = [alloc([128, 2, 512], BF16) for _ in range(3)]
        rc = alloc([128, 512], F32)
        ot = alloc([64, 4, 512], BF16)
        mset('pool', va[:, :, 64:128], 1.0, ['va1'])
        cnt = 0
        for kv in range(2):
            dma('sp', ka[0:64, :], qkT[512 + kv * 64:512 + (kv + 1) * 64, :], [], ['ka'])
            dma('sp', va[:, :, 0:64], tmo[:, kv * 64:(kv + 1) * 64].rearrange("(n p) d -> p n d", p=128), [], ['va0'])
            for (g0, N) in glist:
                kbs = [0, 1] if (g0 == 0 and not own) else list(range(NT))
                dma('sp', qt[0:64, :, 0:N], Qsrc[kv * 256:(kv + 1) * 256, g0:g0 + N].rearrange("(h d) t -> d h t", d=64), [], ['qt'])
                for pr in range(2):
                    its = list(kbs)
                    base = cnt
                    cnt += len(its)

                    def emit_S(ii, its=its, base=base, N=N, pr=pr):
                        kb = its[ii]
                        r_ = (base + ii) % 3
                        mms([(pw[r_][:, a_ * 512:a_ * 512 + N], [(ka[:, kb * 128:(kb + 1) * 128], qt[:, 2 * pr + a_, 0:N])]) for a_ in range(2)],
                            ['ka', 'qt', 'ka_z', 'qt_z'], [PK[2 * r_], PK[2 * r_ + 1]])
                        act(pt[r_][:, :, 0:N], pw[r_][:, :].rearrange("p (a b) -> p a b", a=2)[:, :, 0:N], AF.Exp,
                            [PK[2 * r_], PK[2 * r_ + 1]], ['pt%d' % r_], scale=0.125)

                    def emit_PV(ii, its=its, base=base, N=N, pr=pr):
                        kb = its[ii]
                        r_ = (base + ii) % 3

                        def fn(e, kb=kb, r_=r_, N=N, s_=(ii == 0), sp_=(ii == len(its) - 1)):
                            ins = None
                            for a_ in range(2):
                                ins = e.matmul(psf[6 + a_][:, 0:N], lhsT=va[:, kb, :], rhs=pt[r_][:, a_, 0:N], start=s_, stop=sp_)
                            return ins
                        P.add('pe', fn, ['va0', 'va1', 'pt%d' % r_], [PK[6], PK[7]])

                    for ii in range(min(2, len(its))):
                        emit_S(ii)
                    for ii in range(len(its)):
                        if ii + 2 < len(its):
                            emit_S(ii + 2)
                        emit_PV(ii)
                    for a_ in range(2):
                        hh = 2 * pr + a_
                        recip(rc[64:128, 0:N], psf[6 + a_][64:128, 0:N], [PK[6 + a_]], ['rc'])
                        tt('dve', ot[:, hh, 0:N], psf[6 + a_][0:64, 0:N], rc[64:128, 0:N], ALU.mult, [PK[6 + a_], 'rc'], ['ot'])
                dma('sp', Bdst[512 + kv * 256:512 + (kv + 1) * 256, g0:g0 + N].rearrange("(h d) t -> d h t", d=64), ot[:, :, 0:N], ['ot'], ['brT'])
        P.barrier()

    def stage_fnet(i, with_ctx, tr=True):
        areset()
        LB = min(16, L0)
        x1b = alloc([128, LB, 512], BF16); x2b = alloc([128, LB, 512], BF16); zb = alloc([128, LB, 512], BF16)
        u1 = alloc([128, 256], F32); u2 = alloc([128, 256], F32)
        z3 = alloc([L0, 16, 512], BF16); ysb = alloc([L0, 16, 256], BF16)
        xc = alloc([128, 2, 512], BF16); yc = alloc([128, 2, 256], BF16)
        yt = [alloc([128, 256], BF16) for _ in range(2)]
        yT = [alloc([128, 2, 128], BF16) for _ in range(2)]
        a1v = ab1[256:256 + L, :].rearrange("(p a) f -> p a f", a=L0)
        a2v = ab2[256:256 + L, :].rearrange("(p a) f -> p a f", a=L0)
        twc = CF('twc'); tws = CF('tws')
        for blk in range(L0 // LB):
            bs = slice(blk * LB, (blk + 1) * LB)
            dma('sp', x1b, a1v[:, bs, :], [], ['x1b'])
            dma('sp', x2b, a2v[:, bs, :], [], ['x2b'])
            for a in range(LB):
                l0 = blk * LB + a
                pz = psf[a % 2]
                k_ = PK[a % 2]
                mmg(pz[:, :], [(CB('c128'), x1b[:, a, :]), (CB('s128'), x2b[:, a, :])], ['x1b', 'x2b', 'cb'], [k_])
                ts('dve', u1, pz[:, 256:512], tws[:, l0:l0 + 1], None, ALU.mult, None, [k_, 'cf'], ['u1'])
                stt(zb[:, a, 0:256], pz[:, 0:256], twc[:, l0:l0 + 1], u1, ALU.mult, ALU.add, [k_, 'u1', 'cf'], ['zb'])
                ts('dve', u2, pz[:, 0:256], tws[:, l0:l0 + 1], None, ALU.mult, None, [k_, 'cf'], ['u2'])
                stt(zb[:, a, 256:512], pz[:, 256:512], twc[:, l0:l0 + 1], u2, ALU.mult, ALU.subtract, [k_, 'u2', 'cf'], ['zb'])
            dma('sp', zs[:, bs, :], zb, ['zb'], ['zs'])
        P.barrier()
        yv = ytm[256:256 + L, :].rearrange("(k0 k1) f -> k0 k1 f", k1=128)
        for blk in range(8):
            bs = slice(blk * 16, (blk + 1) * 16)
            dma('sp', z3, zs[bs, :, :].rearrange("k a f -> a k f"), [], ['z3'])
            for kk in range(16):
                pz = psf[2 + (kk // 2) % 2]
                k_ = PK[2 + (kk // 2) % 2]
                mmg(pz[0:L0, (kk % 2) * 256:(kk % 2 + 1) * 256], [(CB('c64', L0), z3[:, kk, 0:256]), (CB('s64', L0), z3[:, kk, 256:512])],
                    ['z3', 'cb'], [k_])
                if kk % 2 == 1:
                    cp('act', ysb[:, kk - 1:kk + 1, :], pz[0:L0, :].rearrange("p (a b) -> p a b", a=2), [k_], ['ysb'])
            dma('sp', yv[:, bs, :], ysb, ['ysb'], ['ytm'])
        if with_ctx:
            dma('sp', xc, ab1[0:256, :].rearrange("(c p) f -> p c f", p=128), [], ['xc'])
            ccv = CB('cc').rearrange("p (a b) -> p a b", a=2); scv = CB('sc').rearrange("p (a b) -> p a b", a=2)
            for kc in range(2):
                ks = slice(kc * 128, (kc + 1) * 128)
                mmg(psf[4 + kc][:, 0:256], [(ccv[:, lc, ks], xc[:, lc, 0:256]) for lc in range(2)] + [(scv[:, lc, ks], xc[:, lc, 256:512]) for lc in range(2)],
                    ['xc', 'cb'], [PK[4 + kc]])
                cp('act', yc[:, kc, :], psf[4 + kc][:, 0:256], [PK[4 + kc]], ['yc'])
            dma('sp', ytm[0:256, :].rearrange("(c p) f -> p c f", p=128), yc, ['yc'], ['ytm'])
        P.barrier()
        for t in (range(0 if with_ctx else 2, NT) if tr else []):
            r_ = t % 2
            dma('sp', yt[r_], ytm[t * 128:(t + 1) * 128, :], [], ['yt%d' % r_])
            trs([(psb[:, c_ * 128:(c_ + 1) * 128], yt[r_][:, c_ * 128:(c_ + 1) * 128]) for c_ in range(2)], ['yt%d' % r_, 'cb'], ['ps7'])
            cp('act', yT[r_], psb[:, 0:256].rearrange("p (a b) -> p a b", a=2), ['ps7'], ['yT%d' % r_])
            dma('sp', brT[0:256, t * 128:(t + 1) * 128].rearrange("(c p) t -> p c t", p=128), yT[r_], ['yT%d' % r_], ['brT'])
        P.barrier()

    def stage_ret(i, with_ctx):
        areset()
        Sf_all = alloc([64, NT, 4, 64], BF16); Sb_all = alloc([64, NT, 4, 64], BF16)
        lgt = alloc([128, 8], F32); e1 = alloc([128, 8], F32)
        dcomb = alloc([128, 4, 128], BF16); ef = alloc([128, 128], F32); eb = alloc([128, 128], F32)
        xif = alloc([64, 4, 128], BF16); xib = alloc([64, 4, 128], BF16)
        zf = alloc([128, 4], F32); zbk = alloc([128, 4], F32)
        gcf = alloc([64, 4, 64], F32); gcb = alloc([64, 4, 64], F32)
        grn = alloc([64, 4], F32)
        S = alloc([64, 4, 64], F32)
        tmn = [alloc([128, 640], BF16) for _ in range(2)]
        kz = alloc([128, 4, 64], BF16)
        qk = [alloc([64, 8, 128], BF16) for _ in range(2)]
        rgn = [alloc([64, 4, 128], BF16) for _ in range(2)]
        am = alloc([128, 4, 128], BF16); qxf = alloc([64, 4, 128], BF16); qxb = alloc([64, 4, 128], BF16)
        osq = alloc([64, 512], F32); rs = alloc([64, 512], F32); o1 = alloc([64, 512], F32)
        ob = [alloc([64, 4, 128], BF16) for _ in range(2)]
        dma('sp', lgt, dec2[i].partition_broadcast(128), [], ['lgt'])
        dma('sp', grn, rng[i], [], ['grn'])
        act(e1, lgt, AF.Exp, ['lgt'], ['e1'], scale=-1.0)
        act(e1, e1, AF.Ln, ['e1'], ['e1'], bias=1.0)
        ts('dve', lgt, e1, -1.0, None, ALU.mult, None, ['e1'], ['lgt'])
        for h in range(4):
            act(ef, CF('DIFFP'), AF.Exp, ['lgt', 'cf'], ['ef'], scale=lgt[:, h:h + 1])
            tt('dve', ef, ef, CF('MLT'), ALU.mult, ['ef', 'cf'], ['ef'])
            act(eb, CF('DIFFN'), AF.Exp, ['lgt', 'cf'], ['eb'], scale=lgt[:, 4 + h:5 + h])
            tt('dve', eb, eb, CF('MGT'), ALU.mult, ['eb', 'cf'], ['eb'])
            tt('dve', ef, ef, eb, ALU.add, ['ef', 'eb'], ['ef'])
            tt('dve', dcomb[:, h, :], ef, CF('I2'), ALU.add, ['ef', 'cf'], ['dcomb'])
            act(xif[:, h, :], CF('IP1', 64), AF.Exp, ['lgt', 'cf'], ['xif'], scale=lgt[0:64, h:h + 1])
            act(xib[:, h, :], CF('IB', 64), AF.Exp, ['lgt', 'cf'], ['xib'], scale=lgt[0:64, 4 + h:5 + h])
            act(zf[:, h:h + 1], CF('P127'), AF.Exp, ['lgt', 'cf'], ['zf'], scale=lgt[:, h:h + 1])
            act(zbk[:, h:h + 1], CF('PJ'), AF.Exp, ['lgt', 'cf'], ['zbk'], scale=lgt[:, 4 + h:5 + h])
            act(gcf[:, h, :], CF('C128T', 64), AF.Exp, ['lgt', 'cf'], ['gcf'], scale=lgt[0:64, h:h + 1])
            act(gcb[:, h, :], CF('C128T', 64), AF.Exp, ['lgt', 'cf'], ['gcb'], scale=lgt[0:64, 4 + h:5 + h])

        S2 = alloc([64, 4, 64], F32)
        kz2 = alloc([128, 4, 64], BF16)
        tmb = [alloc([128, 640], BF16) for _ in range(2)]

        def mk_sweep(order, z, zk, gc, gk, S_all, sk, Sx, skey, kzx, kzkey, tmx, tmkey, psx, pskey):
            def init():
                mset('dve', Sx, 0.0, [skey])

            def step(ii):
                n = order[ii]
                r_ = ii % 2
                cp('act', S_all[:, n], Sx, [skey], [sk])
                dma('sp', tmx[r_], tmo[n * 128:(n + 1) * 128, :], [], [tmkey + str(r_)])
                tt('dve', kzx, tmx[r_][:, 384:640].rearrange("p (h d) -> p h d", h=4), z.unsqueeze(2).to_broadcast([128, 4, 64]), ALU.mult,
                   [tmkey + str(r_), zk], [kzkey])
                mms([(psx[0:64, h * 64:(h + 1) * 64], [(kzx[:, h, :], tmx[r_][:, 128 + h * 64:128 + (h + 1) * 64])]) for h in range(4)],
                    [kzkey, tmkey + str(r_)], [pskey])
                tt('dve', Sx, Sx, gc, ALU.mult, [skey, gk], [skey])
                tt('dve', Sx.rearrange("p h d -> p (h d)"), Sx.rearrange("p h d -> p (h d)"), psx[0:64, 0:256], ALU.add, [skey, pskey], [skey])
            return init, step

        fi, fs_ = mk_sweep(list(range(NT)), zf, 'zf', gcf, 'gcf', Sf_all, 'Sf', S, 'S', kz, 'kz', tmn, 'tmn', psf[0], PK[0])
        bi, bs_ = mk_sweep([1, 0] + list(range(NT - 1, 1, -1)), zbk, 'zbk', gcb, 'gcb', Sb_all, 'Sb', S2, 'S2', kz2, 'kz2', tmb, 'tmb', psf[4], PK[4])
        fi(); bi()
        for ii in range(NT):
            fs_(ii)
            bs_(ii)
        amL = [alloc([128, 4, 128], BF16) for _ in range(2)]
        qxfL = [alloc([64, 4, 128], BF16) for _ in range(2)]; qxbL = [alloc([64, 4, 128], BF16) for _ in range(2)]
        osqL = [alloc([64, 512], F32) for _ in range(2)]; rsL = [alloc([64, 512], F32) for _ in range(2)]; o1L = [alloc([64, 512], F32) for _ in range(2)]
        chunks = list(range(0 if with_ctx else 2, NT))

        def oload(ii):
            n = chunks[ii]
            r_ = ii % 2
            ns = slice(n * 128, (n + 1) * 128)
            dma('sp', qk[r_], qkT[640:1152, ns].rearrange("(h d) t -> d h t", d=64), [], ['qk%d' % r_])
            dma('sp', tmn[r_], tmo[ns, :], [], ['tmn%d' % r_])
            dma('sp', rgn[r_], rgT[:, ns].rearrange("(h e) t -> e h t", e=64), [], ['rgn%d' % r_])

        if chunks:
            oload(0)
        for ii, n in enumerate(chunks):
            if ii + 1 < len(chunks):
                oload(ii + 1)
            r_ = ii % 2
            q_ = str(r_)
            pA, pB, pC = psf[1 + 4 * r_], psf[2 + 4 * r_], psf[3 + 4 * r_]
            kA, kB, kC = PK[1 + 4 * r_], PK[2 + 4 * r_], PK[3 + 4 * r_]
            am_ = amL[r_]; qxf_ = qxfL[r_]; qxb_ = qxbL[r_]; osq_ = osqL[r_]; rs_ = rsL[r_]; o1_ = o1L[r_]
            ns = slice(n * 128, (n + 1) * 128)
            mms([(pA[:, h * 128:(h + 1) * 128], [(qk[r_][:, 4 + h, :], qk[r_][:, h, :])]) for h in range(4)], ['qk%d' % r_], [kA])
            tt('dve', am_.rearrange("p h t -> p (h t)"), pA[:, :], dcomb.rearrange("p h t -> p (h t)"), ALU.mult, [kA, 'dcomb'], ['am' + q_])
            tt('dve', qxf_, qk[r_][:, 0:4, :], xif, ALU.mult, ['qk%d' % r_, 'xif'], ['qxf' + q_])
            tt('pool', qxb_, qk[r_][:, 0:4, :], xib, ALU.mult, ['qk%d' % r_, 'xib'], ['qxb' + q_])
            mms([(pB[0:64, h * 128:(h + 1) * 128],
                  [(tmn[r_][:, 128 + h * 64:128 + (h + 1) * 64], am_[:, h, :]), (Sf_all[:, n, h, :], qxf_[:, h, :]), (Sb_all[:, n, h, :], qxb_[:, h, :])])
                 for h in range(4)], ['tmn%d' % r_, 'am' + q_, 'Sf', 'Sb', 'qxf' + q_, 'qxb' + q_], [kB])
            act(osq_, pB[0:64, :], AF.Square, [kB], ['osq' + q_])
            mmg(pC[0:64, :], [(CF('on64', 64), osq_)], ['osq' + q_, 'cf'], [kC])
            act(rs_, pC[0:64, :], AF.Sqrt, [kC], ['rs' + q_], bias=EPS)
            recip(rs_, rs_, ['rs' + q_], ['rs' + q_])
            tt('dve', o1_, pB[0:64, :], rs_, ALU.mult, [kB, 'rs' + q_], ['o1' + q_])
            o13 = o1_.rearrange("p (h t) -> p h t", h=4)
            tt('dve', o13, o13, grn.unsqueeze(2).to_broadcast([64, 4, 128]), ALU.mult, ['o1' + q_, 'grn'], ['o1' + q_])
            tt('pool', ob[r_], o13, rgn[r_], ALU.mult, ['o1' + q_, 'rgn%d' % r_], ['ob%d' % r_])
            dma('sp', brT[256:512, ns].rearrange("(h e) t -> e h t", e=64), ob[r_], ['ob%d' % r_], ['brT'])
        P.barrier()

    def stage_conv(i, with_ctx):
        areset()
        yb = alloc([128, 2, L + 30], BF16)
        ybc = alloc([128, 2, C + 30], BF16)
        diag = alloc([128, 2, 31, 128], BF16)
        dgw = alloc([128, 62], F32); cbp = alloc([128, 6], F32)
        z = alloc([128, 2, 512], F32); zq = alloc([128, 2, 512], F32)
        mu = alloc([128, 512], F32); var = alloc([128, 512], F32)
        co = [alloc([128, 2, 512], BF16) for _ in range(2)]
        dma('sp', dgw, dwa[i], [], ['dgw'])
        dma('sp', cbp, cba[i], [], ['cbp'])
        mset('pool', yb[:, :, 0:15], 0.0, ['yb_h0'])
        mset('pool', yb[:, :, L + 15:L + 30], 0.0, ['yb_h1'])
        mset('pool', ybc[:, :, 0:15], 0.0, ['ybc_h0'])
        mset('pool', ybc[:, :, C + 15:C + 30], 0.0, ['ybc_h1'])
        dma('sp', yb[:, :, 15:15 + L], cyT[:, 256:256 + L].rearrange("(c p) t -> p c t", p=128), [], ['yb'])
        dma('sp', ybc[:, :, 15:15 + C], cyT[:, 0:C].rearrange("(c p) t -> p c t", p=128), [], ['ybc'])
        for c_ in range(2):
            for tap in range(31):
                ts('dve', diag[:, c_, tap, :], CF('identF'), dgw[:, c_ * 31 + tap:c_ * 31 + tap + 1], None, ALU.mult, None, ['cf', 'dgw'], ['diag'])
        for gi, (g0, N) in enumerate(groups):
            if gi == 0 and not with_ctx:
                continue
            buf, bk, off = (ybc, ['ybc', 'ybc_h0', 'ybc_h1'], 0) if gi == 0 else (yb, ['yb', 'yb_h0', 'yb_h1'], g0 - 256)
            r_ = gi % 2
            for c_ in range(2):
                mmg(psf[c_][:, 0:N], [(diag[:, c_, tap, :], buf[:, c_, off + tap:off + tap + N]) for tap in range(31)], ['diag'] + bk, [PK[c_]])
                act(z[:, c_, 0:N], psf[c_][:, 0:N], AF.Identity, [PK[c_], 'cbp'], ['z%d' % c_], bias=cbp[:, c_ * 3:c_ * 3 + 1])
                act(zq[:, c_, 0:N], z[:, c_, 0:N], AF.Square, ['z%d' % c_], ['zq%d' % c_])
            mmg(psf[2][:, 0:N], [(CF('on128'), z[:, c_, 0:N]) for c_ in range(2)], ['z0', 'z1', 'cf'], [PK[2]])
            mmg(psf[3][:, 0:N], [(CF('on128'), zq[:, c_, 0:N]) for c_ in range(2)], ['zq0', 'zq1', 'cf'], [PK[3]])
            cp('dve', mu[:, 0:N], psf[2][:, 0:N], [PK[2]], ['mu'])
            tt('dve', var[:, 0:N], mu[:, 0:N], mu[:, 0:N], ALU.mult, ['mu'], ['var'])
            tt('dve', var[:, 0:N], psf[3][:, 0:N], var[:, 0:N], ALU.subtract, [PK[3], 'var'], ['var'])
            act(var[:, 0:N], var[:, 0:N], AF.Sqrt, ['var'], ['var'], bias=EPS)
            recip(var[:, 0:N], var[:, 0:N], ['var'], ['var'])
            for c_ in range(2):
                zk = 'z%d' % c_
                tt('dve', z[:, c_, 0:N], z[:, c_, 0:N], mu[:, 0:N], ALU.subtract, [zk, 'mu'], [zk])
                tt('dve', z[:, c_, 0:N], z[:, c_, 0:N], var[:, 0:N], ALU.mult, [zk, 'var'], [zk])
                ts('dve', z[:, c_, 0:N], z[:, c_, 0:N], cbp[:, c_ * 3 + 1:c_ * 3 + 2], cbp[:, c_ * 3 + 2:c_ * 3 + 3], ALU.mult, ALU.add, [zk, 'cbp'], [zk])
                act(co[r_][:, c_, 0:N], z[:, c_, 0:N], AF.Silu, [zk], ['co%d' % r_])
            dma('sp', brT[1024:1280, g0:g0 + N].rearrange("(c p) t -> p c t", p=128), co[r_][:, :, 0:N], ['co%d' % r_], ['brT'])
        P.barrier()

    def stage_merge(i, with_ctx, own=False):
        layer0 = (i == 0)
        areset()
        Asrc = aTq1 if own else aT
        Bsrc = brTq if own else brT
        mgl = [(0, g0, N) for (g0, N) in qgroups] if own else [((1 if gi_ == 0 else 0), g0, N) for gi_, (g0, N) in enumerate(groups) if not (gi_ == 0 and not with_ctx)]
        if own:
            idx = alloc([128, LQ // 128], U32)
            dma('sp', idx, own_idx, [], ['idx'])
        wg = alloc([128, 8, 4096], BF16)
        wbr = alloc([128, 10, 1024], BF16)
        wo = alloc([128, 8, 1024], BF16)
        dma('pool', wg, w_in[i, :, 2560:6656].rearrange("(k p) n -> p k n", p=128), [], ['wg'])
        dma('pool', wbr[:, 0:2, :], fnet_w[i].rearrange("(k p) n -> p k n", p=128), [], ['wbr0'])
        dma('pool', wbr[:, 2:4, :], ret_w[i].rearrange("(k p) n -> p k n", p=128), [], ['wbr1'])
        dma('pool', wbr[:, 4:8, :], attn_w[i].rearrange("(k p) n -> p k n", p=128), [], ['wbr2'])
        dma('pool', wbr[:, 8:10, :], conv_wo[i].rearrange("(k p) n -> p k n", p=128), [], ['wbr3'])
        dma('pool', wo, w_out[i].rearrange("(k p) n -> p k n", p=128), [], ['wo'])
        gate = [alloc([128, 1024], F32) for _ in range(2)]
        for wh in range(2):
            dma('sp', gate[wh], modr[wh, :, 2048:3072], [], ['gate%d' % wh])
        at = [alloc([128, 8, 512], BF16)] * 2
        bt = [alloc([128, 10, 512], BF16)] * 2
        sg = [alloc([128, 512], BF16) for _ in range(2)]
        macc = alloc([128, 512], F32); tmp = alloc([128, 512], F32)
        mg = alloc([128, 8, 512], BF16)
        ht = [alloc([128, 1024], F32) for _ in range(2)]
        tq = alloc([128, 1024], F32)
        KB = {0: [0, 1], 1: [2, 3], 2: [4, 5, 6, 7], 3: [8, 9]}
        tix = 0
        mtiles = [g0 // 128 + j for (wh_, g0, N) in mgl for j in range(N // 128)]

        def mload(k):
            t_ = mtiles[k]
            if own:
                P.add('pool', (lambda e, o=ht[k % 2], ix=idx[:, t_:t_ + 1]: e.indirect_dma_start(
                    out=o, out_offset=None, in_=hb, in_offset=bass.IndirectOffsetOnAxis(ap=ix, axis=0))),
                    ['idx'], ['ht%d' % (k % 2)], dma=True)
            else:
                dma('sp', ht[k % 2], hsrc(layer0, t_), ['hb%d' % t_], ['ht%d' % (k % 2)])

        for (wh, g0, N) in mgl:
            r_ = 0
            dma('sp', at[r_][:, :, 0:N], Asrc[:, g0:g0 + N].rearrange("(k p) t -> p k t", p=128), [], ['at%d' % r_])
            dma('sp', bt[r_][:, :, 0:N], Bsrc[:, g0:g0 + N].rearrange("(k p) t -> p k t", p=128), [], ['bt%d' % r_])
            for fc in range(8):
                for b in range(4):
                    mmg(psf[b % 2][:, 0:N], [(wg[:, k, b * 1024 + fc * 128:b * 1024 + (fc + 1) * 128], at[r_][:, k, 0:N]) for k in range(8)],
                        ['wg', 'at%d' % r_], [PK[b % 2]])
                    act(sg[b % 2][:, 0:N], psf[b % 2][:, 0:N], AF.Sigmoid, [PK[b % 2]], ['sg%d' % (b % 2)])
                    mmg(psf[2 + b % 2][:, 0:N], [(wbr[:, kb, fc * 128:(fc + 1) * 128], bt[r_][:, kb, 0:N]) for kb in KB[b]],
                        ['wbr%d' % b, 'bt%d' % r_], [PK[2 + b % 2]])
                    if b == 0:
                        tt('dve', macc[:, 0:N], psf[2][:, 0:N], sg[0][:, 0:N], ALU.mult, [PK[2], 'sg0'], ['macc'])
                    else:
                        tt('dve', tmp[:, 0:N], psf[2 + b % 2][:, 0:N], sg[b % 2][:, 0:N], ALU.mult, [PK[2 + b % 2], 'sg%d' % (b % 2)], ['tmp'])
                        if b < 3:
                            tt('pool', macc[:, 0:N], macc[:, 0:N], tmp[:, 0:N], ALU.add, ['macc', 'tmp'], ['macc'])
                        else:
                            tt('pool', mg[:, fc, 0:N], macc[:, 0:N], tmp[:, 0:N], ALU.add, ['macc', 'tmp'], ['mg'])
            for j in range(N // 128):
                t = g0 // 128 + j
                h_ = tix % 2
                tix += 1
                js = slice(j * 128, (j + 1) * 128)
                if tix == 1:
                    mload(0)
                if tix < len(mtiles):
                    mload(tix)
                for half in range(2):
                    hs = slice(half * 512, (half + 1) * 512)
                    mmg(psf[4 + half][:, :], [(mg[:, fc, js], wo[:, fc, hs]) for fc in range(8)], ['mg', 'wo'], [PK[4 + half]])
                    tt('dve', tq[:, hs], psf[4 + half][:, :], gate[wh][:, hs], ALU.mult, [PK[4 + half], 'gate%d' % wh], ['tq'])
                    tt('pool', ht[h_][:, hs], ht[h_][:, hs], tq[:, hs], ALU.add, ['ht%d' % h_, 'tq'], ['ht%d' % h_])
                dma('sp', (hq if own else hb)[t * 128:(t + 1) * 128, :], ht[h_], ['ht%d' % h_], ['hb%d' % t])
        P.barrier()

    def stage_fm2tm(i):
        areset()
        fm = [alloc([128, 4, 128], BF16) for _ in range(2)]
        tmr = [alloc([128, 512], BF16) for _ in range(2)]
        for t in range(2, NT):
            r_ = t % 2
            tsl = slice(t * 128, (t + 1) * 128)
            dma('sp', fm[r_][:, 0:2, :], brT[256:512, tsl].rearrange("(c p) t -> p c t", p=128), [], ['fm%da' % r_])
            dma('sp', fm[r_][:, 2:4, :], brT[1024:1280, tsl].rearrange("(c p) t -> p c t", p=128), [], ['fm%db' % r_])
            trs([(psb[:, c_ * 128:(c_ + 1) * 128], fm[r_][:, c_, :]) for c_ in range(4)], ['fm%da' % r_, 'fm%db' % r_, 'cb'], ['ps7'])
            cp('act', tmr[r_], psb[:, 0:512], ['ps7'], ['tmr%d' % r_])
            dma('sp', rctm[tsl, :], tmr[r_], ['tmr%d' % r_], ['rctm'])
        P.barrier()

    def stage_compact(i, srcs):
        areset()
        idx = alloc([128, LQ // 128], U32)
        dma('sp', idx, own_idx, [], ['idx'])
        gbuf = {}
        cbuf = {}
        for si, (src, W, dsts) in enumerate(srcs):
            gbuf[si] = [alloc([128, W], BF16) for _ in range(2)]
            cbuf[si] = [alloc([128, W // 128, 128], BF16) for _ in range(2)]
        for j in range(LQ // 128):
            r_ = j % 2
            jsl = slice(j * 128, (j + 1) * 128)
            for si, (src, W, dsts) in enumerate(srcs):
                gk = 'g%d_%d' % (si, r_)
                ck = 'c%d_%d' % (si, r_)
                P.add('pool', (lambda e, o=gbuf[si][r_], ix=idx[:, j:j + 1], src=src: e.indirect_dma_start(
                    out=o, out_offset=None, in_=src, in_offset=bass.IndirectOffsetOnAxis(ap=ix, axis=0))),
                    ['idx'], [gk], dma=True)
                nb = W // 128
                trs([(psb[:, b_ * 128:(b_ + 1) * 128], gbuf[si][r_][:, b_ * 128:(b_ + 1) * 128]) for b_ in range(nb)], [gk, 'cb'], ['ps7'])
                cp('act', cbuf[si][r_], psb[:, 0:W].rearrange("p (k t) -> p k t", k=nb), ['ps7'], [ck])
                for (dst, row0, blk0, nblk) in dsts:
                    dma('sp', dst[row0:row0 + nblk * 128, jsl].rearrange("(c p) t -> p c t", p=128), cbuf[si][r_][:, blk0:blk0 + nblk, :], [ck], ['cdst'])
        P.barrier()

    def stage_ffn_norm(i, with_ctx, moe):
        areset()
        gs, sh = load_mod(i, n2g, 3, 4)
        ht = [alloc([128, 1024], F32) for _ in range(2)]
        ss = [alloc([128, 1], F32) for _ in range(2)]
        junk = alloc([128, 1024], BF16)
        a1 = alloc([128, 1024], F32); a2 = alloc([128, 1024], BF16)
        agrp = [alloc([128, 8, 512], BF16) for _ in range(2)]
        if moe:
            a2f = alloc([128, 1024], F32)
            rw = alloc([128, NE, 1024], F32)
            junk2 = alloc([128, 1024], F32)
            lg = alloc([128, 8], F32); lg2 = alloc([128, 8], F32)
            m1 = alloc([128, 1], F32); m2 = alloc([128, 1], F32); dd = alloc([128, 1], F32); w1 = alloc([128, 1], F32)
            eq1 = alloc([128, 8], F32); eq2 = alloc([128, 8], F32)
            idx = alloc([128, LQ // 128], U32)
            dma('sp', rw, rwT.partition_broadcast(128), [], ['rw'])
            dma('sp', idx, own_idx, [], ['idx'])
            glist = [(0, k * 512, 512) for k in range(LQ // 512)]
        else:
            glist = [((1 if gi == 0 else 0), g0, N) for gi, (g0, N) in enumerate(groups) if not (gi == 0 and not with_ctx)]
        tiles = [(gi, j) for gi, (wh, g0, N) in enumerate(glist) for j in range(N // 128)]

        def load(k):
            gi, j = tiles[k]
            wh, g0, N = glist[gi]
            t = g0 // 128 + j
            r_ = k % 2
            dma('sp', ht[r_], (hq if moe else hb)[t * 128:(t + 1) * 128, :], [], ['r%dht' % r_])

        if tiles:
            load(0)
        for k in range(len(tiles)):
            if k + 1 < len(tiles):
                load(k + 1)
            gi, j = tiles[k]
            wh, g0, N = glist[gi]
            t = g0 // 128 + j
            r_ = k % 2
            k_ = 'r%d' % r_
            gb = gi % 2
            ag = agrp[gb]
            AK = 'ag%d' % gb
            js = slice(j * 128, (j + 1) * 128)
            norm_mod_tile(t, wh, ht[r_], ss[r_], a1, a2, gs, sh, junk, k_, a2f=(a2f if moe else None))
            trs([(psb[:, kk * 128:(kk + 1) * 128], a2[:, kk * 128:(kk + 1) * 128]) for kk in range(8)], ['a2', 'cb'], ['ps7'])
            cp('act', ag[:, :, js], psb[:, :].rearrange("p (k t) -> p k t", k=8), ['ps7'], [AK])
            if moe:
                for e_ in range(NE):
                    tt('dve', junk2, a2f, rw[:, e_, :], ALU.mult, ['a2f', 'rw'], ['junk2'])
                    red(lg[:, e_:e_ + 1], junk2, ALU.add, ['junk2'], ['lg'])
                red(m1, lg, ALU.max, ['lg'], ['m1'])
                ts('dve', eq1, lg, m1[:, 0:1], None, ALU.is_equal, None, ['lg', 'm1'], ['eq1'])
                stt(lg2, eq1, -1e30, lg, ALU.mult, ALU.add, ['eq1', 'lg'], ['lg2'])
                red(m2, lg2, ALU.max, ['lg2'], ['m2'])
                ts('dve', eq2, lg2, m2[:, 0:1], None, ALU.is_equal, None, ['lg2', 'm2'], ['eq2'])
                tt('dve', dd, m2, m1, ALU.subtract, ['m1', 'm2'], ['dd'])
                act(dd, dd, AF.Sigmoid, ['dd'], ['dd'])
                ts('dve', w1, dd, -1.0, 1.0, ALU.mult, ALU.add, ['dd'], ['w1'])
                ts('dve', eq1, eq1, w1[:, 0:1], None, ALU.mult, None, ['eq1', 'w1'], ['eq1'])
                stt(wts[:, t, :], eq2, dd[:, 0:1], eq1, ALU.mult, ALU.add, ['eq2', 'dd', 'eq1'], ['wts'])
            if j == N // 128 - 1:
                dst = aTq if moe else aT
                dma('sp', dst[:, g0:g0 + N].rearrange("(k p) t -> p k t", p=128), ag[:, :, 0:N], [AK], ['aT'])
        P.barrier()

    def stage_ffn_pass(i, with_ctx, wgd, wud, wdd, hp, expert, final):
        areset()
        moe = expert is not None
        c0 = hp * CH * 128
        wgt = alloc([128, 8, CH * 128], BF16); wut = alloc([128, 8, CH * 128], BF16); wdt = alloc([128, CH, 1024], BF16)
        dma('pool', wgt, wgd[:, c0:c0 + CH * 128].rearrange("(k p) n -> p k n", p=128), [], ['wgt'])
        dma('pool', wut, wud[:, c0:c0 + CH * 128].rearrange("(k p) n -> p k n", p=128), [], ['wut'])
        dma('pool', wdt, wdd[c0:c0 + CH * 128, :].rearrange("(k p) n -> p k n", p=128), [], ['wdt'])
        gate = [alloc([128, 1024], F32) for _ in range(2)]
        for wh in range(2):
            dma('sp', gate[wh], modr[wh, :, 5120:6144], [], ['gate%d' % wh])
        ft = [alloc([128, 8, 512], BF16) for _ in range(2)]
        sl = [alloc([128, 512], BF16) for _ in range(2)]
        actT = alloc([128, CH, 512], BF16)
        ht = [alloc([128, 1024], F32) for _ in range(2)]
        tq = alloc([128, 1024], F32)
        if moe:
            glist = [(0, k * 512, 512) for k in range(LQ // 512)]
            hsrc_, asrc_ = hq, aTq
        else:
            glist = [((1 if gi == 0 else 0), g0, N) for gi, (g0, N) in enumerate(groups) if not (gi == 0 and not with_ctx)]
            hsrc_, asrc_ = hb, aT
        tiles = [(gi, j) for gi, (wh, g0, N) in enumerate(glist) for j in range(N // 128)]

        def load_ft(gi):
            wh, g0, N = glist[gi]
            dma('sp', ft[gi % 2][:, :, 0:N], asrc_[:, g0:g0 + N].rearrange("(k p) t -> p k t", p=128), [], ['ft%d' % (gi % 2)])

        def load_ht(k):
            gi, j = tiles[k]
            wh, g0, N = glist[gi]
            t = g0 // 128 + j
            dma('sp', ht[k % 2], hsrc_[t * 128:(t + 1) * 128, :], ['hb%d' % t], ['ht%d' % (k % 2)])

        load_ft(0)
        load_ht(0)
        k = 0
        for gi, (wh, g0, N) in enumerate(glist):
            r_ = gi % 2
            if gi + 1 < len(glist):
                load_ft(gi + 1)
            for c_ in range(CH):
                q_ = c_ % 2
                cs_ = slice(c_ * 128, (c_ + 1) * 128)
                mmg(psf[q_][:, 0:N], [(wgt[:, kk, cs_], ft[r_][:, kk, 0:N]) for kk in range(8)], ['wgt', 'ft%d' % r_], [PK[q_]])
                mmg(psf[2 + q_][:, 0:N], [(wut[:, kk, cs_], ft[r_][:, kk, 0:N]) for kk in range(8)], ['wut', 'ft%d' % r_], [PK[2 + q_]])
                act(sl[q_][:, 0:N], psf[q_][:, 0:N], AF.Silu, [PK[q_]], ['sl%d' % q_])
                tt('dve', actT[:, c_, 0:N], psf[2 + q_][:, 0:N], sl[q_][:, 0:N], ALU.mult, [PK[2 + q_], 'sl%d' % q_], ['actT'])
            for j in range(N // 128):
                t = g0 // 128 + j
                h_ = k % 2
                if k + 1 < len(tiles):
                    load_ht(k + 1)
                k += 1
                js = slice(j * 128, (j + 1) * 128)
                for half in range(2):
                    hs = slice(half * 512, (half + 1) * 512)
                    mmg(psf[4 + half][:, :], [(actT[:, c_, js], wdt[:, c_, hs]) for c_ in range(CH)], ['actT', 'wdt'], [PK[4 + half]])
                    if not moe:
                        tt('dve', tq[:, hs], psf[4 + half][:, :], gate[wh][:, hs], ALU.mult, [PK[4 + half], 'gate%d' % wh], ['tq'])
                    else:
                        stt(tq[:, hs], psf[4 + half][:, :], wts[:, t, expert:expert + 1], gate[wh][:, hs], ALU.mult, ALU.mult,
                            [PK[4 + half], 'gate%d' % wh, 'wts'], ['tq'])
                    tt('pool', ht[h_][:, hs], ht[h_][:, hs], tq[:, hs], ALU.add, ['ht%d' % h_, 'tq'], ['ht%d' % h_])
                if final:
                    dma('sp', out[t * 128:(t + 1) * 128, :], ht[h_], ['ht%d' % h_], ['out'])
                else:
                    dma('sp', hsrc_[t * 128:(t + 1) * 128, :], ht[h_], ['ht%d' % h_], ['hb%d' % t])
        P.barrier()

    def stage_moe(i):
        areset()
        wset = [(alloc([128, 8, CH * 128], BF16), alloc([128, 8, CH * 128], BF16), alloc([128, CH, 1024], BF16)) for _ in range(2)]
        gate0 = alloc([128, 1024], F32)
        dma('sp', gate0, modr[0, :, 5120:6144], [], ['gate0'])
        ftL = [alloc([128, 8, 512], BF16) for _ in range(2)]
        sl = [alloc([128, 512], BF16) for _ in range(2)]
        actT = alloc([128, CH, 512], BF16)
        ht = [alloc([128, 1024], F32) for _ in range(2)]
        tq = alloc([128, 1024], F32)
        passes = [(e_, hp) for e_ in range(NE) for hp in range(HP)]
        glist = [(k_ * 512, 512) for k_ in range(LQ // 512)]
        tiles = [(gi, j) for gi, (g0, N) in enumerate(glist) for j in range(N // 128)]
        seq = [(pi, k) for pi in range(len(passes)) for k in range(len(tiles))]

        def loadw(pi):
            e_, hp = passes[pi]
            c0 = hp * CH * 128
            w_ = wset[pi % 2]
            sfx = str(pi % 2)
            dma('pool', w_[0], moe_wg[0, e_][:, c0:c0 + CH * 128].rearrange("(k p) n -> p k n", p=128), [], ['wgt' + sfx])
            dma('pool', w_[1], moe_wu[0, e_][:, c0:c0 + CH * 128].rearrange("(k p) n -> p k n", p=128), [], ['wut' + sfx])
            dma('pool', w_[2], moe_wd[0, e_][c0:c0 + CH * 128, :].rearrange("(k p) n -> p k n", p=128), [], ['wdt' + sfx])

        def load_ht(si):
            pi, k = seq[si]
            gi, j = tiles[k]
            t = glist[gi][0] // 128 + j
            dma('sp', ht[si % 2], hq[t * 128:(t + 1) * 128, :], ['hb%d' % t], ['ht%d' % (si % 2)])

        gseq = [(pi_, gi_) for pi_ in range(len(passes)) for gi_ in range(len(glist))]

        def load_ft(gq):
            g0_, N_ = glist[gseq[gq][1]]
            dma('sp', ftL[gq % 2][:, :, 0:N_], aTq[:, g0_:g0_ + N_].rearrange("(k p) t -> p k t", p=128), [], ['ft%d' % (gq % 2)])

        loadw(0)
        load_ht(0)
        load_ft(0)
        si = 0
        gq = 0
        for pi, (e_, hp) in enumerate(passes):
            if pi + 1 < len(passes):
                loadw(pi + 1)
            wgt, wut, wdt = wset[pi % 2]
            sfx = str(pi % 2)
            final = (pi == len(passes) - 1)
            for gi, (g0, N) in enumerate(glist):
                ft = ftL[gq % 2]
                fk = 'ft%d' % (gq % 2)
                if gq + 1 < len(gseq):
                    load_ft(gq + 1)
                gq += 1
                for c_ in range(CH):
                    q_ = c_ % 2
                    cs_ = slice(c_ * 128, (c_ + 1) * 128)
                    mmg(psf[q_][:, 0:N], [(wgt[:, kk, cs_], ft[:, kk, 0:N]) for kk in range(8)], ['wgt' + sfx, fk], [PK[q_]])
                    mmg(psf[2 + q_][:, 0:N], [(wut[:, kk, cs_], ft[:, kk, 0:N]) for kk in range(8)], ['wut' + sfx, fk], [PK[2 + q_]])
                    act(sl[q_][:, 0:N], psf[q_][:, 0:N], AF.Silu, [PK[q_]], ['sl%d' % q_])
                    tt('dve', actT[:, c_, 0:N], psf[2 + q_][:, 0:N], sl[q_][:, 0:N], ALU.mult, [PK[2 + q_], 'sl%d' % q_], ['actT'])
                for j in range(N // 128):
                    t = g0 // 128 + j
                    h_ = si % 2
                    if si + 1 < len(seq):
                        load_ht(si + 1)
                    si += 1
                    js = slice(j * 128, (j + 1) * 128)
                    for half in range(2):
                        hs = slice(half * 512, (half + 1) * 512)
                        mmg(psf[4 + half][:, :], [(actT[:, c_, js], wdt[:, c_, hs]) for c_ in range(CH)], ['actT', 'wdt' + sfx], [PK[4 + half]])
                        stt(tq[:, hs], psf[4 + half][:, :], wts[:, t, e_:e_ + 1], gate0[:, hs], ALU.mult, ALU.mult,
                            [PK[4 + half], 'gate0', 'wts'], ['tq'])
                        tt('pool', ht[h_][:, hs], ht[h_][:, hs], tq[:, hs], ALU.add, ['ht%d' % h_, 'tq'], ['ht%d' % h_])
                    if final:
                        dma('sp', out[t * 128:(t + 1) * 128, :], ht[h_], ['ht%d' % h_], ['out'])
                    else:
                        dma('sp', hq[t * 128:(t + 1) * 128, :], ht[h_], ['ht%d' % h_], ['hb%d' % t])
        P.barrier()

    P.barrier()
    maxstage = int(os.environ.get('KSTAGES', '999'))
    sc = {'n': 0}

    def S(fn, *a, **kw):
        if sc['n'] < maxstage:
            fn(*a, **kw)
        sc['n'] += 1

    for i in range(2):
        with_ctx = (i == 0)
        S(stage_mod, i)
        print("ops after mod", P.nadd)
        S(stage_proj, i)
        print("ops after proj", P.nadd)
        if i == 0:
            S(stage_attn, i, with_ctx)
            S(stage_fnet, i, with_ctx)
            S(stage_ret, i, with_ctx)
            S(stage_conv, i, with_ctx)
            S(stage_merge, i, with_ctx)
        else:
            S(stage_compact, i, [(qtm, 512, [(qTq, 0, 0, 4)]), (atm, 1024, [(aTq1, 0, 0, 8)])])
            S(stage_attn, i, with_ctx, own=True)
            S(stage_fnet, i, with_ctx, tr=False)
            S(stage_ret, i, with_ctx)
            S(stage_conv, i, with_ctx)
            S(stage_fm2tm, i)
            S(stage_compact, i, [(ytm, 256, [(brTq, 0, 0, 2)]), (rctm, 512, [(brTq, 256, 0, 2), (brTq, 1024, 2, 2)])])
            S(stage_merge, i, with_ctx, own=True)
        if i == 0:
            S(stage_ffn_norm, i, with_ctx, moe=False)
            for hp in range(HP):
                S(stage_ffn_pass, i, with_ctx, ffn_wg[0], ffn_wu[0], ffn_wd[0], hp, None, False)
        else:
            S(stage_ffn_norm, i, with_ctx, moe=True)
            S(stage_moe, i)
    P.add('sp', None, ['out'], [])
    P.emit()
    P.close()
    return nc


def host_inputs(inp, L, b):
    f = lambda a: np.ascontiguousarray(np.asarray(a), dtype=np.float32)
    cf, cb, rope = make_consts(L)
    c = f(inp["c"])[b]; cc = f(inp["c_ctx"])
    ccols = np.concatenate([c.reshape(8, 128).T, cc.reshape(8, 128).T], 1)
    dec2 = np.concatenate([f(inp["ret_decay_fwd"]), f(inp["ret_decay_bwd"])], 1)
    rng = f(inp["ret_norm_g"]).reshape(2, 4, 64).transpose(0, 2, 1)
    dwa = f(inp["conv_dw_w"]).reshape(2, 31, 2, 128).transpose(0, 3, 2, 1).reshape(2, 128, 62)
    cba = np.stack([f(inp["conv_dw_b"]), f(inp["conv_ln_g"]), f(inp["conv_ln_b"])], -1).reshape(2, 2, 128, 3).transpose(0, 2, 1, 3).reshape(2, 128, 6)
    gqk = np.concatenate([np.tile(f(inp["attn_qn_g"]), (1, 8)), np.tile(f(inp["attn_kn_g"]), (1, 2))], 1)
    rwT = f(inp["router_w"])[0].T
    d = {"x": f(inp["x"])[b], "ctx": f(inp["ctx"])[b], "ccols": ccols, "dec2": dec2, "rng": rng, "dwa": dwa, "cba": cba,
         "gqk": gqk, "rwT": rwT, "cf": cf, "cb": cb, "rope": rope}
    for k in ["ada_w", "ada_b", "norm1_g", "norm2_g", "w_in", "fnet_w", "ret_w", "attn_w", "conv_w_out", "w_out",
              "ffn_w_gate", "ffn_w_up", "ffn_w_down", "moe_w_gate", "moe_w_up", "moe_w_down"]:
        d[k] = f(inp[k])
    return {k: np.ascontiguousarray(v, dtype=np.float32) for k, v in d.items()}


def run(inputs, debug=False):
    L = int(np.asarray(inputs["x"]).shape[1])
    FD = int(np.asarray(inputs["ffn_w_gate"]).shape[2])
    NQ = 4
    LQ = L // NQ
    nc = build(L, FD // 128, debug=debug, NQ=NQ)
    base = [host_inputs(inputs, L, b) for b in range(2)]
    in_maps = []
    for cid in range(2 * NQ):
        b, q = cid // NQ, cid % NQ
        d = dict(base[b])
        jj = np.arange(LQ // 128)[None, :]
        pp = np.arange(128)[:, None]
        d["own_idx"] = np.ascontiguousarray((256 + q * LQ + jj * 128 + pp).astype(np.uint32))
        in_maps.append(d)
    res = run_bass_kernel_spmd(nc, in_maps, core_ids=list(range(2 * NQ)))
    outp = np.zeros((2, L, 1024), np.float32)
    for cid in range(2 * NQ):
        b, q = cid // NQ, cid % NQ
        outp[b, q * LQ:(q + 1) * LQ] = np.asarray(res.results[cid]["out"], dtype=np.float32)
    if debug:
        return outp, res.results
    return outp


def kernel(**inputs):
    return run(inputs)
```

```python
from contextlib import ExitStack
import os
import numpy as np
import concourse.bass as bass
import concourse.mybir as mybir
from concourse.bass_utils import run_bass_kernel_spmd

F32 = mybir.dt.float32
BF16 = mybir.dt.bfloat16
U32 = mybir.dt.uint32
ALU = mybir.AluOpType
AF = mybir.ActivationFunctionType
AX = mybir.AxisListType

ENGS = ['pe', 'act', 'dve', 'pool', 'sp']
NS_DMA = 8
SAME_ENG_GAP = 3
EPS = 1e-6


class Op:
    __slots__ = ('eng', 'fn', 'idx', 'waits', 'signal', 'sigval', 'dma', 'dsem', 'dval', 'dpre', 'ndma')


class Prog:
    def __init__(self, nc):
        self.nc = nc
        self.ops = {e: [] for e in ENGS}
        self.last_w = {}
        self.readers = {}
        self.known = {e: {f: -1 for f in ENGS} for e in ENGS}
        self.known_dma = {e: set() for e in ENGS}
        self.dma_count = {e: 0 for e in ENGS}
        self.dma_semtot = {e: [0] * NS_DMA for e in ENGS}
        self.es = ExitStack()
        self.sems = {}
        self.dsems = {}
        self.bar = None

    def sb(self, name, shape, dt):
        return self.es.enter_context(self.nc.sbuf_tensor(name, list(shape), dt))

    def ps(self, name, shape, dt):
        return self.es.enter_context(self.nc.psum_tensor(name, list(shape), dt))

    def add(self, eng, fn, reads=(), writes=(), dma=False, ndma=1, force=False):
        self.nadd = getattr(self, 'nadd', 0) + 1
        if self.nadd > int(os.environ.get('KOPS', '100000000')) and not force:
            return None
        op = Op()
        op.eng = eng; op.fn = fn; op.dma = dma; op.signal = False; op.sigval = None; op.ndma = ndma
        lst = self.ops[eng]
        op.idx = len(lst)
        deps = []
        pr = [r for r in reads if r.startswith('ps')]
        if pr:
            reads = [r for r in reads if not r.startswith('ps')]
            writes = list(writes) + [r for r in pr if r not in writes]
        if self.bar is not None:
            deps.append(self.bar)
        for r in reads:
            w = self.last_w.get(r)
            if w is not None:
                deps.append(w)
        for w_ in writes:
            w = self.last_w.get(w_)
            if w is not None:
                deps.append(w)
            deps.extend(self.readers.get(w_, ()))
        waits = []
        best = {}
        for d in deps:
            if d is op:
                continue
            if d.dma:
                if id(d) in self.known_dma[eng]:
                    continue
                self.known_dma[eng].add(id(d))
                waits.append(d)
            else:
                if d.idx <= self.known[eng][d.eng]:
                    continue
                if d.eng == eng and not force:
                    if eng == 'pe' or eng == 'sp':
                        continue
                    if op.idx - d.idx >= SAME_ENG_GAP and not dma and eng != 'pool':
                        continue
                b = best.get(d.eng)
                if b is None or d.idx > b.idx:
                    best[d.eng] = d
        for f, d in best.items():
            self.known[eng][f] = d.idx
            d.signal = True
            waits.append(d)
        op.waits = waits
        if dma:
            i = self.dma_count[eng]
            self.dma_count[eng] += 1
            s = i % NS_DMA
            op.dsem = s
            op.dpre = self.dma_semtot[eng][s]
            self.dma_semtot[eng][s] += 16 * ndma
            op.dval = self.dma_semtot[eng][s]
        for r in reads:
            self.readers.setdefault(r, []).append(op)
        for w_ in writes:
            self.last_w[w_] = op
            self.readers[w_] = []
        lst.append(op)
        return op

    def barrier(self):
        keys = set(self.last_w.keys()) | set(self.readers.keys())
        keys = list(keys)
        last = None
        for e in ENGS:
            own = self.ops[e][-1] if self.ops[e] else None
            last = self.add(e, (lambda en: en.nop()), reads=(), writes=keys, force=True)
            if own is not None and not own.dma and own.fn is not None and own not in last.waits and own.idx > self.known[e][e]:
                own.signal = True
                last.waits.append(own)
                self.known[e][e] = own.idx
        self.bar = last
        self.last_w = {}
        self.readers = {}

    def emit(self):
        nc = self.nc
        for e in ENGS:
            self.sems[e] = self.es.enter_context(nc.semaphore('s_' + e))
            self.dsems[e] = [self.es.enter_context(nc.semaphore('d_%s%d' % (e, i))) for i in range(NS_DMA)]
        for e in ENGS:
            c = 0
            for op in self.ops[e]:
                if op.signal and not op.dma:
                    c += 1
                    op.sigval = c
        block = self.es.enter_context(nc.Block())
        prog = self

        def run(ename, eng):
            for op in prog.ops[ename]:
                for d in op.waits:
                    if d.dma:
                        eng.wait_ge(prog.dsems[d.eng][d.dsem], d.dval)
                    else:
                        eng.wait_ge(prog.sems[d.eng], d.sigval)
                if op.fn is None:
                    continue
                if op.dma:
                    sem = prog.dsems[ename][op.dsem]
                    if op.dpre > 0:
                        eng.wait_ge(sem, op.dpre)
                    r = op.fn(eng)
                    if not isinstance(r, (list, tuple)):
                        r = [r]
                    assert len(r) == op.ndma
                    for ins in r:
                        ins.then_inc(sem, 16)
                else:
                    r = op.fn(eng)
                    if op.signal:
                        if isinstance(r, (list, tuple)):
                            r = r[-1]
                        r.then_inc(prog.sems[ename], 1)

        @block.tensor
        def _(eng):
            run('pe', eng)

        @block.scalar
        def _(eng):
            run('act', eng)

        @block.vector
        def _(eng):
            run('dve', eng)

        @block.gpsimd
        def _(eng):
            run('pool', eng)

        @block.sync
        def _(eng):
            run('sp', eng)

    def close(self):
        self.es.close()


CF_COLS = {}


def _layout_cf(L0):
    names = [('identF', 128), ('DIFFP', 128), ('DIFFN', 128), ('MLT', 128), ('MGT', 128), ('I2', 128),
             ('IP1', 128), ('IB', 128), ('P127', 1), ('PJ', 1), ('C128T', 64), ('on128', 128), ('on64', 64),
             ('twc', L0), ('tws', L0)]
    off = 0
    d = {}
    for n, w in names:
        d[n] = (off, w)
        off += w
    return d, off


def _layout_cb(L0):
    names = [('ident', 128), ('c128', 128), ('s128', 128), ('c64', L0), ('s64', L0), ('bd1', 1024), ('bd2', 1024),
             ('cc', 512), ('sc', 512)]
    off = 0
    d = {}
    for n, w in names:
        d[n] = (off, w)
        off += w
    return d, off


def make_consts(L):
    L0 = L // 128
    T = 256 + L
    cfl, ncf = _layout_cf(L0)
    cbl, ncb = _layout_cb(L0)
    cf = np.zeros((128, ncf), np.float32)
    cb = np.zeros((128, ncb), np.float32)

    def setf(n, a):
        o, w = cfl[n]
        cf[:a.shape[0], o:o + w] = a

    def setb(n, a):
        o, w = cbl[n]
        cb[:a.shape[0], o:o + w] = a

    p = np.arange(128)
    j = p[:, None].astype(np.float64)
    i = p[None, :].astype(np.float64)
    setf('identF', np.eye(128))
    setf('DIFFP', np.maximum(i - j, 0))
    setf('DIFFN', np.maximum(j - i, 0))
    setf('MLT', (j < i).astype(np.float64))
    setf('MGT', (j > i).astype(np.float64))
    setf('I2', 2 * np.eye(128))
    setf('IP1', np.broadcast_to(i + 1, (64, 128)))
    setf('IB', np.broadcast_to(128 - i, (64, 128)))
    setf('P127', 127 - j)
    setf('PJ', j)
    setf('C128T', np.full((64, 64), 128.0))
    setf('on128', np.full((128, 128), 1.0 / 256))
    setf('on64', np.full((64, 64), 1.0 / 64))
    l0 = np.arange(L0)[None, :].astype(np.float64)
    setf('twc', np.cos(2 * np.pi * j * l0 / L))
    setf('tws', np.sin(2 * np.pi * j * l0 / L))
    setb('ident', np.eye(128))
    setb('c128', np.cos(2 * np.pi * j * i / 128))
    setb('s128', np.sin(2 * np.pi * j * i / 128))
    a0 = np.arange(L0)[:, None].astype(np.float64)
    b0 = np.arange(L0)[None, :].astype(np.float64)
    setb('c64', np.cos(2 * np.pi * a0 * b0 / L0) / np.sqrt(L))
    setb('s64', np.sin(2 * np.pi * a0 * b0 / L0) / np.sqrt(L))
    c = np.arange(64)[:, None].astype(np.float64)
    jj = np.arange(64)[None, :].astype(np.float64)
    C64 = np.cos(2 * np.pi * c * jj / 64) / 8.0
    S64 = np.sin(2 * np.pi * c * jj / 64) / 8.0
    BDC = np.zeros((256, 256)); BDS = np.zeros((256, 256))
    for g in range(4):
        BDC[g * 64:(g + 1) * 64, g * 64:(g + 1) * 64] = C64
        BDS[g * 64:(g + 1) * 64, g * 64:(g + 1) * 64] = S64
    bd1 = np.concatenate([BDC, -BDS], 1)
    bd2 = np.concatenate([-BDS, -BDC], 1)
    setb('bd1', bd1.reshape(2, 128, 512).transpose(1, 0, 2).reshape(128, 1024))
    setb('bd2', bd2.reshape(2, 128, 512).transpose(1, 0, 2).reshape(128, 1024))
    lc = np.arange(256)[:, None].astype(np.float64)
    kc = np.arange(256)[None, :].astype(np.float64)
    CC = np.cos(2 * np.pi * lc * kc / 256) / 16.0
    SC = np.sin(2 * np.pi * lc * kc / 256) / 16.0
    setb('cc', CC.reshape(2, 128, 256).transpose(1, 0, 2).reshape(128, 512))
    setb('sc', SC.reshape(2, 128, 256).transpose(1, 0, 2).reshape(128, 512))
    rows = L // 64
    row = np.repeat(np.arange(rows), 64).astype(np.float32)
    col = np.tile(np.arange(64), rows).astype(np.float32)
    inv = (np.float32(10000.0) ** (-np.arange(16, dtype=np.float32) / np.float32(16))).astype(np.float32)
    ang = np.concatenate([row[:, None] * inv, col[:, None] * inv], -1).astype(np.float32)
    rope = np.zeros((T, 64), np.float32)
    rope[:256, :32] = 1.0
    rope[256:, :32] = np.cos(ang)
    rope[256:, 32:] = np.sin(ang)
    return cf, cb, rope


def build(L, FFC, debug=False, NQ=4):
    C = 256
    LQ = L // NQ
    T = C + L
    NT = T // 128
    L0 = L // 128
    HP = 2 if FFC % 2 == 0 else 1
    CH = FFC // HP
    FD = FFC * 128
    NE = 8
    cfl, ncf = _layout_cf(L0)
    cbl, ncb = _layout_cb(L0)
    nc = bass.Bass("TRN2", target_bir_lowering=False)
    P = Prog(nc)

    def din(name, shape, dt=F32):
        return nc.dram_tensor(name, list(shape), dt, kind="ExternalInput").ap()

    def dscr(name, shape, dt):
        return nc.dram_tensor(name, list(shape), dt, kind=("ExternalOutput" if debug else "Internal")).ap()

    x = din("x", [L, 1024]); ctx = din("ctx", [C, 1024]); ccols = din("ccols", [128, 16])
    ada_w = din("ada_w", [2, 1024, 6144]); ada_b = din("ada_b", [2, 6144])
    n1g = din("norm1_g", [2, 1024]); n2g = din("norm2_g", [2, 1024])
    w_in = din("w_in", [2, 1024, 6656])
    fnet_w = din("fnet_w", [2, 256, 1024]); ret_w = din("ret_w", [2, 256, 1024]); attn_w = din("attn_w", [2, 512, 1024])
    conv_wo = din("conv_w_out", [2, 256, 1024]); w_out = din("w_out", [2, 1024, 1024])
    ffn_wg = din("ffn_w_gate", [1, 1024, FD]); ffn_wu = din("ffn_w_up", [1, 1024, FD]); ffn_wd = din("ffn_w_down", [1, FD, 1024])
    moe_wg = din("moe_w_gate", [1, NE, 1024, FD]); moe_wu = din("moe_w_up", [1, NE, 1024, FD]); moe_wd = din("moe_w_down", [1, NE, FD, 1024])
    dec2 = din("dec2", [2, 8]); rng = din("rng", [2, 64, 4]); dwa = din("dwa", [2, 128, 62]); cba = din("cba", [2, 128, 6])
    gqk_in = din("gqk", [2, 640]); rwT = din("rwT", [NE, 1024])
    cf_in = din("cf", [128, ncf]); cb_in = din("cb", [128, ncb]); rope_in = din("rope", [T, 64])
    own_idx = nc.dram_tensor("own_idx", [128, LQ // 128], U32, kind="ExternalInput").ap()
    out = nc.dram_tensor("out", [LQ, 1024], F32, kind="ExternalOutput").ap()
    hq = dscr("hq", [LQ, 1024], F32)
    qtm = dscr("qtm", [T, 512], BF16)
    atm = dscr("atm", [T, 1024], BF16)
    rctm = dscr("rctm", [T, 512], BF16)
    qTq = dscr("qTq", [512, LQ], BF16)
    aTq1 = dscr("aTq1", [1024, LQ], BF16)
    brTq = dscr("brTq", [1280, LQ], BF16)
    qgroups = [(k_ * 512, 512) for k_ in range(LQ // 512)]
    aTq = dscr("aTq", [1024, LQ], BF16)

    hb = dscr("hb", [T, 1024], F32)
    aT = dscr("aT", [1024, T], BF16)
    modr = dscr("modr", [2, 128, 6144], F32)
    qkT = dscr("qkT", [1152, T], BF16)
    tmo = dscr("tmo", [T, 640], BF16)
    ab1 = dscr("ab1", [T, 512], BF16); ab2 = dscr("ab2", [T, 512], BF16)
    rgT = dscr("rgT", [256, T], BF16); cyT = dscr("cyT", [256, T], BF16)
    zs = dscr("zs", [128, L0, 512], BF16)
    ytm = dscr("ytm", [T, 256], BF16)
    brT = dscr("brT", [1280, T], BF16)

    groups = [(0, 256)] + [(256 + 512 * i_, 512) for i_ in range(L // 512)]

    cf = P.sb("cf_sb", [128, ncf], F32)
    cb = P.sb("cb_sb", [128, ncb], BF16)
    wts = P.sb("wts_sb", [128, NT, 8], F32)
    ARW = 46600
    arena = P.sb("arena", [128, ARW], F32)
    pw = [P.ps("pw%d" % i_, [128, 1024], F32) for i_ in range(4)]
    psf = [pw[i_ // 2][:, (i_ % 2) * 512:(i_ % 2 + 1) * 512] for i_ in range(8)]
    psb = pw[3][:, 512:1024].bitcast(BF16)
    PK = ['ps%d' % i_ for i_ in range(8)]
    st = {'off': 0, 'n': 0}

    def CF(n, parts=128):
        o, w = cfl[n]
        return cf[0:parts, o:o + w]

    def CB(n, parts=128):
        o, w = cbl[n]
        return cb[0:parts, o:o + w]

    def areset():
        st['off'] = 0

    def alloc(shape, dt, name=None):
        n = 1
        for s in shape[1:]:
            n *= s
        words = n if dt in (F32, U32) else (n + 1) // 2
        words = (words + 7) // 8 * 8
        assert st['off'] + words <= ARW, ("arena overflow", st['off'], words)
        v = arena[0:shape[0], st['off']:st['off'] + words]
        st['off'] += words
        if dt != F32:
            v = v.bitcast(dt)
        v = v[:, 0:n]
        if len(shape) == 3:
            v = v.rearrange("p (a b) -> p a b", a=shape[1])
        elif len(shape) == 4:
            v = v.rearrange("p (a b c) -> p a b c", a=shape[1], b=shape[2])
        st['n'] += 1
        return v

    def dma(q, o, i, r, w):
        P.add(q, (lambda e, o=o, i=i: e.dma_start(out=o, in_=i)), r, w, dma=True)

    def mmg(o, pairs, r, w, tr=False):
        def fn(e, o=o, pairs=pairs):
            n = len(pairs)
            ins = None
            for ii, (l, rh) in enumerate(pairs):
                ins = e.matmul(o, lhsT=l, rhs=rh, start=(ii == 0), stop=(ii == n - 1))
            return ins
        P.add('pe', fn, r, w)

    def mms(items, r, w):
        def fn(e, items=items):
            ins = None
            for o, pairs in items:
                n = len(pairs)
                for ii, (l, rh) in enumerate(pairs):
                    ins = e.matmul(o, lhsT=l, rhs=rh, start=(ii == 0), stop=(ii == n - 1))
            return ins
        P.add('pe', fn, r, w)

    def trs(items, r, w):
        def fn(e, items=items):
            ins = None
            for o, i in items:
                ins = e.transpose(o, i, CB('ident'))
            return ins
        P.add('pe', fn, r, w)

    def act(o, i, func, r, w, **kw):
        P.add('act', (lambda e, o=o, i=i, func=func, kw=kw: e.activation(out=o, in_=i, func=func, **kw)), r, w)

    def tt(eng, o, a, b, op, r, w):
        P.add(eng, (lambda e, o=o, a=a, b=b, op=op: e.tensor_tensor(out=o, in0=a, in1=b, op=op)), r, w)

    def ts(eng, o, a, s1, s2, op0, op1, r, w):
        if s2 is None:
            P.add(eng, (lambda e, o=o, a=a, s1=s1, op0=op0: e.tensor_scalar(out=o, in0=a, scalar1=s1, scalar2=None, op0=op0)), r, w)
        else:
            P.add(eng, (lambda e, o=o, a=a, s1=s1, s2=s2, op0=op0, op1=op1: e.tensor_scalar(out=o, in0=a, scalar1=s1, scalar2=s2, op0=op0, op1=op1)), r, w)

    def stt(o, a, s, b, op0, op1, r, w):
        P.add('dve', (lambda e, o=o, a=a, s=s, b=b, op0=op0, op1=op1: e.scalar_tensor_tensor(out=o, in0=a, scalar=s, in1=b, op0=op0, op1=op1)), r, w)

    def cp(eng, o, i, r, w):
        if eng == 'act':
            act(o, i, AF.Copy, r, w)
        else:
            P.add(eng, (lambda e, o=o, i=i: e.tensor_copy(out=o, in_=i)), r, w)

    def recip(o, i, r, w):
        P.add('dve', (lambda e, o=o, i=i: e.reciprocal(out=o, in_=i)), r, w)

    def red(o, i, op, r, w):
        P.add('dve', (lambda e, o=o, i=i, op=op: e.tensor_reduce(out=o, in_=i, axis=AX.X, op=op)), r, w)

    def mset(eng, o, v, w):
        P.add(eng, (lambda e, o=o, v=v: e.memset(o, v)), (), w)

    dma('sp', cf[:], cf_in, [], ['cf'])
    dma('pool', cb[:], cb_in, [], ['cb'])
    CK = ['cf', 'cb']

    def hsrc(layer0, t):
        if layer0:
            return ctx[t * 128:(t + 1) * 128, :] if t < 2 else x[(t - 2) * 128:(t - 1) * 128, :]
        return hb[t * 128:(t + 1) * 128, :]

    def stage_mod(i):
        areset()
        cc_t = alloc([128, 16], F32); scol = alloc([128, 16], F32); srep = alloc([128, 16, 128], F32)
        adw = [alloc([128, 8, 512], F32) for _ in range(2)]
        adb = [alloc([128, 512], F32) for _ in range(2)]
        mo = [alloc([128, 512], F32) for _ in range(2)]
        dma('sp', cc_t, ccols, [], ['cc_t'])
        act(scol, cc_t, AF.Silu, ['cc_t'], ['scol'])
        cp('dve', srep, scol.unsqueeze(2).to_broadcast([128, 16, 128]), ['scol'], ['srep'])
        for cg in range(12):
            r_ = cg % 2
            cs_ = slice(cg * 512, (cg + 1) * 512)
            dma('sp', adw[r_], ada_w[i, :, cs_].rearrange("(k p) n -> p k n", p=128), [], ['adw%d' % r_])
            dma('sp', adb[r_], ada_b[i, cs_].partition_broadcast(128), [], ['adb%d' % r_])
            for wh in range(2):
                mmg(psf[wh][:, :], [(srep[:, wh * 8 + k, :], adw[r_][:, k, :]) for k in range(8)],
                    ['srep', 'adw%d' % r_], [PK[wh]])
                tt('dve', mo[wh], psf[wh][:, :], adb[r_], ALU.add, [PK[wh], 'adb%d' % r_], ['mo%d' % wh])
                dma('sp', modr[wh, :, cs_], mo[wh], ['mo%d' % wh], ['modr'])
        P.barrier()

    def norm_mod_tile(t, wh, ht, ss, a1, a2, gs, sh, junk, ktag, a2f=None):
        gk = 'gs_l%d' % wh
        sk = 'sh_l%d' % wh
        act(junk, ht, AF.Square, [ktag + 'ht'], ['junk', ktag + 'ss'], accum_out=ss)
        act(ss, ss, AF.Sqrt, [ktag + 'ss'], [ktag + 'ss'], scale=1.0 / 1024, bias=EPS)
        recip(ss, ss, [ktag + 'ss'], [ktag + 'ss'])
        stt(a1, ht, ss[:, 0:1], gs[wh], ALU.mult, ALU.mult, [ktag + 'ht', ktag + 'ss', gk], ['a1'])
        if a2f is not None:
            tt('dve', a2f, a1, sh[wh], ALU.add, ['a1', sk], ['a2f'])
            cp('pool', a2, a2f, ['a2f'], ['a2'])
        else:
            tt('pool', a2, a1, sh[wh], ALU.add, ['a1', sk], ['a2'])

    def load_mod(i, gvec, shift_col, scale_col):
        gs = [alloc([128, 1024], F32) for _ in range(2)]
        sh = [alloc([128, 1024], F32) for _ in range(2)]
        grep = alloc([128, 1024], F32)
        dma('sp', grep, gvec[i].partition_broadcast(128), [], ['grep'])
        for wh in range(2):
            dma('sp', sh[wh], modr[wh, :, shift_col * 1024:(shift_col + 1) * 1024], [], ['sh_l%d' % wh])
            dma('sp', gs[wh], modr[wh, :, scale_col * 1024:(scale_col + 1) * 1024], [], ['gs_l%d' % wh])
            stt(gs[wh], gs[wh], 1.0, grep, ALU.add, ALU.mult, ['gs_l%d' % wh, 'grep'], ['gs_l%d' % wh])
        return gs, sh

    def stage_proj(i):
        layer0 = (i == 0)
        areset()
        wi = alloc([128, 8, 2560], BF16)
        dma('pool', wi, w_in[i, :, 0:2560].rearrange("(k p) n -> p k n", p=128), [], ['wi'])
        gs, sh = load_mod(i, n1g, 0, 1)
        gq = alloc([128, 640], F32)
        dma('sp', gq, gqk_in[i].partition_broadcast(128), [], ['gq'])
        ht = [alloc([128, 1024], F32) for _ in range(2)]
        ss = [alloc([128, 1], F32) for _ in range(2)]
        junk = alloc([128, 1024], BF16)
        a1 = alloc([128, 1024], F32); a2 = alloc([128, 1024], BF16)
        agrp = [alloc([128, 8, 512], BF16) for _ in range(2)]
        qkg = alloc([128, 9, 512], BF16)
        xq = alloc([128, 1152], F32); sq = alloc([128, 640], F32); ssq = alloc([128, 10], F32)
        xr = alloc([128, 18, 64], BF16)
        t1 = alloc([128, 18, 32], F32); t2 = alloc([128, 18, 32], F32); t3 = alloc([128, 18, 32], F32); t4 = alloc([128, 18, 32], F32)
        tmt = alloc([128, 640], BF16)
        cs_t = [alloc([128, 64], F32) for _ in range(2)]
        fnT = alloc([128, 2, 512], BF16); rgt = alloc([128, 2, 512], BF16); cy = alloc([128, 2, 512], BF16)
        sg = alloc([128, 512], F32)
        abt1 = alloc([128, 4, 512], BF16); abt2 = alloc([128, 4, 512], BF16)
        bd1 = CB('bd1').rearrange("p (a b) -> p a b", a=2); bd2 = CB('bd2').rearrange("p (a b) -> p a b", a=2)
        x3 = xq.rearrange("p (h d) -> p h d", h=18)
        xrf = xr.rearrange("p h d -> p (h d)")
        tix = 0
        ptiles = [g0 // 128 + j for (g0, N) in groups for j in range(N // 128)]

        def pload(k):
            t_ = ptiles[k]
            dma('sp', ht[k % 2], hsrc(layer0, t_), [], ['r%dht' % (k % 2)])
            dma('sp', cs_t[k % 2], rope_in[t_ * 128:(t_ + 1) * 128, :], [], ['r%dcs' % (k % 2)])

        for gi, (g0, N) in enumerate(groups):
            gb = gi % 2
            ag = agrp[gb]
            AK = 'ag%d' % gb
            wh = 1 if gi == 0 else 0
            nt = N // 128
            for j in range(nt):
                t = g0 // 128 + j
                r_ = tix % 2
                tix += 1
                k_ = 'r%d' % r_
                js = slice(j * 128, (j + 1) * 128)
                if tix == 1:
                    pload(0)
                if tix < len(ptiles):
                    pload(tix)
                norm_mod_tile(t, wh, ht[r_], ss[r_], a1, a2, gs, sh, junk, k_)
                if not layer0:
                    dma('sp', atm[t * 128:(t + 1) * 128, :], a2, ['a2'], ['atm'])
                trs([(psb[:, k * 128:(k + 1) * 128], a2[:, k * 128:(k + 1) * 128]) for k in range(8)], ['a2', 'cb'], ['ps7'])
                cp('act', ag[:, :, js], psb[:, :].rearrange("p (k t) -> p k t", k=8), ['ps7'], [AK])
                lhs = [ag[:, k, js] for k in range(8)]
                mms([(psf[0][:, 0:512], [(lhs[k], wi[:, k, 1280:1792]) for k in range(8)]),
                     (psf[1][:, 0:128], [(lhs[k], wi[:, k, 1792:1920]) for k in range(8)]),
                     (psf[1][:, 128:384], [(lhs[k], wi[:, k, 256:512]) for k in range(8)]),
                     (psf[2][:, 0:256], [(lhs[k], wi[:, k, 512:768]) for k in range(8)]),
                     (psf[2][:, 256:384], [(lhs[k], wi[:, k, 1920:2048]) for k in range(8)]),
                     (psf[3][:, 0:256], [(lhs[k], wi[:, k, 768:1024]) for k in range(8)])],
                    [AK, 'wi'], [PK[0], PK[1], PK[2], PK[3]])
                cp('act', xq[:, 0:512], psf[0][:, 0:512], [PK[0]], ['xq_a'])
                cp('dve', xq[:, 512:896], psf[1][:, 0:384], [PK[1]], ['xq_b'])
                act(xq[:, 896:1152], psf[2][:, 0:256], AF.Copy, [PK[2]], ['xq_c'], scale=0.125)
                cp('dve', tmt[:, 0:128], psf[2][:, 256:384], [PK[2]], ['tmt_a'])
                cp('act', tmt[:, 128:384], psf[3][:, 0:256], [PK[3]], ['tmt_b'])
                tt('dve', sq, xq[:, 0:640], xq[:, 0:640], ALU.mult, ['xq_a', 'xq_b'], ['sq'])
                red(ssq, sq.rearrange("p (h d) -> p h d", h=10), ALU.add, ['sq'], ['ssq'])
                act(ssq, ssq, AF.Sqrt, ['ssq'], ['ssq'], scale=1.0 / 64, bias=EPS)
                recip(ssq, ssq, ['ssq'], ['ssq'])
                tt('dve', x3[:, 0:10, :], x3[:, 0:10, :], ssq.unsqueeze(2).to_broadcast([128, 10, 64]), ALU.mult,
                   ['xq_a', 'xq_b', 'ssq'], ['xq_a', 'xq_b'])
                tt('pool', xq[:, 0:640], xq[:, 0:640], gq, ALU.mult, ['xq_a', 'xq_b', 'gq'], ['xq_a', 'xq_b'])
                cb_ = cs_t[r_][:, 0:32].unsqueeze(1).to_broadcast([128, 18, 32])
                sb_ = cs_t[r_][:, 32:64].unsqueeze(1).to_broadcast([128, 18, 32])
                XQ = ['xq_a', 'xq_b', 'xq_c']
                tt('dve', t1, x3[:, :, 0:32], cb_, ALU.mult, XQ + [k_ + 'cs'], ['t1'])
                tt('pool', t2, x3[:, :, 32:64], sb_, ALU.mult, XQ + [k_ + 'cs'], ['t2'])
                tt('dve', xr[:, :, 0:32], t1, t2, ALU.subtract, ['t1', 't2'], ['xr_a'])
                tt('pool', t3, x3[:, :, 0:32], sb_, ALU.mult, XQ + [k_ + 'cs'], ['t3'])
                tt('dve', t4, x3[:, :, 32:64], cb_, ALU.mult, XQ + [k_ + 'cs'], ['t4'])
                tt('pool', xr[:, :, 32:64], t3, t4, ALU.add, ['t3', 't4'], ['xr_b'])
                if not layer0:
                    dma('sp', qtm[t * 128:(t + 1) * 128, :], xrf[:, 0:512], ['xr_a', 'xr_b'], ['qtm'])
                cp('pool', tmt[:, 384:640], xrf[:, 896:1152], ['xr_a', 'xr_b'], ['tmt_c'])
                dma('sp', tmo[t * 128:(t + 1) * 128, :], tmt, ['tmt_a', 'tmt_b', 'tmt_c'], ['tmo'])
                trs([(psb[:, b * 128:(b + 1) * 128], xrf[:, b * 128:(b + 1) * 128]) for b in range(8)], ['xr_a', 'xr_b', 'cb'], ['ps7'])
                cp('act', qkg[:, 0:8, js], psb[:, :].rearrange("p (k t) -> p k t", k=8), ['ps7'], ['qkg'])
                trs([(psb[:, 0:128], xrf[:, 1024:1152])], ['xr_a', 'xr_b', 'cb'], ['ps7'])
                cp('act', qkg[:, 8, js], psb[:, 0:128], ['ps7'], ['qkg'])
            dma('sp', qkT[:, g0:g0 + N].rearrange("(b p) t -> p b t", p=128), qkg[:, :, 0:N], ['qkg'], ['qkT'])
            dma('sp', aT[:, g0:g0 + N].rearrange("(k p) t -> p k t", p=128), ag[:, :, 0:N], [AK], ['aT'])
            rhs = [ag[:, k, 0:N] for k in range(8)]
            for oc in range(2):
                mmg(psf[4][:, 0:N], [(wi[:, k, oc * 128:(oc + 1) * 128], rhs[k]) for k in range(8)], [AK, 'wi'], [PK[4]])
                cp('act', fnT[:, oc, 0:N], psf[4][:, 0:N], [PK[4]], ['fnT'])
                mmg(psf[5][:, 0:N], [(wi[:, k, 1024 + oc * 128:1024 + (oc + 1) * 128], rhs[k]) for k in range(8)], [AK, 'wi'], [PK[5]])
                act(rgt[:, oc, 0:N], psf[5][:, 0:N], AF.Silu, [PK[5]], ['rgt'])
            dma('sp', rgT[:, g0:g0 + N].rearrange("(c p) t -> p c t", p=128), rgt[:, :, 0:N], ['rgt'], ['rgT'])
            for oc in range(2):
                mmg(psf[4][:, 0:N], [(wi[:, k, 2048 + oc * 128:2048 + (oc + 1) * 128], rhs[k]) for k in range(8)], [AK, 'wi'], [PK[4]])
                mmg(psf[5][:, 0:N], [(wi[:, k, 2304 + oc * 128:2304 + (oc + 1) * 128], rhs[k]) for k in range(8)], [AK, 'wi'], [PK[5]])
                act(sg[:, 0:N], psf[5][:, 0:N], AF.Sigmoid, [PK[5]], ['sg'])
                tt('dve', cy[:, oc, 0:N], psf[4][:, 0:N], sg[:, 0:N], ALU.mult, [PK[4], 'sg'], ['cy'])
            dma('sp', cyT[:, g0:g0 + N].rearrange("(c p) t -> p c t", p=128), cy[:, :, 0:N], ['cy'], ['cyT'])
            for j in range(nt):
                js = slice(j * 128, (j + 1) * 128)
                mmg(psf[6][:, :], [(fnT[:, fc, js], bd1[:, fc, :]) for fc in range(2)], ['fnT', 'cb'], [PK[6]])
                cp('act', abt1[:, j, :], psf[6][:, :], [PK[6]], ['abt1'])
                mmg(psf[6][:, :], [(fnT[:, fc, js], bd2[:, fc, :]) for fc in range(2)], ['fnT', 'cb'], [PK[6]])
                cp('dve', abt2[:, j, :], psf[6][:, :], [PK[6]], ['abt2'])
            dma('sp', ab1[g0:g0 + N, :].rearrange("(j p) f -> p j f", p=128), abt1[:, 0:nt, :], ['abt1'], ['ab1'])
            dma('sp', ab2[g0:g0 + N, :].rearrange("(j p) f -> p j f", p=128), abt2[:, 0:nt, :], ['abt2'], ['ab2'])
        P.barrier()

    def stage_attn(i, with_ctx, own=False):
        areset()
        Qsrc = qTq if own else qkT
        Bdst = brTq if own else brT
        glist = qgroups if own else [g for gi_, g in enumerate(groups) if not (gi_ == 0 and not with_ctx)]
        ka = alloc([128, T], BF16)
        va = alloc([128, NT, 128], BF16)
        qt = alloc([128, 4, 512], BF16)
        mset('pool', ka[64:128, :], 0.0, ['ka_z'])
        mset('pool', qt[64:128, :, :], 0.0, ['qt_z'])
        pt = [alloc([128, 2, 512], BF16) for _ in range(3)]
        rc = alloc([128, 512], F32)
        ot = alloc([64, 4, 512], BF16)
        mset('pool', va[:, :, 64:128], 1.0, ['va1'])
        cnt = 0
        for kv in range(2):
            dma('sp', ka[0:64, :], qkT[512 + kv * 64:512 + (kv + 1) * 64, :], [], ['ka'])
            dma('sp', va[:, :, 0:64], tmo[:, kv * 64:(kv + 1) * 64].rearrange("(n p) d -> p n d", p=128), [], ['va0'])
            for (g0, N) in glist:
                kbs = [0, 1] if (g0 == 0 and not own) else list(range(NT))
                dma('sp', qt[0:64, :, 0:N], Qsrc[kv * 256:(kv + 1) * 256, g0:g0 + N].rearrange("(h d) t -> d h t", d=64), [], ['qt'])
                for pr in range(2):
                    its = list(kbs)
                    base = cnt
                    cnt += len(its)

                    def emit_S(ii, its=its, base=base, N=N, pr=pr):
                        kb = its[ii]
                        r_ = (base + ii) % 3
                        mms([(pw[r_][:, a_ * 512:a_ * 512 + N], [(ka[:, kb * 128:(kb + 1) * 128], qt[:, 2 * pr + a_, 0:N])]) for a_ in range(2)],
                            ['ka', 'qt', 'ka_z', 'qt_z'], [PK[2 * r_], PK[2 * r_ + 1]])
                        act(pt[r_][:, :, 0:N], pw[r_][:, :].rearrange("p (a b) -> p a b", a=2)[:, :, 0:N], AF.Exp,
                            [PK[2 * r_], PK[2 * r_ + 1]], ['pt%d' % r_], scale=0.125)

                    def emit_PV(ii, its=its, base=base, N=N, pr=pr):
                        kb = its[ii]
                        r_ = (base + ii) % 3

                        def fn(e, kb=kb, r_=r_, N=N, s_=(ii == 0), sp_=(ii == len(its) - 1)):
                            ins = None
                            for a_ in range(2):
                                ins = e.matmul(psf[6 + a_][:, 0:N], lhsT=va[:, kb, :], rhs=pt[r_][:, a_, 0:N], start=s_, stop=sp_)
                            return ins
                        P.add('pe', fn, ['va0', 'va1', 'pt%d' % r_], [PK[6], PK[7]])

                    for ii in range(min(2, len(its))):
                        emit_S(ii)
                    for ii in range(len(its)):
                        if ii + 2 < len(its):
                            emit_S(ii + 2)
                        emit_PV(ii)
                    for a_ in range(2):
                        hh = 2 * pr + a_
                        recip(rc[64:128, 0:N], psf[6 + a_][64:128, 0:N], [PK[6 + a_]], ['rc'])
                        tt('dve', ot[:, hh, 0:N], psf[6 + a_][0:64, 0:N], rc[64:128, 0:N], ALU.mult, [PK[6 + a_], 'rc'], ['ot'])
                dma('sp', Bdst[512 + kv * 256:512 + (kv + 1) * 256, g0:g0 + N].rearrange("(h d) t -> d h t", d=64), ot[:, :, 0:N], ['ot'], ['brT'])
        P.barrier()

    def stage_fnet(i, with_ctx, tr=True):
        areset()
        LB = min(16, L0)
        x1b = alloc([128, LB, 512], BF16); x2b = alloc([128, LB, 512], BF16); zb = alloc([128, LB, 512], BF16)
        u1 = alloc([128, 256], F32); u2 = alloc([128, 256], F32)
        z3 = alloc([L0, 16, 512], BF16); ysb = alloc([L0, 16, 256], BF16)
        xc = alloc([128, 2, 512], BF16); yc = alloc([128, 2, 256], BF16)
        yt = [alloc([128, 256], BF16) for _ in range(2)]
        yT = [alloc([128, 2, 128], BF16) for _ in range(2)]
        a1v = ab1[256:256 + L, :].rearrange("(p a) f -> p a f", a=L0)
        a2v = ab2[256:256 + L, :].rearrange("(p a) f -> p a f", a=L0)
        twc = CF('twc'); tws = CF('tws')
        for blk in range(L0 // LB):
            bs = slice(blk * LB, (blk + 1) * LB)
            dma('sp', x1b, a1v[:, bs, :], [], ['x1b'])
            dma('sp', x2b, a2v[:, bs, :], [], ['x2b'])
            for a in range(LB):
                l0 = blk * LB + a
                pz = psf[a % 2]
                k_ = PK[a % 2]
                mmg(pz[:, :], [(CB('c128'), x1b[:, a, :]), (CB('s128'), x2b[:, a, :])], ['x1b', 'x2b', 'cb'], [k_])
                ts('dve', u1, pz[:, 256:512], tws[:, l0:l0 + 1], None, ALU.mult, None, [k_, 'cf'], ['u1'])
                stt(zb[:, a, 0:256], pz[:, 0:256], twc[:, l0:l0 + 1], u1, ALU.mult, ALU.add, [k_, 'u1', 'cf'], ['zb'])
                ts('dve', u2, pz[:, 0:256], tws[:, l0:l0 + 1], None, ALU.mult, None, [k_, 'cf'], ['u2'])
                stt(zb[:, a, 256:512], pz[:, 256:512], twc[:, l0:l0 + 1], u2, ALU.mult, ALU.subtract, [k_, 'u2', 'cf'], ['zb'])
            dma('sp', zs[:, bs, :], zb, ['zb'], ['zs'])
        P.barrier()
        yv = ytm[256:256 + L, :].rearrange("(k0 k1) f -> k0 k1 f", k1=128)
        for blk in range(8):
            bs = slice(blk * 16, (blk + 1) * 16)
            dma('sp', z3, zs[bs, :, :].rearrange("k a f -> a k f"), [], ['z3'])
            for kk in range(16):
                pz = psf[2 + (kk // 2) % 2]
                k_ = PK[2 + (kk // 2) % 2]
                mmg(pz[0:L0, (kk % 2) * 256:(kk % 2 + 1) * 256], [(CB('c64', L0), z3[:, kk, 0:256]), (CB('s64', L0), z3[:, kk, 256:512])],
                    ['z3', 'cb'], [k_])
                if kk % 2 == 1:
                    cp('act', ysb[:, kk - 1:kk + 1, :], pz[0:L0, :].rearrange("p (a b) -> p a b", a=2), [k_], ['ysb'])
            dma('sp', yv[:, bs, :], ysb, ['ysb'], ['ytm'])
        if with_ctx:
            dma('sp', xc, ab1[0:256, :].rearrange("(c p) f -> p c f", p=128), [], ['xc'])
            ccv = CB('cc').rearrange("p (a b) -> p a b", a=2); scv = CB('sc').rearrange("p (a b) -> p a b", a=2)
            for kc in range(2):
                ks = slice(kc * 128, (kc + 1) * 128)
                mmg(psf[4 + kc][:, 0:256], [(ccv[:, lc, ks], xc[:, lc, 0:256]) for lc in range(2)] + [(scv[:, lc, ks], xc[:, lc, 256:512]) for lc in range(2)],
                    ['xc', 'cb'], [PK[4 + kc]])
                cp('act', yc[:, kc, :], psf[4 + kc][:, 0:256], [PK[4 + kc]], ['yc'])
            dma('sp', ytm[0:256, :].rearrange("(c p) f -> p c f", p=128), yc, ['yc'], ['ytm'])
        P.barrier()
        for t in (range(0 if with_ctx else 2, NT) if tr else []):
            r_ = t % 2
            dma('sp', yt[r_], ytm[t * 128:(t + 1) * 128, :], [], ['yt%d' % r_])
            trs([(psb[:, c_ * 128:(c_ + 1) * 128], yt[r_][:, c_ * 128:(c_ + 1) * 128]) for c_ in range(2)], ['yt%d' % r_, 'cb'], ['ps7'])
            cp('act', yT[r_], psb[:, 0:256].rearrange("p (a b) -> p a b", a=2), ['ps7'], ['yT%d' % r_])
            dma('sp', brT[0:256, t * 128:(t + 1) * 128].rearrange("(c p) t -> p c t", p=128), yT[r_], ['yT%d' % r_], ['brT'])
        P.barrier()

    def stage_ret(i, with_ctx):
        areset()
        Sf_all = alloc([64, NT, 4, 64], BF16); Sb_all = alloc([64, NT, 4, 64], BF16)
        lgt = alloc([128, 8], F32); e1 = alloc([128, 8], F32)
        dcomb = alloc([128, 4, 128], BF16); ef = alloc([128, 128], F32); eb = alloc([128, 128], F32)
        xif = alloc([64, 4, 128], BF16); xib = alloc([64, 4, 128], BF16)
        zf = alloc([128, 4], F32); zbk = alloc([128, 4], F32)
        gcf = alloc([64, 4, 64], F32); gcb = alloc([64, 4, 64], F32)
        grn = alloc([64, 4], F32)
        S = alloc([64, 4, 64], F32)
        tmn = [alloc([128, 640], BF16) for _ in range(2)]
        kz = alloc([128, 4, 64], BF16)
        qk = [alloc([64, 8, 128], BF16) for _ in range(2)]
        rgn = [alloc([64, 4, 128], BF16) for _ in range(2)]
        am = alloc([128, 4, 128], BF16); qxf = alloc([64, 4, 128], BF16); qxb = alloc([64, 4, 128], BF16)
        osq = alloc([64, 512], F32); rs = alloc([64, 512], F32); o1 = alloc([64, 512], F32)
        ob = [alloc([64, 4, 128], BF16) for _ in range(2)]
        dma('sp', lgt, dec2[i].partition_broadcast(128), [], ['lgt'])
        dma('sp', grn, rng[i], [], ['grn'])
        act(e1, lgt, AF.Exp, ['lgt'], ['e1'], scale=-1.0)
        act(e1, e1, AF.Ln, ['e1'], ['e1'], bias=1.0)
        ts('dve', lgt, e1, -1.0, None, ALU.mult, None, ['e1'], ['lgt'])
        for h in range(4):
            act(ef, CF('DIFFP'), AF.Exp, ['lgt', 'cf'], ['ef'], scale=lgt[:, h:h + 1])
            tt('dve', ef, ef, CF('MLT'), ALU.mult, ['ef', 'cf'], ['ef'])
            act(eb, CF('DIFFN'), AF.Exp, ['lgt', 'cf'], ['eb'], scale=lgt[:, 4 + h:5 + h])
            tt('dve', eb, eb, CF('MGT'), ALU.mult, ['eb', 'cf'], ['eb'])
            tt('dve', ef, ef, eb, ALU.add, ['ef', 'eb'], ['ef'])
            tt('dve', dcomb[:, h, :], ef, CF('I2'), ALU.add, ['ef', 'cf'], ['dcomb'])
            act(xif[:, h, :], CF('IP1', 64), AF.Exp, ['lgt', 'cf'], ['xif'], scale=lgt[0:64, h:h + 1])
            act(xib[:, h, :], CF('IB', 64), AF.Exp, ['lgt', 'cf'], ['xib'], scale=lgt[0:64, 4 + h:5 + h])
            act(zf[:, h:h + 1], CF('P127'), AF.Exp, ['lgt', 'cf'], ['zf'], scale=lgt[:, h:h + 1])
            act(zbk[:, h:h + 1], CF('PJ'), AF.Exp, ['lgt', 'cf'], ['zbk'], scale=lgt[:, 4 + h:5 + h])
            act(gcf[:, h, :], CF('C128T', 64), AF.Exp, ['lgt', 'cf'], ['gcf'], scale=lgt[0:64, h:h + 1])
            act(gcb[:, h, :], CF('C128T', 64), AF.Exp, ['lgt', 'cf'], ['gcb'], scale=lgt[0:64, 4 + h:5 + h])

        S2 = alloc([64, 4, 64], F32)
        kz2 = alloc([128, 4, 64], BF16)
        tmb = [alloc([128, 640], BF16) for _ in range(2)]

        def mk_sweep(order, z, zk, gc, gk, S_all, sk, Sx, skey, kzx, kzkey, tmx, tmkey, psx, pskey):
            def init():
                mset('dve', Sx, 0.0, [skey])

            def step(ii):
                n = order[ii]
                r_ = ii % 2
                cp('act', S_all[:, n], Sx, [skey], [sk])
                dma('sp', tmx[r_], tmo[n * 128:(n + 1) * 128, :], [], [tmkey + str(r_)])
                tt('dve', kzx, tmx[r_][:, 384:640].rearrange("p (h d) -> p h d", h=4), z.unsqueeze(2).to_broadcast([128, 4, 64]), ALU.mult,
                   [tmkey + str(r_), zk], [kzkey])
                mms([(psx[0:64, h * 64:(h + 1) * 64], [(kzx[:, h, :], tmx[r_][:, 128 + h * 64:128 + (h + 1) * 64])]) for h in range(4)],
                    [kzkey, tmkey + str(r_)], [pskey])
                tt('dve', Sx, Sx, gc, ALU.mult, [skey, gk], [skey])
                tt('dve', Sx.rearrange("p h d -> p (h d)"), Sx.rearrange("p h d -> p (h d)"), psx[0:64, 0:256], ALU.add, [skey, pskey], [skey])
            return init, step

        fi, fs_ = mk_sweep(list(range(NT)), zf, 'zf', gcf, 'gcf', Sf_all, 'Sf', S, 'S', kz, 'kz', tmn, 'tmn', psf[0], PK[0])
        bi, bs_ = mk_sweep([1, 0] + list(range(NT - 1, 1, -1)), zbk, 'zbk', gcb, 'gcb', Sb_all, 'Sb', S2, 'S2', kz2, 'kz2', tmb, 'tmb', psf[4], PK[4])
        fi(); bi()
        for ii in range(NT):
            fs_(ii)
            bs_(ii)
        amL = [alloc([128, 4, 128], BF16) for _ in range(2)]
        qxfL = [alloc([64, 4, 128], BF16) for _ in range(2)]; qxbL = [alloc([64, 4, 128], BF16) for _ in range(2)]
        osqL = [alloc([64, 512], F32) for _ in range(2)]; rsL = [alloc([64, 512], F32) for _ in range(2)]; o1L = [alloc([64, 512], F32) for _ in range(2)]
        chunks = list(range(0 if with_ctx else 2, NT))

        def oload(ii):
            n = chunks[ii]
            r_ = ii % 2
            ns = slice(n * 128, (n + 1) * 128)
            dma('sp', qk[r_], qkT[640:1152, ns].rearrange("(h d) t -> d h t", d=64), [], ['qk%d' % r_])
            dma('sp', tmn[r_], tmo[ns, :], [], ['tmn%d' % r_])
            dma('sp', rgn[r_], rgT[:, ns].rearrange("(h e) t -> e h t", e=64), [], ['rgn%d' % r_])

        if chunks:
            oload(0)
        for ii, n in enumerate(chunks):
            if ii + 1 < len(chunks):
                oload(ii + 1)
            r_ = ii % 2
            q_ = str(r_)
            pA, pB, pC = psf[1 + 4 * r_], psf[2 + 4 * r_], psf[3 + 4 * r_]
            kA, kB, kC = PK[1 + 4 * r_], PK[2 + 4 * r_], PK[3 + 4 * r_]
            am_ = amL[r_]; qxf_ = qxfL[r_]; qxb_ = qxbL[r_]; osq_ = osqL[r_]; rs_ = rsL[r_]; o1_ = o1L[r_]
            ns = slice(n * 128, (n + 1) * 128)
            mms([(pA[:, h * 128:(h + 1) * 128], [(qk[r_][:, 4 + h, :], qk[r_][:, h, :])]) for h in range(4)], ['qk%d' % r_], [kA])
            tt('dve', am_.rearrange("p h t -> p (h t)"), pA[:, :], dcomb.rearrange("p h t -> p (h t)"), ALU.mult, [kA, 'dcomb'], ['am' + q_])
            tt('dve', qxf_, qk[r_][:, 0:4, :], xif, ALU.mult, ['qk%d' % r_, 'xif'], ['qxf' + q_])
            tt('pool', qxb_, qk[r_][:, 0:4, :], xib, ALU.mult, ['qk%d' % r_, 'xib'], ['qxb' + q_])
            mms([(pB[0:64, h * 128:(h + 1) * 128],
                  [(tmn[r_][:, 128 + h * 64:128 + (h + 1) * 64], am_[:, h, :]), (Sf_all[:, n, h, :], qxf_[:, h, :]), (Sb_all[:, n, h, :], qxb_[:, h, :])])
                 for h in range(4)], ['tmn%d' % r_, 'am' + q_, 'Sf', 'Sb', 'qxf' + q_, 'qxb' + q_], [kB])
            act(osq_, pB[0:64, :], AF.Square, [kB], ['osq' + q_])
            mmg(pC[0:64, :], [(CF('on64', 64), osq_)], ['osq' + q_, 'cf'], [kC])
            act(rs_, pC[0:64, :], AF.Sqrt, [kC], ['rs' + q_], bias=EPS)
            recip(rs_, rs_, ['rs' + q_], ['rs' + q_])
            tt('dve', o1_, pB[0:64, :], rs_, ALU.mult, [kB, 'rs' + q_], ['o1' + q_])
            o13 = o1_.rearrange("p (h t) -> p h t", h=4)
            tt('dve', o13, o13, grn.unsqueeze(2).to_broadcast([64, 4, 128]), ALU.mult, ['o1' + q_, 'grn'], ['o1' + q_])
            tt('pool', ob[r_], o13, rgn[r_], ALU.mult, ['o1' + q_, 'rgn%d' % r_], ['ob%d' % r_])
            dma('sp', brT[256:512, ns].rearrange("(h e) t -> e h t", e=64), ob[r_], ['ob%d' % r_], ['brT'])
        P.barrier()

    def stage_conv(i, with_ctx):
        areset()
        yb = alloc([128, 2, L + 30], BF16)
        ybc = alloc([128, 2, C + 30], BF16)
        diag = alloc([128, 2, 31, 128], BF16)
        dgw = alloc([128, 62], F32); cbp = alloc([128, 6], F32)
        z = alloc([128, 2, 512], F32); zq = alloc([128, 2, 512], F32)
        mu = alloc([128, 512], F32); var = alloc([128, 512], F32)
        co = [alloc([128, 2, 512], BF16) for _ in range(2)]
        dma('sp', dgw, dwa[i], [], ['dgw'])
        dma('sp', cbp, cba[i], [], ['cbp'])
        mset('pool', yb[:, :, 0:15], 0.0, ['yb_h0'])
        mset('pool', yb[:, :, L + 15:L + 30], 0.0, ['yb_h1'])
        mset('pool', ybc[:, :, 0:15], 0.0, ['ybc_h0'])
        mset('pool', ybc[:, :, C + 15:C + 30], 0.0, ['ybc_h1'])
        dma('sp', yb[:, :, 15:15 + L], cyT[:, 256:256 + L].rearrange("(c p) t -> p c t", p=128), [], ['yb'])
        dma('sp', ybc[:, :, 15:15 + C], cyT[:, 0:C].rearrange("(c p) t -> p c t", p=128), [], ['ybc'])
        for c_ in range(2):
            for tap in range(31):
                ts('dve', diag[:, c_, tap, :], CF('identF'), dgw[:, c_ * 31 + tap:c_ * 31 + tap + 1], None, ALU.mult, None, ['cf', 'dgw'], ['diag'])
        for gi, (g0, N) in enumerate(groups):
            if gi == 0 and not with_ctx:
                continue
            buf, bk, off = (ybc, ['ybc', 'ybc_h0', 'ybc_h1'], 0) if gi == 0 else (yb, ['yb', 'yb_h0', 'yb_h1'], g0 - 256)
            r_ = gi % 2
            for c_ in range(2):
                mmg(psf[c_][:, 0:N], [(diag[:, c_, tap, :], buf[:, c_, off + tap:off + tap + N]) for tap in range(31)], ['diag'] + bk, [PK[c_]])
                act(z[:, c_, 0:N], psf[c_][:, 0:N], AF.Identity, [PK[c_], 'cbp'], ['z%d' % c_], bias=cbp[:, c_ * 3:c_ * 3 + 1])
                act(zq[:, c_, 0:N], z[:, c_, 0:N], AF.Square, ['z%d' % c_], ['zq%d' % c_])
            mmg(psf[2][:, 0:N], [(CF('on128'), z[:, c_, 0:N]) for c_ in range(2)], ['z0', 'z1', 'cf'], [PK[2]])
            mmg(psf[3][:, 0:N], [(CF('on128'), zq[:, c_, 0:N]) for c_ in range(2)], ['zq0', 'zq1', 'cf'], [PK[3]])
            cp('dve', mu[:, 0:N], psf[2][:, 0:N], [PK[2]], ['mu'])
            tt('dve', var[:, 0:N], mu[:, 0:N], mu[:, 0:N], ALU.mult, ['mu'], ['var'])
            tt('dve', var[:, 0:N], psf[3][:, 0:N], var[:, 0:N], ALU.subtract, [PK[3], 'var'], ['var'])
            act(var[:, 0:N], var[:, 0:N], AF.Sqrt, ['var'], ['var'], bias=EPS)
            recip(var[:, 0:N], var[:, 0:N], ['var'], ['var'])
            for c_ in range(2):
                zk = 'z%d' % c_
                tt('dve', z[:, c_, 0:N], z[:, c_, 0:N], mu[:, 0:N], ALU.subtract, [zk, 'mu'], [zk])
                tt('dve', z[:, c_, 0:N], z[:, c_, 0:N], var[:, 0:N], ALU.mult, [zk, 'var'], [zk])
                ts('dve', z[:, c_, 0:N], z[:, c_, 0:N], cbp[:, c_ * 3 + 1:c_ * 3 + 2], cbp[:, c_ * 3 + 2:c_ * 3 + 3], ALU.mult, ALU.add, [zk, 'cbp'], [zk])
                act(co[r_][:, c_, 0:N], z[:, c_, 0:N], AF.Silu, [zk], ['co%d' % r_])
            dma('sp', brT[1024:1280, g0:g0 + N].rearrange("(c p) t -> p c t", p=128), co[r_][:, :, 0:N], ['co%d' % r_], ['brT'])
        P.barrier()

    def stage_merge(i, with_ctx, own=False):
        layer0 = (i == 0)
        areset()
        Asrc = aTq1 if own else aT
        Bsrc = brTq if own else brT
        mgl = [(0, g0, N) for (g0, N) in qgroups] if own else [((1 if gi_ == 0 else 0), g0, N) for gi_, (g0, N) in enumerate(groups) if not (gi_ == 0 and not with_ctx)]
        if own:
            idx = alloc([128, LQ // 128], U32)
            dma('sp', idx, own_idx, [], ['idx'])
        wg = alloc([128, 8, 4096], BF16)
        wbr = alloc([128, 10, 1024], BF16)
        wo = alloc([128, 8, 1024], BF16)
        dma('pool', wg, w_in[i, :, 2560:6656].rearrange("(k p) n -> p k n", p=128), [], ['wg'])
        dma('pool', wbr[:, 0:2, :], fnet_w[i].rearrange("(k p) n -> p k n", p=128), [], ['wbr0'])
        dma('pool', wbr[:, 2:4, :], ret_w[i].rearrange("(k p) n -> p k n", p=128), [], ['wbr1'])
        dma('pool', wbr[:, 4:8, :], attn_w[i].rearrange("(k p) n -> p k n", p=128), [], ['wbr2'])
        dma('pool', wbr[:, 8:10, :], conv_wo[i].rearrange("(k p) n -> p k n", p=128), [], ['wbr3'])
        dma('pool', wo, w_out[i].rearrange("(k p) n -> p k n", p=128), [], ['wo'])
        gate = [alloc([128, 1024], F32) for _ in range(2)]
        for wh in range(2):
            dma('sp', gate[wh], modr[wh, :, 2048:3072], [], ['gate%d' % wh])
        at = [alloc([128, 8, 512], BF16) for _ in range(2)]
        bt = [alloc([128, 10, 512], BF16) for _ in range(2)]
        sg = [alloc([128, 512], BF16) for _ in range(2)]
        macc = alloc([128, 512], F32); tmp = alloc([128, 512], F32)
        mg = alloc([128, 8, 512], BF16)
        ht = [alloc([128, 1024], F32) for _ in range(2)]
        tq = alloc([128, 1024], F32)
        KB = {0: [0, 1], 1: [2, 3], 2: [4, 5, 6, 7], 3: [8, 9]}
        tix = 0
        mtiles = [g0 // 128 + j for (wh_, g0, N) in mgl for j in range(N // 128)]

        def mload(k):
            t_ = mtiles[k]
            if own:
                P.add('pool', (lambda e, o=ht[k % 2], ix=idx[:, t_:t_ + 1]: e.indirect_dma_start(
                    out=o, out_offset=None, in_=hb, in_offset=bass.IndirectOffsetOnAxis(ap=ix, axis=0))),
                    ['idx'], ['ht%d' % (k % 2)], dma=True)
            else:
                dma('sp', ht[k % 2], hsrc(layer0, t_), ['hb%d' % t_], ['ht%d' % (k % 2)])

        def gload(q):
            wh_, g0_, N_ = mgl[q]
            dma('sp', at[q % 2][:, :, 0:N_], Asrc[:, g0_:g0_ + N_].rearrange("(k p) t -> p k t", p=128), [], ['at%d' % (q % 2)])
            dma('sp', bt[q % 2][:, :, 0:N_], Bsrc[:, g0_:g0_ + N_].rearrange("(k p) t -> p k t", p=128), [], ['bt%d' % (q % 2)])

        gload(0)
        for q, (wh, g0, N) in enumerate(mgl):
            r_ = q % 2
            if q + 1 < len(mgl):
                gload(q + 1)
            for fc in range(8):
                for b in range(4):
                    mmg(psf[b % 2][:, 0:N], [(wg[:, k, b * 1024 + fc * 128:b * 1024 + (fc + 1) * 128], at[r_][:, k, 0:N]) for k in range(8)],
                        ['wg', 'at%d' % r_], [PK[b % 2]])
                    act(sg[b % 2][:, 0:N], psf[b % 2][:, 0:N], AF.Sigmoid, [PK[b % 2]], ['sg%d' % (b % 2)])
                    mmg(psf[2 + b % 2][:, 0:N], [(wbr[:, kb, fc * 128:(fc + 1) * 128], bt[r_][:, kb, 0:N]) for kb in KB[b]],
                        ['wbr%d' % b, 'bt%d' % r_], [PK[2 + b % 2]])
                    if b == 0:
                        tt('dve', macc[:, 0:N], psf[2][:, 0:N], sg[0][:, 0:N], ALU.mult, [PK[2], 'sg0'], ['macc'])
                    else:
                        tt('dve', tmp[:, 0:N], psf[2 + b % 2][:, 0:N], sg[b % 2][:, 0:N], ALU.mult, [PK[2 + b % 2], 'sg%d' % (b % 2)], ['tmp'])
                        if b < 3:
                            tt('pool', macc[:, 0:N], macc[:, 0:N], tmp[:, 0:N], ALU.add, ['macc', 'tmp'], ['macc'])
                        else:
                            tt('pool', mg[:, fc, 0:N], macc[:, 0:N], tmp[:, 0:N], ALU.add, ['macc', 'tmp'], ['mg'])
            for j in range(N // 128):
                t = g0 // 128 + j
                h_ = tix % 2
                tix += 1
                js = slice(j * 128, (j + 1) * 128)
                if tix == 1:
                    mload(0)
                if tix < len(mtiles):
                    mload(tix)
                for half in range(2):
                    hs = slice(half * 512, (half + 1) * 512)
                    mmg(psf[4 + half][:, :], [(mg[:, fc, js], wo[:, fc, hs]) for fc in range(8)], ['mg', 'wo'], [PK[4 + half]])
                    tt('dve', tq[:, hs], psf[4 + half][:, :], gate[wh][:, hs], ALU.mult, [PK[4 + half], 'gate%d' % wh], ['tq'])
                    tt('pool', ht[h_][:, hs], ht[h_][:, hs], tq[:, hs], ALU.add, ['ht%d' % h_, 'tq'], ['ht%d' % h_])
                dma('sp', (hq if own else hb)[t * 128:(t + 1) * 128, :], ht[h_], ['ht%d' % h_], ['hb%d' % t])
        P.barrier()

    def stage_fm2tm(i):
        areset()
        fm = [alloc([128, 4, 128], BF16) for _ in range(2)]
        tmr = [alloc([128, 512], BF16) for _ in range(2)]
        for t in range(2, NT):
            r_ = t % 2
            tsl = slice(t * 128, (t + 1) * 128)
            dma('sp', fm[r_][:, 0:2, :], brT[256:512, tsl].rearrange("(c p) t -> p c t", p=128), [], ['fm%da' % r_])
            dma('sp', fm[r_][:, 2:4, :], brT[1024:1280, tsl].rearrange("(c p) t -> p c t", p=128), [], ['fm%db' % r_])
            trs([(psb[:, c_ * 128:(c_ + 1) * 128], fm[r_][:, c_, :]) for c_ in range(4)], ['fm%da' % r_, 'fm%db' % r_, 'cb'], ['ps7'])
            cp('act', tmr[r_], psb[:, 0:512], ['ps7'], ['tmr%d' % r_])
            dma('sp', rctm[tsl, :], tmr[r_], ['tmr%d' % r_], ['rctm'])
        P.barrier()

    def stage_compact(i, srcs):
        areset()
        idx = alloc([128, LQ // 128], U32)
        dma('sp', idx, own_idx, [], ['idx'])
        gbuf = {}
        cbuf = {}
        for si, (src, W, dsts) in enumerate(srcs):
            gbuf[si] = [alloc([128, W], BF16) for _ in range(2)]
            cbuf[si] = [alloc([128, W // 128, 128], BF16) for _ in range(2)]
        for j in range(LQ // 128):
            r_ = j % 2
            jsl = slice(j * 128, (j + 1) * 128)
            for si, (src, W, dsts) in enumerate(srcs):
                gk = 'g%d_%d' % (si, r_)
                ck = 'c%d_%d' % (si, r_)
                P.add('pool', (lambda e, o=gbuf[si][r_], ix=idx[:, j:j + 1], src=src: e.indirect_dma_start(
                    out=o, out_offset=None, in_=src, in_offset=bass.IndirectOffsetOnAxis(ap=ix, axis=0))),
                    ['idx'], [gk], dma=True)
                nb = W // 128
                trs([(psb[:, b_ * 128:(b_ + 1) * 128], gbuf[si][r_][:, b_ * 128:(b_ + 1) * 128]) for b_ in range(nb)], [gk, 'cb'], ['ps7'])
                cp('act', cbuf[si][r_], psb[:, 0:W].rearrange("p (k t) -> p k t", k=nb), ['ps7'], [ck])
                for (dst, row0, blk0, nblk) in dsts:
                    dma('sp', dst[row0:row0 + nblk * 128, jsl].rearrange("(c p) t -> p c t", p=128), cbuf[si][r_][:, blk0:blk0 + nblk, :], [ck], ['cdst'])
        P.barrier()

    def stage_ffn_norm(i, with_ctx, moe):
        areset()
        gs, sh = load_mod(i, n2g, 3, 4)
        ht = [alloc([128, 1024], F32) for _ in range(2)]
        ss = [alloc([128, 1], F32) for _ in range(2)]
        junk = alloc([128, 1024], BF16)
        a1 = alloc([128, 1024], F32); a2 = alloc([128, 1024], BF16)
        agrp = [alloc([128, 8, 512], BF16) for _ in range(2)]
        if moe:
            a2f = alloc([128, 1024], F32)
            rw = alloc([128, NE, 1024], F32)
            junk2 = alloc([128, 1024], F32)
            lg = alloc([128, 8], F32); lg2 = alloc([128, 8], F32)
            m1 = alloc([128, 1], F32); m2 = alloc([128, 1], F32); dd = alloc([128, 1], F32); w1 = alloc([128, 1], F32)
            eq1 = alloc([128, 8], F32); eq2 = alloc([128, 8], F32)
            idx = alloc([128, LQ // 128], U32)
            dma('sp', rw, rwT.partition_broadcast(128), [], ['rw'])
            dma('sp', idx, own_idx, [], ['idx'])
            glist = [(0, k * 512, 512) for k in range(LQ // 512)]
        else:
            glist = [((1 if gi == 0 else 0), g0, N) for gi, (g0, N) in enumerate(groups) if not (gi == 0 and not with_ctx)]
        tiles = [(gi, j) for gi, (wh, g0, N) in enumerate(glist) for j in range(N // 128)]

        def load(k):
            gi, j = tiles[k]
            wh, g0, N = glist[gi]
            t = g0 // 128 + j
            r_ = k % 2
            dma('sp', ht[r_], (hq if moe else hb)[t * 128:(t + 1) * 128, :], [], ['r%dht' % r_])

        if tiles:
            load(0)
        for k in range(len(tiles)):
            if k + 1 < len(tiles):
                load(k + 1)
            gi, j = tiles[k]
            wh, g0, N = glist[gi]
            t = g0 // 128 + j
            r_ = k % 2
            k_ = 'r%d' % r_
            gb = gi % 2
            ag = agrp[gb]
            AK = 'ag%d' % gb
            js = slice(j * 128, (j + 1) * 128)
            norm_mod_tile(t, wh, ht[r_], ss[r_], a1, a2, gs, sh, junk, k_, a2f=(a2f if moe else None))
            trs([(psb[:, kk * 128:(kk + 1) * 128], a2[:, kk * 128:(kk + 1) * 128]) for kk in range(8)], ['a2', 'cb'], ['ps7'])
            cp('act', ag[:, :, js], psb[:, :].rearrange("p (k t) -> p k t", k=8), ['ps7'], [AK])
            if moe:
                for e_ in range(NE):
                    tt('dve', junk2, a2f, rw[:, e_, :], ALU.mult, ['a2f', 'rw'], ['junk2'])
                    red(lg[:, e_:e_ + 1], junk2, ALU.add, ['junk2'], ['lg'])
                red(m1, lg, ALU.max, ['lg'], ['m1'])
                ts('dve', eq1, lg, m1[:, 0:1], None, ALU.is_equal, None, ['lg', 'm1'], ['eq1'])
                stt(lg2, eq1, -1e30, lg, ALU.mult, ALU.add, ['eq1', 'lg'], ['lg2'])
                red(m2, lg2, ALU.max, ['lg2'], ['m2'])
                ts('dve', eq2, lg2, m2[:, 0:1], None, ALU.is_equal, None, ['lg2', 'm2'], ['eq2'])
                tt('dve', dd, m2, m1, ALU.subtract, ['m1', 'm2'], ['dd'])
                act(dd, dd, AF.Sigmoid, ['dd'], ['dd'])
                ts('dve', w1, dd, -1.0, 1.0, ALU.mult, ALU.add, ['dd'], ['w1'])
                ts('dve', eq1, eq1, w1[:, 0:1], None, ALU.mult, None, ['eq1', 'w1'], ['eq1'])
                stt(wts[:, t, :], eq2, dd[:, 0:1], eq1, ALU.mult, ALU.add, ['eq2', 'dd', 'eq1'], ['wts'])
            if j == N // 128 - 1:
                dst = aTq if moe else aT
                dma('sp', dst[:, g0:g0 + N].rearrange("(k p) t -> p k t", p=128), ag[:, :, 0:N], [AK], ['aT'])
        P.barrier()

    def stage_ffn_pass(i, with_ctx, wgd, wud, wdd, hp, expert, final):
        areset()
        moe = expert is not None
        c0 = hp * CH * 128
        wgt = alloc([128, 8, CH * 128], BF16); wut = alloc([128, 8, CH * 128], BF16); wdt = alloc([128, CH, 1024], BF16)
        dma('pool', wgt, wgd[:, c0:c0 + CH * 128].rearrange("(k p) n -> p k n", p=128), [], ['wgt'])
        dma('pool', wut, wud[:, c0:c0 + CH * 128].rearrange("(k p) n -> p k n", p=128), [], ['wut'])
        dma('pool', wdt, wdd[c0:c0 + CH * 128, :].rearrange("(k p) n -> p k n", p=128), [], ['wdt'])
        gate = [alloc([128, 1024], F32) for _ in range(2)]
        for wh in range(2):
            dma('sp', gate[wh], modr[wh, :, 5120:6144], [], ['gate%d' % wh])
        ft = [alloc([128, 8, 512], BF16) for _ in range(2)]
        sl = [alloc([128, 512], BF16) for _ in range(2)]
        actT = alloc([128, CH, 512], BF16)
        ht = [alloc([128, 1024], F32) for _ in range(2)]
        tq = alloc([128, 1024], F32)
        if moe:
            glist = [(0, k * 512, 512) for k in range(LQ // 512)]
            hsrc_, asrc_ = hq, aTq
        else:
            glist = [((1 if gi == 0 else 0), g0, N) for gi, (g0, N) in enumerate(groups) if not (gi == 0 and not with_ctx)]
            hsrc_, asrc_ = hb, aT
        tiles = [(gi, j) for gi, (wh, g0, N) in enumerate(glist) for j in range(N // 128)]

        def load_ft(gi):
            wh, g0, N = glist[gi]
            dma('sp', ft[gi % 2][:, :, 0:N], asrc_[:, g0:g0 + N].rearrange("(k p) t -> p k t", p=128), [], ['ft%d' % (gi % 2)])

        def load_ht(k):
            gi, j = tiles[k]
            wh, g0, N = glist[gi]
            t = g0 // 128 + j
            dma('sp', ht[k % 2], hsrc_[t * 128:(t + 1) * 128, :], ['hb%d' % t], ['ht%d' % (k % 2)])

        load_ft(0)
        load_ht(0)
        k = 0
        for gi, (wh, g0, N) in enumerate(glist):
            r_ = gi % 2
            if gi + 1 < len(glist):
                load_ft(gi + 1)
            for c_ in range(CH):
                q_ = c_ % 2
                cs_ = slice(c_ * 128, (c_ + 1) * 128)
                mmg(psf[q_][:, 0:N], [(wgt[:, kk, cs_], ft[r_][:, kk, 0:N]) for kk in range(8)], ['wgt', 'ft%d' % r_], [PK[q_]])
                mmg(psf[2 + q_][:, 0:N], [(wut[:, kk, cs_], ft[r_][:, kk, 0:N]) for kk in range(8)], ['wut', 'ft%d' % r_], [PK[2 + q_]])
                act(sl[q_][:, 0:N], psf[q_][:, 0:N], AF.Silu, [PK[q_]], ['sl%d' % q_])
                tt('dve', actT[:, c_, 0:N], psf[2 + q_][:, 0:N], sl[q_][:, 0:N], ALU.mult, [PK[2 + q_], 'sl%d' % q_], ['actT'])
            for j in range(N // 128):
                t = g0 // 128 + j
                h_ = k % 2
                if k + 1 < len(tiles):
                    load_ht(k + 1)
                k += 1
                js = slice(j * 128, (j + 1) * 128)
                for half in range(2):
                    hs = slice(half * 512, (half + 1) * 512)
                    mmg(psf[4 + half][:, :], [(actT[:, c_, js], wdt[:, c_, hs]) for c_ in range(CH)], ['actT', 'wdt'], [PK[4 + half]])
                    if not moe:
                        tt('dve', tq[:, hs], psf[4 + half][:, :], gate[wh][:, hs], ALU.mult, [PK[4 + half], 'gate%d' % wh], ['tq'])
                    else:
                        stt(tq[:, hs], psf[4 + half][:, :], wts[:, t, expert:expert + 1], gate[wh][:, hs], ALU.mult, ALU.mult,
                            [PK[4 + half], 'gate%d' % wh, 'wts'], ['tq'])
                    tt('pool', ht[h_][:, hs], ht[h_][:, hs], tq[:, hs], ALU.add, ['ht%d' % h_, 'tq'], ['ht%d' % h_])
                if final:
                    dma('sp', out[t * 128:(t + 1) * 128, :], ht[h_], ['ht%d' % h_], ['out'])
                else:
                    dma('sp', hsrc_[t * 128:(t + 1) * 128, :], ht[h_], ['ht%d' % h_], ['hb%d' % t])
        P.barrier()

    def stage_moe(i):
        areset()
        wset = [(alloc([128, 8, CH * 128], BF16), alloc([128, 8, CH * 128], BF16), alloc([128, CH, 1024], BF16)) for _ in range(2)]
        gate0 = alloc([128, 1024], F32)
        dma('sp', gate0, modr[0, :, 5120:6144], [], ['gate0'])
        ftL = [alloc([128, 8, 512], BF16) for _ in range(2)]
        sl = [alloc([128, 512], BF16) for _ in range(2)]
        actT = alloc([128, CH, 512], BF16)
        ht = [alloc([128, 1024], F32) for _ in range(2)]
        tq = alloc([128, 1024], F32)
        passes = [(e_, hp) for e_ in range(NE) for hp in range(HP)]
        glist = [(k_ * 512, 512) for k_ in range(LQ // 512)]
        tiles = [(gi, j) for gi, (g0, N) in enumerate(glist) for j in range(N // 128)]
        seq = [(pi, k) for pi in range(len(passes)) for k in range(len(tiles))]

        def loadw(pi):
            e_, hp = passes[pi]
            c0 = hp * CH * 128
            w_ = wset[pi % 2]
            sfx = str(pi % 2)
            dma('pool', w_[0], moe_wg[0, e_][:, c0:c0 + CH * 128].rearrange("(k p) n -> p k n", p=128), [], ['wgt' + sfx])
            dma('pool', w_[1], moe_wu[0, e_][:, c0:c0 + CH * 128].rearrange("(k p) n -> p k n", p=128), [], ['wut' + sfx])
            dma('pool', w_[2], moe_wd[0, e_][c0:c0 + CH * 128, :].rearrange("(k p) n -> p k n", p=128), [], ['wdt' + sfx])

        def load_ht(si):
            pi, k = seq[si]
            gi, j = tiles[k]
            t = glist[gi][0] // 128 + j
            dma('sp', ht[si % 2], hq[t * 128:(t + 1) * 128, :], ['hb%d' % t], ['ht%d' % (si % 2)])

        gseq = [(pi_, gi_) for pi_ in range(len(passes)) for gi_ in range(len(glist))]

        def load_ft(gq):
            g0_, N_ = glist[gseq[gq][1]]
            dma('sp', ftL[gq % 2][:, :, 0:N_], aTq[:, g0_:g0_ + N_].rearrange("(k p) t -> p k t", p=128), [], ['ft%d' % (gq % 2)])

        loadw(0)
        load_ht(0)
        load_ft(0)
        si = 0
        gq = 0
        for pi, (e_, hp) in enumerate(passes):
            if pi + 1 < len(passes):
                loadw(pi + 1)
            wgt, wut, wdt = wset[pi % 2]
            sfx = str(pi % 2)
            final = (pi == len(passes) - 1)
            for gi, (g0, N) in enumerate(glist):
                ft = ftL[gq % 2]
                fk = 'ft%d' % (gq % 2)
                if gq + 1 < len(gseq):
                    load_ft(gq + 1)
                gq += 1
                for c_ in range(CH):
                    q_ = c_ % 2
                    cs_ = slice(c_ * 128, (c_ + 1) * 128)
                    mmg(psf[q_][:, 0:N], [(wgt[:, kk, cs_], ft[:, kk, 0:N]) for kk in range(8)], ['wgt' + sfx, fk], [PK[q_]])
                    mmg(psf[2 + q_][:, 0:N], [(wut[:, kk, cs_], ft[:, kk, 0:N]) for kk in range(8)], ['wut' + sfx, fk], [PK[2 + q_]])
                    act(sl[q_][:, 0:N], psf[q_][:, 0:N], AF.Silu, [PK[q_]], ['sl%d' % q_])
                    tt('dve', actT[:, c_, 0:N], psf[2 + q_][:, 0:N], sl[q_][:, 0:N], ALU.mult, [PK[2 + q_], 'sl%d' % q_], ['actT'])
                for j in range(N // 128):
                    t = g0 // 128 + j
                    h_ = si % 2
                    if si + 1 < len(seq):
                        load_ht(si + 1)
                    si += 1
                    js = slice(j * 128, (j + 1) * 128)
                    for half in range(2):
                        hs = slice(half * 512, (half + 1) * 512)
                        mmg(psf[4 + half][:, :], [(actT[:, c_, js], wdt[:, c_, hs]) for c_ in range(CH)], ['actT', 'wdt' + sfx], [PK[4 + half]])
                        stt(tq[:, hs], psf[4 + half][:, :], wts[:, t, e_:e_ + 1], gate0[:, hs], ALU.mult, ALU.mult,
                            [PK[4 + half], 'gate0', 'wts'], ['tq'])
                        tt('pool', ht[h_][:, hs], ht[h_][:, hs], tq[:, hs], ALU.add, ['ht%d' % h_, 'tq'], ['ht%d' % h_])
                    if final:
                        dma('sp', out[t * 128:(t + 1) * 128, :], ht[h_], ['ht%d' % h_], ['out'])
                    else:
                        dma('sp', hq[t * 128:(t + 1) * 128, :], ht[h_], ['ht%d' % h_], ['hb%d' % t])
        P.barrier()

    P.barrier()
    maxstage = int(os.environ.get('KSTAGES', '999'))
    sc = {'n': 0}

    def S(fn, *a, **kw):
        if sc['n'] < maxstage:
            fn(*a, **kw)
        sc['n'] += 1

    for i in range(2):
        with_ctx = (i == 0)
        S(stage_mod, i)
        print("ops after mod", P.nadd)
        S(stage_proj, i)
        print("ops after proj", P.nadd)
        if i == 0:
            S(stage_attn, i, with_ctx)
            S(stage_fnet, i, with_ctx)
            S(stage_ret, i, with_ctx)
            S(stage_conv, i, with_ctx)
            S(stage_merge, i, with_ctx)
        else:
            S(stage_compact, i, [(qtm, 512, [(qTq, 0, 0, 4)]), (atm, 1024, [(aTq1, 0, 0, 8)])])
            S(stage_attn, i, with_ctx, own=True)
            S(stage_fnet, i, with_ctx, tr=False)
            S(stage_ret, i, with_ctx)
            S(stage_conv, i, with_ctx)
            S(stage_fm2tm, i)
            S(stage_compact, i, [(ytm, 256, [(brTq, 0, 0, 2)]), (rctm, 512, [(brTq, 256, 0, 2), (brTq, 1024, 2, 2)])])
            S(stage_merge, i, with_ctx, own=True)
        if i == 0:
            S(stage_ffn_norm, i, with_ctx, moe=False)
            for hp in range(HP):
                S(stage_ffn_pass, i, with_ctx, ffn_wg[0], ffn_wu[0], ffn_wd[0], hp, None, False)
        else:
            S(stage_ffn_norm, i, with_ctx, moe=True)
            S(stage_moe, i)
    P.add('sp', None, ['out'], [])
    P.emit()
    P.close()
    return nc


def host_inputs(inp, L, b):
    f = lambda a: np.ascontiguousarray(np.asarray(a), dtype=np.float32)
    cf, cb, rope = make_consts(L)
    c = f(inp["c"])[b]; cc = f(inp["c_ctx"])
    ccols = np.concatenate([c.reshape(8, 128).T, cc.reshape(8, 128).T], 1)
    dec2 = np.concatenate([f(inp["ret_decay_fwd"]), f(inp["ret_decay_bwd"])], 1)
    rng = f(inp["ret_norm_g"]).reshape(2, 4, 64).transpose(0, 2, 1)
    dwa = f(inp["conv_dw_w"]).reshape(2, 31, 2, 128).transpose(0, 3, 2, 1).reshape(2, 128, 62)
    cba = np.stack([f(inp["conv_dw_b"]), f(inp["conv_ln_g"]), f(inp["conv_ln_b"])], -1).reshape(2, 2, 128, 3).transpose(0, 2, 1, 3).reshape(2, 128, 6)
    gqk = np.concatenate([np.tile(f(inp["attn_qn_g"]), (1, 8)), np.tile(f(inp["attn_kn_g"]), (1, 2))], 1)
    rwT = f(inp["router_w"])[0].T
    d = {"x": f(inp["x"])[b], "ctx": f(inp["ctx"])[b], "ccols": ccols, "dec2": dec2, "rng": rng, "dwa": dwa, "cba": cba,
         "gqk": gqk, "rwT": rwT, "cf": cf, "cb": cb, "rope": rope}
    for k in ["ada_w", "ada_b", "norm1_g", "norm2_g", "w_in", "fnet_w", "ret_w", "attn_w", "conv_w_out", "w_out",
              "ffn_w_gate", "ffn_w_up", "ffn_w_down", "moe_w_gate", "moe_w_up", "moe_w_down"]:
        d[k] = f(inp[k])
    return {k: np.ascontiguousarray(v, dtype=np.float32) for k, v in d.items()}


def run(inputs, debug=False):
    L = int(np.asarray(inputs["x"]).shape[1])
    FD = int(np.asarray(inputs["ffn_w_gate"]).shape[2])
    NQ = 4
    LQ = L // NQ
    nc = build(L, FD // 128, debug=debug, NQ=NQ)
    base = [host_inputs(inputs, L, b) for b in range(2)]
    in_maps = []
    for cid in range(2 * NQ):
        b, q = cid // NQ, cid % NQ
        d = dict(base[b])
        jj = np.arange(LQ // 128)[None, :]
        pp = np.arange(128)[:, None]
        d["own_idx"] = np.ascontiguousarray((256 + q * LQ + jj * 128 + pp).astype(np.uint32))
        in_maps.append(d)
    res = run_bass_kernel_spmd(nc, in_maps, core_ids=list(range(2 * NQ)))
    outp = np.zeros((2, L, 1024), np.float32)
    for cid in range(2 * NQ):
        b, q = cid // NQ, cid % NQ
        outp[b, q * LQ:(q + 1) * LQ] = np.asarray(res.results[cid]["out"], dtype=np.float32)
    if debug:
        return outp, res.results
    return outp


def kernel(**inputs):
    return run(inputs)
```
